# Optimizing a Trainium2 kernel written in Bass

```python
import jax, jax.numpy as jnp
from jax import lax
import numpy as np

D_MODEL = 1024
BATCH = 4
SEQ = 4096
DEPTH = 1

D_MIX = 2 * D_MODEL
D_FNET = D_MIX // 4
N_FNET_GROUPS = 4
FNET_GROUP = D_FNET // N_FNET_GROUPS
D_SSM = D_MIX - D_FNET
SSM_HEAD_DIM = 64
N_SSM_HEADS = D_SSM // SSM_HEAD_DIM
N_BC_GROUPS = 4
HEADS_PER_GROUP = N_SSM_HEADS // N_BC_GROUPS
D_STATE = 128
CONV_WIDTH = 5
SSD_CHUNK = 128
D_CONV = D_SSM + 2 * N_BC_GROUPS * D_STATE
D_IN_PROJ = D_FNET + D_SSM + D_CONV + 2 * N_SSM_HEADS

PEER_HEADS = 8
PEER_KEYS = 128
PEER_EXPERTS = PEER_KEYS * PEER_KEYS
PEER_QUERY_DIM = 256
PEER_HALF = PEER_QUERY_DIM // 2
PEER_TOPK = 16
PEER_TOKEN_BLOCK = 128

EPS = 1e-6
DT_MIN = 0.001
DT_MAX = 0.1

kernel_name = "fnet_ssd_peer_adaln_hybrid"


def rms_norm(x, gain):
    xf = x.astype(jnp.float32)
    y = xf * lax.rsqrt(jnp.mean(xf * xf, axis=-1, keepdims=True) + EPS)
    return (y * gain.astype(jnp.float32)).astype(x.dtype)


def modulate(h, shift, scale):
    return h * (1 + scale[:, None, :]) + shift[:, None, :]


def fnet_mix(h):
    b, s, _ = h.shape
    hg = h.astype(jnp.float32).reshape(b, s, N_FNET_GROUPS, FNET_GROUP)
    f = jnp.fft.fft2(hg, axes=(1, 3), norm="ortho").real
    return f.reshape(b, s, D_FNET).astype(h.dtype)


def centred_depthwise_conv(u, w, bias):
    pad = CONV_WIDTH // 2
    out = lax.conv_general_dilated(
        u, w[:, None, :], window_strides=(1,), padding=[(pad, pad)],
        dimension_numbers=("NWC", "WIO", "NWC"), feature_group_count=u.shape[-1])
    return out + bias


def segsum(a):
    t = a.shape[-1]
    rep = jnp.broadcast_to(a[..., :, None], a.shape + (t,))
    strict = jnp.tril(jnp.ones((t, t), dtype=bool), -1)
    cs = jnp.cumsum(jnp.where(strict, rep, 0.0), axis=-2)
    lower = jnp.tril(jnp.ones((t, t), dtype=bool), 0)
    return jnp.where(lower, cs, -jnp.inf)


def ssd_scan(xdt, adt, bm, cm):
    b, s, g, r, p = xdt.shape
    l = SSD_CHUNK
    c = s // l
    X = xdt.reshape(b, c, l, g, r, p)
    A = jnp.transpose(adt.reshape(b, c, l, g, r), (0, 3, 4, 1, 2))
    Bc = bm.reshape(b, c, l, g, -1)
    Cc = cm.reshape(b, c, l, g, -1)
    A_cs = jnp.cumsum(A, axis=-1)
    Lmat = jnp.exp(segsum(A))
    CB = jnp.einsum("bclgn,bcsgn->bgcls", Cc, Bc)
    y_diag = jnp.einsum("bgcls,bgrcls,bcsgrp->bclgrp", CB, Lmat, X)
    decay_states = jnp.exp(A_cs[..., -1:] - A_cs)
    states = jnp.einsum("bclgn,bgrcl,bclgrp->bcgrpn", Bc, decay_states, X)
    states = jnp.concatenate([jnp.zeros_like(states[:, :1]), states], axis=1)
    chunk_tot = jnp.pad(A_cs[..., -1], ((0, 0), (0, 0), (0, 0), (1, 0)))
    decay_chunk = jnp.exp(segsum(chunk_tot))
    new_states = jnp.einsum("bgrzc,bcgrpn->bzgrpn", decay_chunk, states)
    states = new_states[:, :-1]
    y_off = jnp.einsum("bclgn,bcgrpn,bgrcl->bclgrp", Cc, states, jnp.exp(A_cs))
    return (y_diag + y_off).reshape(b, s, g, r, p)


def ssd_direction(xs, bm, cm, dt_raw, a_log, dt_bias):
    b, s = xs.shape[:2]
    dt = jax.nn.softplus(dt_raw + dt_bias)
    a = -jnp.exp(a_log)
    xdt = (xs * dt[..., None]).reshape(b, s, N_BC_GROUPS, HEADS_PER_GROUP, SSM_HEAD_DIM)
    adt = (dt * a).reshape(b, s, N_BC_GROUPS, HEADS_PER_GROUP)
    return ssd_scan(xdt, adt, bm, cm).reshape(b, s, N_SSM_HEADS, SSM_HEAD_DIM)


def bidirectional_ssd(z, xbc, dt_raw, conv_w, conv_b, a_log_fwd, a_log_bwd,
                      dt_bias_fwd, dt_bias_bwd, d_skip, ssm_norm_g):
    b, s, _ = xbc.shape
    xbc = jax.nn.silu(centred_depthwise_conv(xbc, conv_w, conv_b)).astype(jnp.float32)
    xs = xbc[..., :D_SSM].reshape(b, s, N_SSM_HEADS, SSM_HEAD_DIM)
    bm = xbc[..., D_SSM:D_SSM + N_BC_GROUPS * D_STATE].reshape(b, s, N_BC_GROUPS, D_STATE)
    cm = xbc[..., D_SSM + N_BC_GROUPS * D_STATE:].reshape(b, s, N_BC_GROUPS, D_STATE)
    dt_raw = dt_raw.astype(jnp.float32)
    y_f = ssd_direction(xs, bm, cm, dt_raw[..., :N_SSM_HEADS],
                        a_log_fwd.astype(jnp.float32), dt_bias_fwd.astype(jnp.float32))
    y_b = jnp.flip(ssd_direction(jnp.flip(xs, 1), jnp.flip(bm, 1), jnp.flip(cm, 1),
                                 jnp.flip(dt_raw[..., N_SSM_HEADS:], 1),
                                 a_log_bwd.astype(jnp.float32), dt_bias_bwd.astype(jnp.float32)), 1)
    y = y_f + y_b + xs * d_skip.astype(jnp.float32)[:, None]
    y = y.reshape(b, s, D_SSM) * jax.nn.silu(z.astype(jnp.float32))
    yg = y.reshape(b, s, N_BC_GROUPS, D_SSM // N_BC_GROUPS)
    yg = yg * lax.rsqrt(jnp.mean(yg * yg, axis=-1, keepdims=True) + EPS)
    y = yg.reshape(b, s, D_SSM) * ssm_norm_g.astype(jnp.float32)
    return y.astype(z.dtype)


def peer_ffn(h, w_query, sub_keys, expert_down, expert_up):
    b, s, d = h.shape
    tokens = h.reshape(-1, PEER_TOKEN_BLOCK, d)
    keys32 = sub_keys.astype(jnp.float32)

    def block(hb):
        q = (hb @ w_query).astype(jnp.float32).reshape(PEER_TOKEN_BLOCK, PEER_HEADS, 2, PEER_HALF)
        scores = jnp.einsum("thjc,hjkc->thjk", q, keys32)
        s1, i1 = lax.top_k(scores[:, :, 0], PEER_TOPK)
        s2, i2 = lax.top_k(scores[:, :, 1], PEER_TOPK)
        cand_s = (s1[..., :, None] + s2[..., None, :]).reshape(PEER_TOKEN_BLOCK, PEER_HEADS, -1)
        cand_i = (i1[..., :, None] * PEER_KEYS + i2[..., None, :]).reshape(PEER_TOKEN_BLOCK, PEER_HEADS, -1)
        top_s, pos = lax.top_k(cand_s, PEER_TOPK)
        idx = jnp.take_along_axis(cand_i, pos, axis=-1)
        gates = jax.nn.softmax(top_s, axis=-1)
        u = expert_down[idx]
        v = expert_up[idx]
        act = jnp.einsum("td,thkd->thk", hb, u).astype(jnp.float32)
        w = (gates * jax.nn.gelu(act, approximate=False)).astype(hb.dtype)
        return jnp.einsum("thk,thkd->td", w, v)

    out = lax.map(block, tokens)
    return out.reshape(b, s, d)


def setup_inputs(seed: int = 0) -> dict:
    key = jax.random.key(seed)
    ks = jax.random.split(key, 24)
    f32 = jnp.float32
    nrm = lambda k, shape, scale: jax.random.normal(k, shape, f32) * scale
    dt = jnp.exp(jax.random.uniform(ks[9], (DEPTH, N_SSM_HEADS), f32,
                                    np.log(DT_MIN), np.log(DT_MAX)))
    dt2 = jnp.exp(jax.random.uniform(ks[10], (DEPTH, N_SSM_HEADS), f32,
                                     np.log(DT_MIN), np.log(DT_MAX)))
    return {
        "x": nrm(ks[0], (BATCH, SEQ, D_MODEL), 1.0),
        "c": nrm(ks[1], (BATCH, D_MODEL), 1.0),
        "w_ada": nrm(ks[2], (DEPTH, D_MODEL, 6 * D_MODEL), 0.5 * D_MODEL ** -0.5),
        "b_ada": nrm(ks[3], (DEPTH, 6 * D_MODEL), 0.02),
        "norm_mix_g": 1.0 + nrm(ks[4], (DEPTH, D_MODEL), 0.05),
        "w_in": nrm(ks[5], (DEPTH, D_MODEL, D_IN_PROJ), D_MODEL ** -0.5),
        "conv_w": nrm(ks[6], (DEPTH, CONV_WIDTH, D_CONV), CONV_WIDTH ** -0.5),
        "conv_b": nrm(ks[7], (DEPTH, D_CONV), 0.02),
        "a_log_fwd": jnp.log(jax.random.uniform(ks[8], (DEPTH, N_SSM_HEADS), f32, 1.0, 16.0)),
        "a_log_bwd": jnp.log(jax.random.uniform(ks[11], (DEPTH, N_SSM_HEADS), f32, 1.0, 16.0)),
        "dt_bias_fwd": dt + jnp.log(-jnp.expm1(-dt)),
        "dt_bias_bwd": dt2 + jnp.log(-jnp.expm1(-dt2)),
        "d_skip": 1.0 + nrm(ks[12], (DEPTH, N_SSM_HEADS), 0.1),
        "ssm_norm_g": 1.0 + nrm(ks[13], (DEPTH, D_SSM), 0.05),
        "w_out": nrm(ks[14], (DEPTH, D_MIX, D_MODEL), D_MIX ** -0.5),
        "norm_ffn_g": 1.0 + nrm(ks[15], (DEPTH, D_MODEL), 0.05),
        "w_query": nrm(ks[16], (DEPTH, D_MODEL, PEER_HEADS * PEER_QUERY_DIM), D_MODEL ** -0.5),
        "sub_keys": nrm(ks[17], (DEPTH, PEER_HEADS, 2, PEER_KEYS, PEER_HALF), PEER_HALF ** -0.5),
        "expert_down": nrm(ks[18], (DEPTH, PEER_EXPERTS, D_MODEL), D_MODEL ** -0.5),
        "expert_up": nrm(ks[19], (DEPTH, PEER_EXPERTS, D_MODEL), 1.0),
        "final_norm_g": 1.0 + nrm(ks[20], (D_MODEL,), 0.05),
    }


def reference(x, c, w_ada, b_ada, norm_mix_g, w_in, conv_w, conv_b, a_log_fwd, a_log_bwd,
              dt_bias_fwd, dt_bias_bwd, d_skip, ssm_norm_g, w_out, norm_ffn_g, w_query,
              sub_keys, expert_down, expert_up, final_norm_g):
    c_act = jax.nn.silu(c)
    for layer in range(DEPTH):
        mod = jnp.einsum("bd,de->be", c_act, w_ada[layer]) + b_ada[layer]
        shift_m, scale_m, gate_m, shift_f, scale_f, gate_f = jnp.split(mod, 6, axis=-1)

        h = modulate(rms_norm(x, norm_mix_g[layer]), shift_m, scale_m)
        proj = jnp.einsum("bsd,de->bse", h, w_in[layer])
        f_in = proj[..., :D_FNET]
        z = proj[..., D_FNET:D_FNET + D_SSM]
        xbc = proj[..., D_FNET + D_SSM:D_FNET + D_SSM + D_CONV]
        dt_raw = proj[..., D_FNET + D_SSM + D_CONV:]
        y_fnet = fnet_mix(f_in)
        y_ssm = bidirectional_ssd(z, xbc, dt_raw, conv_w[layer], conv_b[layer],
                                  a_log_fwd[layer], a_log_bwd[layer], dt_bias_fwd[layer],
                                  dt_bias_bwd[layer], d_skip[layer], ssm_norm_g[layer])
        mixed = jnp.concatenate([y_fnet, y_ssm], axis=-1)
        x = x + gate_m[:, None, :] * jnp.einsum("bse,ed->bsd", mixed, w_out[layer])

        h2 = modulate(rms_norm(x, norm_ffn_g[layer]), shift_f, scale_f)
        x = x + gate_f[:, None, :] * peer_ffn(h2, w_query[layer], sub_keys[layer],
                                              expert_down[layer], expert_up[layer])
    return rms_norm(x, final_norm_g)
```

```python
import numpy as np
import ml_dtypes
import concourse.bass as bass
import concourse.mybir as mybir
from concourse.bass_utils import run_bass_kernel_spmd
from contextlib import ExitStack

F32 = mybir.dt.float32
BF16 = mybir.dt.bfloat16
I32 = mybir.dt.int32
U32 = mybir.dt.uint32
ALU = mybir.AluOpType
AF = mybir.ActivationFunctionType
AX = mybir.AxisListType
EPS = 1e-6
NEG = -1.0e30


class Buf:
    __slots__ = ("name", "w", "r")

    def __init__(self, name=""):
        self.name = name
        self.w = None
        self.r = {}


class Sched:
    def __init__(self, nc, es, K=8):
        self.nc = nc
        self.eng = {"pe": nc.tensor, "act": nc.scalar, "dve": nc.vector, "pool": nc.gpsimd, "sp": nc.sync}
        self.csem = {e: es.enter_context(nc.semaphore("c_" + e)) for e in ["pe", "act", "dve", "pool"]}
        self.ccnt = {e: 0 for e in self.csem}
        self.K = K
        self.dsem = {q: [es.enter_context(nc.semaphore("d_%s%d" % (q, i))) for i in range(K)] for q in ["sp", "pool"]}
        self.dcnt = {q: 0 for q in self.dsem}
        self.seen = {f: {} for f in self.eng}

    def _sem(self, key):
        if key[0] == "x":
            return self.xsem[key[1]]
        return self.csem[key[1]] if key[0] == "c" else self.dsem[key[1]][key[2]]

    def _wait(self, F, tok, same_ok):
        if tok is None:
            return
        key, val = tok
        if key[0] == "c" and key[1] == F and (same_ok or F == "pe"):
            return
        if self.seen[F].get(key, 0) >= val:
            return
        self.eng[F].wait_ge(self._sem(key), val)
        self.seen[F][key] = val

    def _deps(self, F, r, w):
        for b in r:
            self._wait(F, b.w, False)
        for b in w:
            self._wait(F, b.w, True)
            for key, val in list(b.r.items()):
                self._wait(F, (key, val), True)

    def _mark(self, tok, r, w):
        for b in r:
            if b.r.get(tok[0], 0) < tok[1]:
                b.r[tok[0]] = tok[1]
        for b in w:
            b.w = tok
            b.r = {}

    def op(self, F, fns, r=(), w=()):
        if callable(fns):
            fns = [fns]
        self._deps(F, r, w)
        e = self.eng[F]
        ins = None
        for fn in fns:
            ins = fn(e)
        self.ccnt[F] += 1
        ins.then_inc(self.csem[F], 1)
        self._mark((("c", F), self.ccnt[F]), r, w)

    def dma(self, q, fn, r=(), w=()):
        i = self.dcnt[q]
        self.dcnt[q] += 1
        si = i % self.K
        val = 16 * (i // self.K + 1)
        key = ("d", q, si)
        if val > 16:
            self._wait(q, (key, val - 16), False)
        for b in r:
            self._wait(q, b.w, False)
        for b in w:
            self._wait(q, b.w, False)
            for k2, v2 in list(b.r.items()):
                self._wait(q, (k2, v2), False)
        ins = fn(self.eng[q])
        ins.then_inc(self.dsem[q][si], 16)
        self._mark((key, val), r, w)

    def all_tokens(self):
        toks = [(("c", e), n) for e, n in self.ccnt.items() if n > 0]
        for q, n in self.dcnt.items():
            for si in range(self.K):
                cnt = (n - si + self.K - 1) // self.K if n > si else 0
                if cnt > 0:
                    toks.append((("d", q, si), 16 * cnt))
        return toks

    def barrier(self, engines=("pe", "act", "dve", "pool", "sp")):
        toks = self.all_tokens()
        for F in engines:
            for tok in toks:
                if tok[0] == ("c", F):
                    continue
                self._wait(F, tok, False)


def build_nc(dbg=None):
    dbg = dbg or set()
    nc = bass.Bass("TRN2", target_bir_lowering=False)

    def din(name, shape, dt=F32):
        return nc.dram_tensor(name, shape, dt, kind="ExternalInput").ap()

    def dscr(name, shape, dt):
        return nc.dram_tensor(name, shape, dt, kind="Internal").ap()

    def dout(name, shape, dt=F32):
        return nc.dram_tensor(name, shape, dt, kind="ExternalOutput").ap()

    x = din("x", [4096, 1024])
    c_col = din("c_col", [128, 8])
    w_ada = din("w_ada", [1024, 6144])
    b_ada = din("b_ada", [1, 6144])
    gmix_col = din("gmix_col", [128, 8])
    gffn = din("gffn", [1, 1024])
    gfin = din("gfin", [1, 1024])
    gssm = din("gssm", [1, 1536])
    w_in = din("w_in", [1024, 4656])
    convw = din("convw", [128, 20, 5])
    convb = din("convb", [128, 20])
    alog = din("alog", [1, 48])
    dtb = din("dtb", [1, 48])
    dskip = din("dskip", [1, 24])
    w_out = din("w_out", [2048, 1024])
    w_q = din("w_q", [1024, 2048])
    keysT = din("keysT", [128, 16, 128])
    e_down = din("e_down", [16384, 1024])
    e_up = din("e_up", [16384, 1024])
    consts = din("consts", [128, 6, 128])
    csc = din("csc", [128, 256], BF16)
    dftc = din("dftc", [4096, 2048], BF16)
    dfts = din("dfts", [4096, 2048], BF16)
    iota16 = din("iota16", [128, 16])
    out = dout("out", [2048, 1024])

    projT_d = dscr("projT_d", [3072, 4096], BF16)
    z_d = dscr("z_d", [2048, 1536], BF16)
    mixedT_d = dscr("mixedT_d", [2048, 2048], BF16)
    mod_d = dscr("mod_d", [1, 6144], F32)
    edu_b = dscr("edu_b", [16384, 2048], BF16)
    wq_d = dscr("wq_d", [1024, 2048], BF16)
    wout_d = dscr("wout_d", [2048, 1024], BF16)
    B_wqd, B_woutd = Buf("wq_d"), Buf("wout_d")
    B_edu = Buf("edu_b")
    B_projT, B_z, B_mixed, B_mod = Buf("projT_d"), Buf("z_d"), Buf("mixedT_d"), Buf("mod_d")

    dbg_out = {}
    if "mod" in dbg:
        dbg_out["mod"] = dout("dbg_mod", [1, 6144])
    if "hT" in dbg:
        dbg_out["hT"] = dout("dbg_hT", [128, 8, 4096], BF16)
    if "proj" in dbg:
        dbg_out["proj"] = dout("dbg_proj", [3072, 4096], BF16)
        dbg_out["z"] = dout("dbg_z", [2048, 1536], BF16)
        dbg_out["dt"] = dout("dbg_dt", [128, 32, 48])
        dbg_out["dec"] = dout("dbg_dec", [128, 32, 144])
    if "mixed" in dbg:
        dbg_out["mixed"] = dout("dbg_mixed", [2048, 2048], BF16)
    if "post" in dbg:
        dbg_out["post"] = dout("dbg_post", [128, 3, 4096], BF16)

    with ExitStack() as es0:
        S = Sched(nc, es0)

        def sb(es, name, shape, dt):
            return es.enter_context(nc.sbuf_tensor(name, shape, dt))

        pb = [es0.enter_context(nc.psum_tensor("pb%d" % i, [128, 512], F32)) for i in range(8)]
        Bpb = [Buf("pb%d" % i) for i in range(8)]

        cst = sb(es0, "cst", [128, 6, 128], F32)
        B_cst = Buf("cst")
        identb = sb(es0, "identb", [128, 128], BF16)
        B_identb = Buf("identb")
        epst = sb(es0, "epst", [128, 1], F32)
        B_eps = Buf("eps")
        S.dma("sp", lambda e: e.dma_start(out=cst[:], in_=consts), w=[B_cst])
        S.op("dve", lambda e: e.tensor_copy(out=identb[:], in_=cst[:, 0, :]), r=[B_cst], w=[B_identb])
        S.op("dve", lambda e: e.memset(epst[:], EPS), w=[B_eps])
        ident_f = cst[:, 0, :]
        mGT, mLT, mLE, mGE, ones_f = cst[:, 1, :], cst[:, 2, :], cst[:, 3, :], cst[:, 4, :], cst[:, 5, :]

        def rms_rstd(ssap, Bss, n, rstd_ap, Brstd):
            S.op("act", lambda e: e.activation(out=rstd_ap, in_=ssap, func=AF.Sqrt, bias=epst[:, 0:1], scale=1.0 / n),
                 r=[Bss, B_eps], w=[Brstd])
            S.op("dve", lambda e: e.reciprocal(out=rstd_ap, in_=rstd_ap), r=[Brstd], w=[Brstd])

        es_h = ExitStack()
        B_hT = [Buf("hT%d" % i) for i in range(32)]
        g1col = sb(es_h, "g1col", [128, 8], F32)
        shcol = sb(es_h, "shcol", [128, 8], F32)
        B_g1, B_sh = Buf("g1col"), Buf("shcol")

        with ExitStack() as es:
            ccol = sb(es, "ccol", [128, 8], F32)
            cact = sb(es, "cact", [128, 8], F32)
            gmc = sb(es, "gmc", [128, 8], F32)
            modrow = sb(es, "modrow", [1, 6144], F32)
            brow = sb(es, "brow", [1, 6144], F32)
            wa = [sb(es, "wa%d" % i, [128, 8, 1024], F32) for i in range(2)]
            modcol = sb(es, "modcol", [128, 16], F32)
            B_ccol, B_cact, B_gmc, B_modrow, B_brow, B_modcol = (Buf() for _ in range(6))
            B_wa = [Buf(), Buf()]
            S.dma("sp", lambda e: e.dma_start(out=ccol[:], in_=c_col), w=[B_ccol])
            S.dma("sp", lambda e: e.dma_start(out=gmc[:], in_=gmix_col), w=[B_gmc])
            S.dma("sp", lambda e: e.dma_start(out=brow[:], in_=b_ada), w=[B_brow])
            S.op("act", lambda e: e.activation(out=cact[:], in_=ccol[:], func=AF.Silu), r=[B_ccol], w=[B_cact])
            wav = w_ada.rearrange("(k p) n -> p k n", p=128)
            for v in range(6):
                wb_, Bw_ = wa[v % 2], B_wa[v % 2]
                for hf in range(2):
                    S.dma("sp", lambda e: e.dma_start(out=wb_[:, :, hf * 512:(hf + 1) * 512],
                                                      in_=wav[:, :, v * 1024 + hf * 512: v * 1024 + (hf + 1) * 512]), w=[Bw_])
                for hf in range(2):
                    bank = (v * 2 + hf) % 4
                    off = v * 1024 + hf * 512
                    S.op("pe", [lambda e, k=k: e.matmul(pb[bank][0:1, :], lhsT=cact[:, k:k + 1],
                                                        rhs=wb_[:, k, hf * 512:(hf + 1) * 512], start=(k == 0), stop=(k == 7))
                                for k in range(8)], r=[B_cact, Bw_], w=[Bpb[bank]])
                    S.op("dve", lambda e: e.tensor_tensor(out=modrow[0:1, off:off + 512], in0=pb[bank][0:1, :],
                                                          in1=brow[0:1, off:off + 512], op=ALU.add),
                         r=[Bpb[bank], B_brow], w=[B_modrow])
            S.dma("sp", lambda e: e.dma_start(out=mod_d, in_=modrow[0:1, :]), r=[B_modrow], w=[B_mod])
            if "mod" in dbg:
                S.dma("sp", lambda e: e.dma_start(out=dbg_out["mod"], in_=modrow[0:1, :]), r=[B_modrow])
            S.op("pe", [lambda e, j=j: e.matmul(pb[4][:, j:j + 1], lhsT=modrow[0:1, j * 128:(j + 1) * 128],
                                                rhs=cst[0:1, 5, 0:1], start=True, stop=True) for j in range(16)],
                 r=[B_modrow, B_cst], w=[Bpb[4]])
            S.op("dve", lambda e: e.tensor_copy(out=modcol[:], in_=pb[4][:, 0:16]), r=[Bpb[4]], w=[B_modcol])
            S.op("dve", lambda e: e.tensor_copy(out=shcol[:], in_=modcol[:, 0:8]), r=[B_modcol], w=[B_sh])
            S.op("dve", lambda e: e.scalar_tensor_tensor(out=g1col[:], in0=modcol[:, 8:16], scalar=1.0, in1=gmc[:],
                                                         op0=ALU.add, op1=ALU.mult), r=[B_modcol, B_gmc], w=[B_g1])
            S.barrier()

        es_ssd = ExitStack()
        dt_all = sb(es_ssd, "dt_all", [128, 32, 48], F32)
        a_all = sb(es_ssd, "a_all", [128, 32, 48], F32)
        dec_all = sb(es_ssd, "dec_all", [128, 32, 144], F32)
        wd_all = sb(es_ssd, "wd_all", [128, 32, 48], F32)
        B_dt, B_a, B_dec, B_wd = Buf("dt"), Buf("a"), Buf("dec"), Buf("wd")
        es_hT = ExitStack()
        hT = sb(es_hT, "hT", [128, 8, 4096], BF16)
        with ExitStack() as es:
            xt = [sb(es, "xt%d" % i, [128, 1024], F32) for i in range(3)]
            B_xt = [Buf() for _ in range(3)]
            xn = [sb(es, "xn%d" % i, [128, 1024], BF16) for i in range(2)]
            B_xn = [Buf() for _ in range(2)]
            junk = sb(es, "junk1", [128, 1024], F32)
            B_junk = Buf()
            ssq = sb(es, "ssq", [128, 32], F32)
            rstd = sb(es, "rstd", [128, 32], F32)
            B_ssq = [Buf() for _ in range(32)]
            B_rstd = [Buf() for _ in range(32)]
            for i in range(32):
                t_, Bt_ = xt[i % 3], B_xt[i % 3]
                n_, Bn_ = xn[i % 2], B_xn[i % 2]
                S.dma("sp", lambda e: e.dma_start(out=t_[:], in_=x[i * 128:(i + 1) * 128, :]), w=[Bt_])
                S.op("act", lambda e: e.activation(out=junk[:], in_=t_[:], func=AF.Square, accum_out=ssq[:, i:i + 1]),
                     r=[Bt_], w=[B_junk, B_ssq[i]])
                rms_rstd(ssq[:, i:i + 1], B_ssq[i], 1024.0, rstd[:, i:i + 1], B_rstd[i])
                S.op("dve", lambda e: e.tensor_scalar(out=n_[:], in0=t_[:], scalar1=rstd[:, i:i + 1], scalar2=None,
                                                      op0=ALU.mult), r=[Bt_, B_rstd[i]], w=[Bn_])
                bank = i % 2
                pT = pb[bank][:].bitcast(BF16)
                S.op("pe", [lambda e, k=k: e.transpose(out=pT[:, k * 128:(k + 1) * 128], in_=n_[:, k * 128:(k + 1) * 128],
                                                       identity=identb[:]) for k in range(8)],
                     r=[Bn_, B_identb], w=[Bpb[bank]])
                for k in range(8):
                    if k % 2 == 0:
                        S.op("act", lambda e: e.activation(out=hT[:, k, i * 128:(i + 1) * 128], in_=pT[:, k * 128:(k + 1) * 128],
                                                           func=AF.Identity, bias=shcol[:, k:k + 1], scale=g1col[:, k:k + 1]),
                             r=[Bpb[bank], B_g1, B_sh], w=[B_hT[i]])
                    else:
                        S.op("dve", lambda e: e.tensor_scalar(out=hT[:, k, i * 128:(i + 1) * 128], in0=pT[:, k * 128:(k + 1) * 128],
                                                              scalar1=g1col[:, k:k + 1], scalar2=shcol[:, k:k + 1],
                                                              op0=ALU.mult, op1=ALU.add),
                             r=[Bpb[bank], B_g1, B_sh], w=[B_hT[i]])
            if "hT" in dbg:
                S.dma("sp", lambda e: e.dma_start(out=dbg_out["hT"], in_=hT[:]), r=B_hT)
            S.barrier()

        with ExitStack() as es:
            wt = [sb(es, "wt%d" % i, [128, 8, 128], BF16) for i in range(2)]
            B_wt = [Buf(), Buf()]
            stg = [sb(es, "stg%d" % i, [128, 4096], BF16) for i in range(2)]
            B_stg = [Buf(), Buf()]
            wz = sb(es, "wz", [128, 8, 1536], BF16)
            B_wz = Buf()
            zst = [sb(es, "zst%d" % i, [128, 1536], BF16) for i in range(2)]
            B_zst = [Buf(), Buf()]
            wdt = sb(es, "wdt", [128, 8, 48], BF16)
            B_wdt = Buf()
            dtb_b = sb(es, "dtb_b", [128, 48], F32)
            aneg_b = sb(es, "aneg_b", [128, 48], F32)
            B_dtb, B_aneg = Buf(), Buf()
            w_in_v = w_in.rearrange("(k p) n -> p k n", p=128)
            S.dma("pool", lambda e: e.dma_start(out=wdt[:], in_=w_in_v[:, :, 4608:4656]), w=[B_wdt])
            S.dma("sp", lambda e: e.dma_start(out=dtb_b[:], in_=dtb.partition_broadcast(128)), w=[B_dtb])
            S.dma("sp", lambda e: e.dma_start(out=aneg_b[:], in_=alog.partition_broadcast(128)), w=[B_aneg])
            S.op("act", lambda e: e.activation(out=aneg_b[:], in_=aneg_b[:], func=AF.Exp), r=[B_aneg], w=[B_aneg])
            S.op("dve", lambda e: e.tensor_scalar(out=aneg_b[:], in0=aneg_b[:], scalar1=-1.0, scalar2=None, op0=ALU.mult),
                 r=[B_aneg], w=[B_aneg])
            for i in range(32):
                bank = 4 + (i % 2)
                S.op("pe", [lambda e, k=k: e.matmul(pb[bank][:, 0:48], lhsT=hT[:, k, i * 128:(i + 1) * 128], rhs=wdt[:, k, :],
                                                    start=(k == 0), stop=(k == 7)) for k in range(8)],
                     r=[B_hT[i], B_wdt], w=[Bpb[bank]])
                S.op("dve", lambda e: e.tensor_tensor(out=dt_all[:, i, :], in0=pb[bank][:, 0:48], in1=dtb_b[:], op=ALU.add),
                     r=[Bpb[bank], B_dtb], w=[B_dt])
            S.op("act", lambda e: e.activation(out=dt_all[:], in_=dt_all[:], func=AF.Exp), r=[B_dt], w=[B_dt])
            S.op("act", lambda e: e.activation(out=dt_all[:], in_=dt_all[:], func=AF.Ln, bias=1.0, scale=1.0), r=[B_dt], w=[B_dt])
            S.op("dve", lambda e: e.tensor_tensor(out=a_all[:], in0=dt_all[:], in1=aneg_b[:].unsqueeze(1).broadcast_to([128, 32, 48]),
                                                  op=ALU.mult), r=[B_dt, B_aneg], w=[B_a])
            for i in range(32):
                bank = 4 + (i % 2)
                S.op("pe", [
                    lambda e: e.matmul(pb[bank][:, 0:24], lhsT=mLE, rhs=a_all[:, i, 0:24], start=True, stop=True),
                    lambda e: e.matmul(pb[bank][:, 24:48], lhsT=mGT, rhs=a_all[:, i, 0:24], start=True, stop=True),
                    lambda e: e.matmul(pb[bank][:, 48:72], lhsT=mLT, rhs=a_all[:, i, 24:48], start=True, stop=True),
                    lambda e: e.matmul(pb[bank][:, 72:96], lhsT=mGE, rhs=a_all[:, i, 24:48], start=True, stop=True),
                    lambda e: e.matmul(pb[bank][:, 96:144], lhsT=ones_f, rhs=a_all[:, i, 0:48], start=True, stop=True),
                ], r=[B_a, B_cst], w=[Bpb[bank]])
                S.op("act", lambda e: e.activation(out=dec_all[:, i, :], in_=pb[bank][:, 0:144], func=AF.Exp),
                     r=[Bpb[bank]], w=[B_dec])
            S.op("dve", lambda e: e.tensor_tensor(out=wd_all[:], in0=dt_all[:], in1=dec_all[:, :, 24:72], op=ALU.mult),
                 r=[B_dt, B_dec], w=[B_wd])
            if "proj" in dbg:
                S.dma("sp", lambda e: e.dma_start(out=dbg_out["dt"], in_=dt_all[:]), r=[B_dt])
                S.dma("sp", lambda e: e.dma_start(out=dbg_out["dec"], in_=dec_all[:]), r=[B_dec])
            ev = 0
            for j in range(24):
                col0 = j * 128 if j < 4 else 2048 + (j - 4) * 128
                w_, Bw_ = wt[j % 2], B_wt[j % 2]
                s_, Bs_ = stg[j % 2], B_stg[j % 2]
                S.dma("pool", lambda e: e.dma_start(out=w_[:], in_=w_in_v[:, :, col0:col0 + 128]), w=[Bw_])
                for tt in range(8):
                    bank = tt % 4
                    S.op("pe", [lambda e, k=k: e.matmul(pb[bank][:, :], lhsT=w_[:, k, :], rhs=hT[:, k, tt * 512:(tt + 1) * 512],
                                                        start=(k == 0), stop=(k == 7)) for k in range(8)],
                         r=[Bw_] + B_hT[tt * 4:(tt + 1) * 4], w=[Bpb[bank]])
                    if ev % 2 == 0:
                        S.op("act", lambda e: e.activation(out=s_[:, tt * 512:(tt + 1) * 512], in_=pb[bank][:, :], func=AF.Copy),
                             r=[Bpb[bank]], w=[Bs_])
                    else:
                        S.op("dve", lambda e: e.tensor_copy(out=s_[:, tt * 512:(tt + 1) * 512], in_=pb[bank][:, :]),
                             r=[Bpb[bank]], w=[Bs_])
                    ev += 1
                S.dma("sp", lambda e: e.dma_start(out=projT_d[j * 128:(j + 1) * 128, :], in_=s_[:]), r=[Bs_], w=[B_projT])
            S.dma("pool", lambda e: e.dma_start(out=wz[:], in_=w_in_v[:, :, 512:2048]), w=[B_wz])
            for i in range(16):
                z_, Bz_ = zst[i % 2], B_zst[i % 2]
                for n3 in range(3):
                    bank = (i * 3 + n3) % 4
                    S.op("pe", [lambda e, k=k: e.matmul(pb[bank][:, :], lhsT=hT[:, k, i * 128:(i + 1) * 128],
                                                        rhs=wz[:, k, n3 * 512:(n3 + 1) * 512], start=(k == 0), stop=(k == 7))
                                for k in range(8)], r=[B_hT[i], B_wz], w=[Bpb[bank]])
                    S.op("act", lambda e: e.activation(out=z_[:, n3 * 512:(n3 + 1) * 512], in_=pb[bank][:, :], func=AF.Silu),
                         r=[Bpb[bank]], w=[Bz_])
                S.dma("sp", lambda e: e.dma_start(out=z_d[i * 128:(i + 1) * 128, :], in_=z_[:]), r=[Bz_], w=[B_z])
            S.barrier()
            if "proj" in dbg:
                S.dma("sp", lambda e: e.dma_start(out=dbg_out["proj"], in_=projT_d), r=[B_projT])
                S.dma("sp", lambda e: e.dma_start(out=dbg_out["z"], in_=z_d), r=[B_z])
                S.barrier()
        es_hT.close()

        with ExitStack() as es:
            fT = [sb(es, "fT%d" % i, [128, 4096], BF16) for i in range(2)]
            B_fT = [Buf(), Buf()]
            Z = sb(es, "Z", [128, 32, 4, 256], BF16)
            B_Z = [Buf() for _ in range(4)]
            csct = sb(es, "csct", [128, 256], BF16)
            B_csc = Buf()
            dc = [sb(es, "dc%d" % i, [128, 16, 512], BF16) for i in range(2)]
            ds_ = [sb(es, "ds%d" % i, [128, 16, 512], BF16) for i in range(2)]
            B_dc = [Buf(), Buf()]
            B_ds = [Buf(), Buf()]
            ost = [sb(es, "ost%d" % i, [128, 512], BF16) for i in range(2)]
            B_ost = [Buf(), Buf()]
            S.dma("sp", lambda e: e.dma_start(out=csct[:], in_=csc), w=[B_csc])
            dftc_v = dftc.rearrange("(i p) k -> p i k", p=128)
            dfts_v = dfts.rearrange("(i p) k -> p i k", p=128)
            oc = 0
            for g in range(4):
                f_, Bf_ = fT[g % 2], B_fT[g % 2]
                S.dma("sp", lambda e: e.dma_start(out=f_[:], in_=projT_d[g * 128:(g + 1) * 128, :]), r=[B_projT], w=[Bf_])
                for i in range(32):
                    bank = i % 4
                    S.op("pe", lambda e: e.matmul(pb[bank][:, 0:256], lhsT=f_[:, i * 128:(i + 1) * 128], rhs=csct[:],
                                                  start=True, stop=True), r=[Bf_, B_csc], w=[Bpb[bank]])
                    if i % 2 == 0:
                        S.op("act", lambda e: e.activation(out=Z[:, i, g, :], in_=pb[bank][:, 0:256], func=AF.Copy),
                             r=[Bpb[bank]], w=[B_Z[g]])
                    else:
                        S.op("dve", lambda e: e.tensor_copy(out=Z[:, i, g, :], in_=pb[bank][:, 0:256]),
                             r=[Bpb[bank]], w=[B_Z[g]])
            for kt in range(4):
                for hf in range(2):
                    S.dma("sp", lambda e: e.dma_start(out=dc[hf][:], in_=dftc_v[:, hf * 16:(hf + 1) * 16, kt * 512:(kt + 1) * 512]),
                          w=[B_dc[hf]])
                    S.dma("sp", lambda e: e.dma_start(out=ds_[hf][:], in_=dfts_v[:, hf * 16:(hf + 1) * 16, kt * 512:(kt + 1) * 512]),
                          w=[B_ds[hf]])
                for hf in range(2):
                    for g in range(4):
                        bank = 4 + g
                        fns = []
                        for ii in range(16):
                            i = hf * 16 + ii
                            fns.append(lambda e, i=i, ii=ii: e.matmul(pb[bank][:, :], lhsT=Z[:, i, g, 0:128], rhs=dc[hf][:, ii, :],
                                                                      start=(i == 0), stop=False))
                            fns.append(lambda e, i=i, ii=ii: e.matmul(pb[bank][:, :], lhsT=Z[:, i, g, 128:256], rhs=ds_[hf][:, ii, :],
                                                                      start=False, stop=(i == 31)))
                        S.op("pe", fns, r=[B_Z[g], B_dc[hf], B_ds[hf]], w=[Bpb[bank]])
                for g in range(4):
                    bank = 4 + g
                    o_, Bo_ = ost[oc % 2], B_ost[oc % 2]
                    oc += 1
                    if g % 2 == 0:
                        S.op("act", lambda e: e.activation(out=o_[:], in_=pb[bank][:, :], func=AF.Copy), r=[Bpb[bank]], w=[Bo_])
                    else:
                        S.op("dve", lambda e: e.tensor_copy(out=o_[:], in_=pb[bank][:, :]), r=[Bpb[bank]], w=[Bo_])
                    row0 = g * 128
                    S.dma("sp", lambda e: e.dma_start(out=mixedT_d[row0:row0 + 128, kt * 512:(kt + 1) * 512], in_=o_[:]),
                          r=[Bo_], w=[B_mixed])
            S.barrier()

        convsem = es0.enter_context(nc.semaphore("convsem"))
        for k in range(16):
            nc.gpsimd.dma_start(out=edu_b[k * 1024:(k + 1) * 1024, 0:1024], in_=e_down[k * 1024:(k + 1) * 1024, :]).then_inc(convsem, 16)
            nc.gpsimd.dma_start(out=edu_b[k * 1024:(k + 1) * 1024, 1024:2048], in_=e_up[k * 1024:(k + 1) * 1024, :]).then_inc(convsem, 16)
        nc.gpsimd.dma_start(out=wq_d, in_=w_q).then_inc(convsem, 16)
        nc.gpsimd.dma_start(out=wout_d, in_=w_out).then_inc(convsem, 16)
        S.xsem = {"conv": convsem}
        B_edu.w = (("x", "conv"), 16 * 34)
        B_wqd.w = (("x", "conv"), 16 * 34)
        B_woutd.w = (("x", "conv"), 16 * 34)
        with ExitStack() as es:
            pre = [sb(es, "pre%d" % i, [128, 4100], BF16) for i in range(2)]
            B_pre = [Buf(), Buf()]
            dgc = [sb(es, "dgc%d" % i, [128, 5, 128], BF16) for i in range(2)]
            B_dgc = [Buf(), Buf()]
            cvb = [0]
            cw = sb(es, "cw", [128, 20, 5], F32)
            cb = sb(es, "cb", [128, 20], F32)
            B_cw = Buf()
            xTt = sb(es, "xTt", [128, 3, 4096], BF16)
            BTt = sb(es, "BTt", [128, 4096], BF16)
            CTt = sb(es, "CTt", [128, 2048], BF16)
            B_xT = [Buf() for _ in range(3)]
            B_BT, B_CT = Buf(), Buf()
            xtok = sb(es, "xtok", [128, 16, 384], BF16)
            Btok = sb(es, "Btok", [128, 16, 128], BF16)
            B_xtok = [Buf() for _ in range(16)]
            B_Btok = [Buf() for _ in range(16)]
            xtk = [sb(es, "xtk%d" % i, [128, 512], BF16) for i in range(2)]
            B_xtk = [Buf(), Buf()]
            Sb_all = sb(es, "Sb_all", [128, 16, 384], BF16)
            B_Sb = [Buf() for _ in range(16)]
            Sf32 = [sb(es, "Sf32_%d" % i, [128, 384], F32) for i in range(2)]
            B_S32 = [Buf(), Buf()]
            Sfb = sb(es, "Sfb", [128, 384], BF16)
            B_Sfb = Buf()
            xw = [sb(es, "xw%d" % i, [128, 384], BF16) for i in range(4)]
            B_xw = [Buf() for _ in range(4)]
            wsm = sb(es, "wsm", [128, 8, 6], F32)
            Rt = [sb(es, "Rt%d" % i, [128, 6, 128], F32) for i in range(2)]
            Et = [sb(es, "Et%d" % i, [128, 6, 128], F32) for i in range(2)]
            MT = [sb(es, "MT%d" % i, [128, 6, 128], BF16) for i in range(2)]
            B_R, B_E, B_MT = [Buf(), Buf()], [Buf(), Buf()], [Buf(), Buf()]
            CBm = [sb(es, "CBm%d" % i, [128, 128], F32) for i in range(2)]
            B_CBm = [Buf(), Buf()]
            xwp = [[sb(es, "xwp%d_%d" % (q, i), [128, 384], BF16) for i in range(3)] for q in range(2)]
            B_xwp = [[Buf() for _ in range(3)] for _ in range(2)]
            tCp = [sb(es, "tCp%d" % q, [128, 384], F32) for q in range(2)]
            B_tCp = [Buf(), Buf()]
            CBmp = [[sb(es, "CBmp%d_%d" % (q, i), [128, 128], F32) for i in range(2)] for q in range(2)]
            B_CBmp = [[Buf(), Buf()], [Buf(), Buf()]]
            MTp = [[sb(es, "MTp%d_%d" % (q, i), [128, 6, 128], BF16) for i in range(2)] for q in range(2)]
            B_MTp = [[Buf(), Buf()], [Buf(), Buf()]]
            tA = sb(es, "tA", [128, 384], F32)
            tB = sb(es, "tB", [128, 384], F32)
            tC = sb(es, "tC", [128, 384], F32)
            ynb = sb(es, "ynb", [128, 384], BF16)
            B_tA, B_tB, B_tC, B_ynb = Buf(), Buf(), Buf(), Buf()
            zt = [sb(es, "zt%d" % i, [128, 384], BF16) for i in range(2)]
            B_zt = [Buf(), Buf()]
            mst = sb(es, "mst", [128, 3, 2048], BF16)
            B_mst = Buf()
            gssm_b = sb(es, "gssm_b", [128, 1536], F32)
            dsk_b = sb(es, "dsk_b", [128, 24], F32)
            B_gssm, B_dsk = Buf(), Buf()
            ss4 = sb(es, "ss4", [128, 1], F32)
            rs4 = sb(es, "rs4", [128, 1], F32)
            B_ss4, B_rs4 = Buf(), Buf()
            junk4 = sb(es, "junk4", [128, 384], F32)
            B_junk4 = Buf()
            S.dma("sp", lambda e: e.dma_start(out=cw[:], in_=convw), w=[B_cw])
            S.dma("sp", lambda e: e.dma_start(out=cb[:], in_=convb), w=[B_cw])
            S.dma("sp", lambda e: e.dma_start(out=gssm_b[:], in_=gssm.partition_broadcast(128)), w=[B_gssm])
            S.dma("sp", lambda e: e.dma_start(out=dsk_b[:], in_=dskip.partition_broadcast(128)), w=[B_dsk])
            for p_ in pre:
                S.op("dve", lambda e: e.memset(p_[:, 0:2], 0.0), w=[B_pre[0], B_pre[1]])
                S.op("dve", lambda e: e.memset(p_[:, 4098:4100], 0.0), w=[B_pre[0], B_pre[1]])
            pc = 0
            for g in range(4):
                tiles = [(512 + g * 384 + i * 128, g * 3 + i, xTt[:, i, :], B_xT[i], 4096) for i in range(3)]
                tiles.append((512 + 1536 + g * 128, 12 + g, BTt[:, :], B_BT, 4096))
                tiles.append((512 + 2048 + g * 128, 16 + g, CTt[:, :], B_CT, 2048))
                for (row0, ci, dst, Bdst, ntok) in tiles:
                    p_, Bp_ = pre[pc % 2], B_pre[pc % 2]
                    dgc_, Bdgc_ = dgc[pc % 2], B_dgc[pc % 2]
                    pc += 1
                    S.dma("sp", lambda e: e.dma_start(out=p_[:, 2:4098], in_=projT_d[row0:row0 + 128, :]), r=[B_projT], w=[Bp_])
                    for k in range(5):
                        S.op("dve", lambda e: e.tensor_scalar(out=dgc_[:, k, :], in0=identb[:], scalar1=cw[:, ci, k:k + 1], scalar2=None, op0=ALU.mult),
                             r=[B_identb, B_cw], w=[Bdgc_])
                    for tb in range(ntok // 512):
                        bank = 2 + (cvb[0] % 4)
                        cvb[0] += 1
                        o0 = tb * 512
                        S.op("pe", [lambda e, k=k: e.matmul(pb[bank][:, :], lhsT=dgc_[:, k, :], rhs=p_[:, o0 + k:o0 + k + 512],
                                                            start=(k == 0), stop=(k == 4)) for k in range(5)],
                             r=[Bdgc_, Bp_], w=[Bpb[bank]])
                        S.op("act", lambda e: e.activation(out=dst[:, o0:o0 + 512], in_=pb[bank][:, :], func=AF.Silu, bias=cb[:, ci:ci + 1], scale=1.0),
                             r=[Bpb[bank], B_cw], w=[Bdst])
                if "post" in dbg and g == 0:
                    S.dma("sp", lambda e: e.dma_start(out=dbg_out["post"], in_=xTt[:]), r=B_xT)

                hs0 = g * 6

                def dests(c):
                    if c < 16:
                        return xtok[:, c, :], B_xtok[c], Btok[:, c, :], B_Btok[c]
                    k_ = xtk[c % 2]
                    return k_[:, 0:384], B_xtk[c % 2], k_[:, 384:512], B_xtk[c % 2]

                def stT(c):
                    tb_ = 0 if c % 2 == 0 else 3
                    pT = pb[tb_][:].bitcast(BF16)
                    fns = [lambda e, i=i: e.transpose(out=pT[:, i * 128:(i + 1) * 128], in_=xTt[:, i, c * 128:(c + 1) * 128],
                                                      identity=identb[:]) for i in range(3)]
                    fns.append(lambda e: e.transpose(out=pT[:, 384:512], in_=BTt[:, c * 128:(c + 1) * 128], identity=identb[:]))
                    S.op("pe", fns, r=B_xT + [B_BT, B_identb], w=[Bpb[tb_]])

                def stE(c):
                    tb_ = 0 if c % 2 == 0 else 3
                    pT = pb[tb_][:].bitcast(BF16)
                    xdst, Bx, bdst, Bb = dests(c)
                    S.op("act", lambda e: e.activation(out=xdst, in_=pT[:, 0:384], func=AF.Copy), r=[Bpb[tb_]], w=[Bx])
                    S.op("act", lambda e: e.activation(out=bdst, in_=pT[:, 384:512], func=AF.Copy), r=[Bpb[tb_]], w=[Bb])

                def weighted(dst, Bd, xsrc, Bx, wap, eng="pool"):
                    S.op(eng, lambda e: e.tensor_tensor(out=dst.rearrange("l (h p) -> l h p", h=6),
                                                          in0=xsrc.rearrange("l (h p) -> l h p", h=6),
                                                          in1=wap.unsqueeze(2).broadcast_to([128, 6, 64]), op=ALU.mult),
                         r=[Bx, B_dt, B_wd, B_dec, B_dsk], w=[Bd])

                def recur(Sold, Bold, Snew, Bnew, st_bank, etot_ap, first):
                    if first:
                        S.op("dve", lambda e: e.tensor_copy(out=Snew[:], in_=pb[st_bank][:, 0:384]), r=[Bpb[st_bank]], w=[Bnew])
                    else:
                        S.op("dve", lambda e: e.tensor_tensor(out=Snew[:].rearrange("n (h p) -> n h p", h=6),
                                                              in0=Sold[:].rearrange("n (h p) -> n h p", h=6),
                                                              in1=etot_ap.unsqueeze(2).broadcast_to([128, 6, 64]), op=ALU.mult),
                             r=[Bold, B_dec], w=[Bnew])
                        S.op("dve", lambda e: e.tensor_tensor(out=Snew[:], in0=Snew[:], in1=pb[st_bank][:, 0:384], op=ALU.add),
                             r=[Bnew, Bpb[st_bank]], w=[Bnew])

                curA = [0]

                def stW(c):
                    xd, Bx, bd, Bb = dests(c)
                    weighted(xw[c % 2][:], B_xw[c % 2], xd, Bx, wd_all[:, c, 24 + hs0:24 + hs0 + 6])

                def stM(c):
                    xd, Bx, bd, Bb = dests(c)
                    sbk = 1 + (c % 2)
                    S.op("pe", lambda e: e.matmul(pb[sbk][:, 0:384], lhsT=bd, rhs=xw[c % 2][:], start=True, stop=True),
                         r=[Bb, B_xw[c % 2]], w=[Bpb[sbk]])

                def stR(c):
                    sbk = 1 + (c % 2)
                    cur = curA[0]
                    nxt = 1 - cur
                    recur(Sf32[cur], B_S32[cur], Sf32[nxt], B_S32[nxt], sbk, dec_all[:, c, 96 + 24 + hs0:96 + 24 + hs0 + 6], first=(c == 31))
                    curA[0] = nxt
                    if c - 1 < 16:
                        S.op("act", lambda e: e.activation(out=Sb_all[:, c - 1, :], in_=Sf32[nxt][:], func=AF.Copy),
                             r=[B_S32[nxt]], w=[B_Sb[c - 1]])

                stages = [stT, stE, stW, stM, stR]
                for k in range(32 + 4):
                    for si in (4, 3, 2, 1, 0):
                        c = 31 - (k - si)
                        if c < 0 or c > 31:
                            continue
                        if si >= 2 and c == 0:
                            continue
                        stages[si](c)

                curB = [0]

                def front(c):
                    q = c % 2
                    z_, Bz_ = zt[q], B_zt[q]
                    S.dma("sp", lambda e: e.dma_start(out=z_[:], in_=z_d[c * 128:(c + 1) * 128, g * 384:(g + 1) * 384]), r=[B_z], w=[Bz_])
                    xs, Bxs = xtok[:, c, :], B_xtok[c]
                    S.op("pe", lambda e: e.matmul(pb[4][:, 0:128], lhsT=BTt[:, c * 128:(c + 1) * 128], rhs=CTt[:, c * 128:(c + 1) * 128],
                                                  start=True, stop=True), r=[B_BT, B_CT], w=[Bpb[4]])
                    yield
                    for d in range(2):
                        acol = d * 24 + hs0
                        U = mLE if d == 0 else mGE
                        S.op("pool", lambda e: e.tensor_tensor(out=Rt[d][:], in0=U.unsqueeze(1).broadcast_to([128, 6, 128]),
                                                               in1=a_all[:, c, acol:acol + 6].unsqueeze(2).broadcast_to([128, 6, 128]),
                                                               op=ALU.mult), r=[B_cst, B_a], w=[B_R[d]])
                        yield
                    for d in range(2):
                        acol = d * 24 + hs0
                        weighted(xwp[q][d][:], B_xwp[q][d], xs, Bxs, dt_all[:, c, acol:acol + 6])
                        yield
                    for d in range(2):
                        LT = mGT if d == 0 else mLT
                        Rf = Rt[d][:].rearrange("m h l -> m (h l)")
                        S.op("pe", [lambda e: e.matmul(pb[2][:, :], lhsT=LT, rhs=Rf[:, 0:512], start=True, stop=True),
                                    lambda e: e.matmul(pb[3][:, 0:256], lhsT=LT, rhs=Rf[:, 512:768], start=True, stop=True)],
                             r=[B_R[d], B_cst], w=[Bpb[2], Bpb[3]])
                        yield
                        if d == 0:
                            if c < 15:
                                weighted(xwp[q][2][:], B_xwp[q][2], xs, Bxs, wd_all[:, c, hs0:hs0 + 6])
                                yield
                            weighted(tCp[q][:], B_tCp[q], xs, Bxs, dsk_b[:, hs0:hs0 + 6])
                            yield
                        Ef = Et[d][:].rearrange("m h l -> m (h l)")
                        S.op("act", lambda e: e.activation(out=Ef[:, 0:512], in_=pb[2][:, :], func=AF.Exp), r=[Bpb[2]], w=[B_E[d]])
                        yield
                        S.op("act", lambda e: e.activation(out=Ef[:, 512:768], in_=pb[3][:, 0:256], func=AF.Exp), r=[Bpb[3]], w=[B_E[d]])
                        yield
                        if d == 0:
                            S.op("dve", lambda e: e.tensor_tensor(out=CBmp[q][0][:], in0=pb[4][:, 0:128], in1=mLE, op=ALU.mult),
                                 r=[Bpb[4], B_cst], w=[B_CBmp[q][0]])
                            yield
                            S.op("dve", lambda e: e.tensor_tensor(out=CBmp[q][1][:], in0=pb[4][:, 0:128], in1=mGE, op=ALU.mult),
                                 r=[Bpb[4], B_cst], w=[B_CBmp[q][1]])
                            yield
                    for d in range(2):
                        S.op("dve", lambda e: e.tensor_tensor(out=MTp[q][d][:], in0=Et[d][:],
                                                              in1=CBmp[q][d][:].unsqueeze(1).broadcast_to([128, 6, 128]), op=ALU.mult),
                             r=[B_E[d], B_CBmp[q][d]], w=[B_MTp[q][d]])
                        yield

                def back(c):
                    q = c % 2
                    z_, Bz_ = zt[q], B_zt[q]
                    MT_, B_MT_ = MTp[q], B_MTp[q]
                    xw_, B_xw_ = xwp[q], B_xwp[q]
                    fns = []
                    for h in range(6):
                        fns.append(lambda e, h=h: e.matmul(pb[6][:, h * 64:(h + 1) * 64], lhsT=MT_[0][:, h, :], rhs=xw_[0][:, h * 64:(h + 1) * 64],
                                                           start=True, stop=False))
                        fns.append(lambda e, h=h: e.matmul(pb[6][:, h * 64:(h + 1) * 64], lhsT=MT_[1][:, h, :], rhs=xw_[1][:, h * 64:(h + 1) * 64],
                                                           start=False, stop=True))
                    S.op("pe", fns, r=[B_MT_[0], B_MT_[1], B_xw_[0], B_xw_[1]], w=[Bpb[6]])
                    yield
                    S.op("pe", lambda e: e.matmul(pb[7][:, 0:384], lhsT=CTt[:, c * 128:(c + 1) * 128], rhs=Sb_all[:, c, :], start=True, stop=True),
                         r=[B_CT, B_Sb[c]], w=[Bpb[7]])
                    yield
                    if c < 15:
                        S.op("pe", lambda e: e.matmul(pb[1][:, 0:384], lhsT=Btok[:, c, :], rhs=xw_[2][:], start=True, stop=True),
                             r=[B_Btok[c], B_xw_[2]], w=[Bpb[1]])
                        yield
                    S.op("dve", lambda e: e.tensor_tensor(out=tA[:].rearrange("l (h p) -> l h p", h=6),
                                                          in0=pb[7][:, 0:384].rearrange("l (h p) -> l h p", h=6),
                                                          in1=dec_all[:, c, 72 + hs0:72 + hs0 + 6].unsqueeze(2).broadcast_to([128, 6, 64]),
                                                          op=ALU.mult), r=[Bpb[7], B_dec], w=[B_tA])
                    yield
                    if c > 0:
                        S.op("pe", lambda e: e.matmul(pb[7][:, 0:384], lhsT=CTt[:, c * 128:(c + 1) * 128], rhs=Sfb[:], start=True, stop=True),
                             r=[B_CT, B_Sfb], w=[Bpb[7]])
                        yield
                    if c < 15:
                        cur = curB[0]
                        nxt = 1 - cur
                        recur(Sf32[cur], B_S32[cur], Sf32[nxt], B_S32[nxt], 1, dec_all[:, c, 96 + hs0:96 + hs0 + 6], first=(c == 0))
                        curB[0] = nxt
                        yield
                    if c > 0:
                        S.op("dve", lambda e: e.tensor_tensor(out=tB[:].rearrange("l (h p) -> l h p", h=6),
                                                              in0=pb[7][:, 0:384].rearrange("l (h p) -> l h p", h=6),
                                                              in1=dec_all[:, c, hs0:hs0 + 6].unsqueeze(2).broadcast_to([128, 6, 64]),
                                                              op=ALU.mult), r=[Bpb[7], B_dec], w=[B_tB])
                        yield
                        S.op("dve", lambda e: e.tensor_tensor(out=tA[:], in0=tA[:], in1=tB[:], op=ALU.add), r=[B_tA, B_tB], w=[B_tA])
                        yield
                    if c < 15:
                        S.op("act", lambda e: e.activation(out=Sfb[:], in_=Sf32[curB[0]][:], func=AF.Copy), r=[B_S32[curB[0]]], w=[B_Sfb])
                        yield
                    S.op("dve", lambda e: e.tensor_tensor(out=tA[:], in0=tA[:], in1=pb[6][:, 0:384], op=ALU.add), r=[B_tA, Bpb[6]], w=[B_tA])
                    yield
                    S.op("dve", lambda e: e.tensor_tensor(out=tA[:], in0=tA[:], in1=tCp[q][:], op=ALU.add), r=[B_tA, B_tCp[q]], w=[B_tA])
                    yield
                    S.op("dve", lambda e: e.tensor_tensor(out=tA[:], in0=tA[:], in1=z_[:], op=ALU.mult), r=[B_tA, Bz_], w=[B_tA])
                    yield
                    S.op("act", lambda e: e.activation(out=junk4[:], in_=tA[:], func=AF.Square, accum_out=ss4[:, 0:1]),
                         r=[B_tA], w=[B_junk4, B_ss4])
                    yield
                    rms_rstd(ss4[:, 0:1], B_ss4, 384.0, rs4[:, 0:1], B_rs4)
                    yield
                    S.op("dve", lambda e: e.scalar_tensor_tensor(out=ynb[:], in0=tA[:], scalar=rs4[:, 0:1], in1=gssm_b[:, g * 384:(g + 1) * 384],
                                                                 op0=ALU.mult, op1=ALU.mult), r=[B_tA, B_rs4, B_gssm], w=[B_ynb])
                    yield
                    pT = pb[0][:].bitcast(BF16)
                    S.op("pe", [lambda e, i=i: e.transpose(out=pT[:, i * 128:(i + 1) * 128], in_=ynb[:, i * 128:(i + 1) * 128], identity=identb[:])
                                for i in range(3)], r=[B_ynb, B_identb], w=[Bpb[0]])
                    yield
                    S.op("act", lambda e: e.activation(out=mst[:, :, c * 128:(c + 1) * 128], in_=pT[:, 0:384].rearrange("p (i l) -> p i l", i=3),
                                                       func=AF.Copy), r=[Bpb[0]], w=[B_mst])
                    yield

                for _ in front(0):
                    pass
                for c in range(16):
                    gb = back(c)
                    gf = front(c + 1) if c + 1 < 16 else None
                    while gb is not None or gf is not None:
                        if gb is not None:
                            try:
                                next(gb)
                            except StopIteration:
                                gb = None
                        if gf is not None:
                            try:
                                next(gf)
                            except StopIteration:
                                gf = None
                for i in range(3):
                    row0 = 512 + g * 384 + i * 128
                    S.dma("sp", lambda e: e.dma_start(out=mixedT_d[row0:row0 + 128, :], in_=mst[:, i, :]), r=[B_mst], w=[B_mixed])
            S.barrier()
        es_ssd.close()
        if "mixed" in dbg:
            S.dma("sp", lambda e: e.dma_start(out=dbg_out["mixed"], in_=mixedT_d), r=[B_mixed])
            S.barrier()

        with ExitStack() as es:
            wout = sb(es, "wout", [128, 16, 1024], BF16)
            wq = sb(es, "wq", [128, 8, 2048], BF16)
            kT = sb(es, "kT", [128, 16, 128], BF16)
            B_wout, B_wq, B_kT = Buf(), Buf(), Buf()
            S.dma("sp", lambda e: e.dma_start(out=wout[:], in_=wout_d.rearrange("(k p) n -> p k n", p=128)), r=[B_woutd], w=[B_wout])
            S.dma("sp", lambda e: e.dma_start(out=wq[:], in_=wq_d.rearrange("(k p) n -> p k n", p=128)), r=[B_wqd], w=[B_wq])
            S.dma("pool", lambda e: e.dma_start(out=kT[:], in_=keysT), w=[B_kT])
            gm_b = sb(es, "gm_b", [128, 1024], F32)
            g2_b = sb(es, "g2_b", [128, 1024], F32)
            sf_b = sb(es, "sf_b", [128, 1024], F32)
            gf_b = sb(es, "gf_b", [128, 1024], F32)
            gfin_b = sb(es, "gfin_b", [128, 1024], F32)
            ot = sb(es, "ot", [128, 1024], F32)
            B_rows, B_ot = Buf(), Buf()
            io16 = sb(es, "io16", [128, 16], F32)
            S.dma("sp", lambda e: e.dma_start(out=io16[:], in_=iota16), w=[B_rows])
            S.dma("sp", lambda e: e.dma_start(out=gm_b[:], in_=mod_d[0:1, 2048:3072].partition_broadcast(128)), r=[B_mod], w=[B_rows])
            S.dma("sp", lambda e: e.dma_start(out=sf_b[:], in_=mod_d[0:1, 3072:4096].partition_broadcast(128)), r=[B_mod], w=[B_rows])
            S.dma("sp", lambda e: e.dma_start(out=g2_b[:], in_=mod_d[0:1, 4096:5120].partition_broadcast(128)), r=[B_mod], w=[B_rows])
            S.dma("sp", lambda e: e.dma_start(out=gf_b[:], in_=mod_d[0:1, 5120:6144].partition_broadcast(128)), r=[B_mod], w=[B_rows])
            S.dma("sp", lambda e: e.dma_start(out=gfin_b[:], in_=gfin.partition_broadcast(128)), w=[B_rows])
            S.dma("sp", lambda e: e.dma_start(out=ot[:], in_=gffn.partition_broadcast(128)), w=[B_ot])
            S.op("dve", lambda e: e.scalar_tensor_tensor(out=g2_b[:], in0=g2_b[:], scalar=1.0, in1=ot[:], op0=ALU.add, op1=ALU.mult),
                 r=[B_rows, B_ot], w=[B_rows])
            mT = sb(es, "mT", [128, 16, 128], BF16)
            xt5 = sb(es, "xt5", [128, 1024], F32)
            B_mT, B_xt5 = Buf(), Buf()
            h2f, B_h2f = xt5, B_xt5
            x1 = [sb(es, "x1_%d" % i, [128, 1024], F32) for i in range(2)]
            h2b = [sb(es, "h2b_%d" % i, [128, 1024], BF16) for i in range(2)]
            B_x1, B_h2b = [Buf(), Buf()], [Buf(), Buf()]
            h2T = sb(es, "h2T", [128, 8, 128], BF16)
            qT = sb(es, "qT", [128, 16, 128], BF16)
            sc = [sb(es, "sc%d" % i, [128, 4, 128], F32) for i in range(2)]
            sc2 = [sb(es, "sc2_%d" % i, [128, 256], F32) for i in range(2)]
            B_sc2 = [Buf() for _ in range(2)]
            B_h2T, B_qT = Buf(), Buf()
            B_tl = [Buf() for _ in range(16)]
            B_ohh = [Buf() for _ in range(4)]
            B_il = [Buf() for _ in range(16)]
            B_cvl = [Buf() for _ in range(8)]
            B_cpl = [Buf() for _ in range(8)]
            B_sc = [Buf(), Buf()]
            junkr = [sb(es, "junkr%d" % i, [128, 1024], BF16) for i in range(3)]
            B_junkr = [Buf() for _ in range(3)]
            junkb, B_junkb = junkr[0], B_junkr[0]
            ss5 = sb(es, "ss5", [128, 2], F32)
            rs5 = sb(es, "rs5", [128, 2], F32)
            B_ss5, B_rs5 = [Buf(), Buf()], [Buf(), Buf()]
            tv = sb(es, "tv", [128, 16, 16], F32)
            ti = sb(es, "ti", [128, 16, 16], U32)
            tif = sb(es, "tif", [128, 16, 16], F32)
            B_tv, B_ti, B_tif = Buf(), Buf(), Buf()
            cand = sb(es, "cand", [128, 8, 256], F32)
            B_cand = Buf()
            oh = cand[:].rearrange("p h (a b) -> p h a b", a=16)
            cv = sb(es, "cv", [128, 8, 16], F32)
            cpos = sb(es, "cpos", [128, 8, 16], U32)
            cpa = sb(es, "cpa", [128, 8, 16], U32)
            cpb_ = sb(es, "cpb", [128, 8, 16], U32)
            cpaf = sb(es, "cpaf", [128, 8, 16], F32)
            cpbf = sb(es, "cpbf", [128, 8, 16], F32)
            B_cv, B_cpos, B_cpa, B_cpb, B_cpaf, B_cpbf = (Buf() for _ in range(6))
            Iv = sb(es, "Iv", [128, 8, 16], F32)
            Jv = sb(es, "Jv", [128, 8, 16], F32)
            idxf = sb(es, "idxf", [128, 128], F32)
            idxi = [sb(es, "idxi%d" % i, [128, 128], I32) for i in range(2)]
            gate = [sb(es, "gate%d" % i, [128, 8, 16], F32) for i in range(2)]
            gsum = sb(es, "gsum", [128, 8], F32)
            B_Iv, B_Jv, B_idxf, B_gsum = (Buf() for _ in range(4))
            B_idxi, B_gate = [Buf(), Buf()], [Buf(), Buf()]
            actv = sb(es, "actv", [128, 128], F32)
            wv = sb(es, "wv", [128, 128], F32)
            GS = 4
            NGRP = 128 // GS
            B_actg = [Buf() for _ in range(NGRP)]
            B_wvg = [Buf() for _ in range(NGRP)]
            NG = 13
            Rg = sb(es, "Rg", [128, NG * 2048], BF16)
            B_gt = [Buf() for _ in range(NG)]
            NDG = 3
            dg = [sb(es, "dg%d" % i, [128, 128], BF16) for i in range(NDG)]
            B_dg = [Buf() for _ in range(NDG)]
            mixv = mixedT_d.rearrange("(k p) t -> p k t", p=128)
            rot = [0]

            def qbank():
                rot[0] += 1
                return 5 + rot[0] % 3

            def gen5a(i):
                p = i % 2
                S.dma("sp", lambda e: e.dma_start(out=mT[:], in_=mixv[:, :, i * 128:(i + 1) * 128]), r=[B_mixed], w=[B_mT])
                S.dma("sp", lambda e: e.dma_start(out=xt5[:], in_=x[i * 128:(i + 1) * 128, :]), w=[B_xt5])

                yield
                for kk in range(4):
                    fns = []
                    for hf in range(2):
                        for k4 in range(4):
                            k = kk * 4 + k4
                            fns.append(lambda e, k=k, hf=hf: e.matmul(pb[2 + hf][:, :], lhsT=mT[:, k, :], rhs=wout[:, k, hf * 512:(hf + 1) * 512],
                                                                      start=(k == 0), stop=(k == 15)))
                    S.op("pe", fns, r=[B_mT, B_wout], w=[Bpb[2], Bpb[3]])
                    yield
                yield
                for hf in range(2):
                    bk = 2 + hf
                    S.op("dve", lambda e: e.tensor_tensor(out=x1[p][:, hf * 512:(hf + 1) * 512], in0=pb[bk][:, :], in1=gm_b[:, hf * 512:(hf + 1) * 512],
                                                          op=ALU.mult), r=[Bpb[bk], B_rows], w=[B_x1[p]])
                yield
                S.op("dve", lambda e: e.tensor_tensor(out=x1[p][:], in0=x1[p][:], in1=xt5[:], op=ALU.add), r=[B_x1[p], B_xt5], w=[B_x1[p]])
                yield
                S.op("act", lambda e: e.activation(out=h2f[:], in_=x1[p][:], func=AF.Square, accum_out=ss5[:, 0:1]),
                     r=[B_x1[p]], w=[B_h2f, B_ss5[0]])
                yield
                S.op("act", lambda e: e.activation(out=rs5[:, 0:1], in_=ss5[:, 0:1], func=AF.Sqrt, bias=epst[:, 0:1], scale=1.0 / 1024.0),
                     r=[B_ss5[0], B_eps], w=[B_rs5[0]])
                yield
                S.op("dve", lambda e: e.reciprocal(out=rs5[:, 0:1], in_=rs5[:, 0:1]), r=[B_rs5[0]], w=[B_rs5[0]])
                yield
                S.op("dve", lambda e: e.scalar_tensor_tensor(out=h2f[:], in0=x1[p][:], scalar=rs5[:, 0:1], in1=g2_b[:], op0=ALU.mult, op1=ALU.mult),
                     r=[B_x1[p], B_rs5[0], B_rows], w=[B_h2f])
                yield
                S.op("dve", lambda e: e.tensor_tensor(out=h2b[p][:], in0=h2f[:], in1=sf_b[:], op=ALU.add), r=[B_h2f, B_rows], w=[B_h2b[p]])

                yield
                yield
                pT = pb[4][:].bitcast(BF16)
                S.op("pe", [lambda e, k=k: e.transpose(out=pT[:, k * 128:(k + 1) * 128], in_=h2b[p][:, k * 128:(k + 1) * 128], identity=identb[:])
                            for k in range(8)], r=[B_h2b[p], B_identb], w=[Bpb[4]])
                yield
                yield
                S.op("act", lambda e: e.activation(out=h2T[:].rearrange("p k t -> p (k t)"), in_=pT[:, :], func=AF.Copy), r=[Bpb[4]], w=[B_h2T])
                yield
                yield
                qb = [5, 6, 7, 5]
                for q4 in range(5):
                    if q4 < 4:
                        bk = qb[q4]
                        fns = []
                        for j in range(4):
                            hs = q4 * 4 + j
                            for k in range(8):
                                fns.append(lambda e, hs=hs, j=j, k=k: e.matmul(pb[bk][:, j * 128:(j + 1) * 128], lhsT=wq[:, k, hs * 128:(hs + 1) * 128],
                                                                               rhs=h2T[:, k, :], start=(k == 0), stop=(k == 7)))
                        S.op("pe", fns, r=[B_wq, B_h2T], w=[Bpb[bk]])
                        yield
                    if q4 > 0:
                        qq = q4 - 1
                        bk = qb[qq]
                        S.op("act", lambda e: e.activation(out=qT[:, qq * 4:(qq + 1) * 4, :].rearrange("p j t -> p (j t)"), in_=pb[bk][:, :], func=AF.Copy),
                             r=[Bpb[bk]], w=[B_qT])
                        yield
                yield
                sb_ = [6, 7, 5, 6]

                def sc_pe(q4):
                    bk = sb_[q4]
                    S.op("pe", [lambda e, j=j: e.matmul(pb[bk][:, j * 128:(j + 1) * 128], lhsT=qT[:, q4 * 4 + j, :], rhs=kT[:, q4 * 4 + j, :],
                                                        start=True, stop=True) for j in range(4)], r=[B_qT, B_kT], w=[Bpb[bk]])

                def sc_act(q4):
                    bk = sb_[q4]
                    s_, Bs_ = sc[q4 % 2], B_sc[q4 % 2]
                    S.op("act", lambda e: e.activation(out=s_[:].rearrange("p j t -> p (j t)"), in_=pb[bk][:, :], func=AF.Copy),
                         r=[Bpb[bk]], w=[Bs_])

                sc_pe(0)
                yield
                sc_pe(1)
                yield
                sc_act(0)
                yield
                for q4 in range(4):
                    s_, Bs_ = sc[q4 % 2], B_sc[q4 % 2]
                    if q4 == 0:
                        sc_act(1)
                    for pr in range(2):
                        js = [pr * 2, pr * 2 + 1]
                        for j in js:
                            hs = q4 * 4 + j
                            S.op("dve", lambda e: e.max(out=tv[:, hs, 0:8], in_=s_[:, j, :]), r=[Bs_], w=[B_tl[hs]])
                        yield
                        for j in js:
                            hs = q4 * 4 + j
                            S.op("dve", lambda e: e.max_index(out=ti[:, hs, 0:8], in_max=tv[:, hs, 0:8], in_values=s_[:, j, :]), r=[Bs_, B_tl[hs]], w=[B_il[hs]])
                        yield
                        for j in js:
                            hs = q4 * 4 + j
                            S.op("dve", lambda e: e.match_replace(out=sc2[j % 2][:, 0:128], in_to_replace=tv[:, hs, 0:8], in_values=s_[:, j, :], imm_value=NEG),
                                 r=[Bs_, B_tl[hs]], w=[B_sc2[j % 2]])
                        if pr == 0 and q4 + 2 < 4:
                            sc_pe(q4 + 2)
                        yield
                        for j in js:
                            hs = q4 * 4 + j
                            S.op("dve", lambda e: e.max(out=tv[:, hs, 8:16], in_=sc2[j % 2][:, 0:128]), r=[B_sc2[j % 2]], w=[B_tl[hs]])
                        yield
                        for j in js:
                            hs = q4 * 4 + j
                            S.op("dve", lambda e: e.max_index(out=ti[:, hs, 8:16], in_max=tv[:, hs, 8:16], in_values=sc2[j % 2][:, 0:128]), r=[B_sc2[j % 2], B_tl[hs]], w=[B_il[hs]])
                        yield
                    if 2 <= q4 + 1 < 4:
                        sc_act(q4 + 1)
                        yield
                S.op("dve", lambda e: e.tensor_copy(out=tif[:], in_=ti[:]), r=B_il, w=[B_tif])
                tv4 = tv[:].rearrange("p (h j) a -> p h j a", j=2)
                tif4 = tif[:].rearrange("p (h j) a -> p h j a", j=2)
                S.op("dve", lambda e: e.tensor_tensor(out=cand[:].rearrange("p h (a b) -> p h a b", a=16),
                                                      in0=tv4[:, :, 0, :].unsqueeze(3).broadcast_to([128, 8, 16, 16]),
                                                      in1=tv4[:, :, 1, :].unsqueeze(2).broadcast_to([128, 8, 16, 16]), op=ALU.add),
                     r=B_tl, w=[B_cand])
                yield
                for h2_ in range(4):
                    hl = [(h2_ * 2, 0), (h2_ * 2 + 1, 1)]
                    for h, j in hl:
                        S.op("dve", lambda e: e.max(out=cv[:, h, 0:8], in_=cand[:, h, :]), r=[B_cand], w=[B_cvl[h]])
                    yield
                    for h, j in hl:
                        S.op("dve", lambda e: e.max_index(out=cpos[:, h, 0:8], in_max=cv[:, h, 0:8], in_values=cand[:, h, :]), r=[B_cand, B_cvl[h]], w=[B_cpl[h]])
                    yield
                    for h, j in hl:
                        S.op("dve", lambda e: e.match_replace(out=sc2[j][:, :], in_to_replace=cv[:, h, 0:8], in_values=cand[:, h, :], imm_value=NEG),
                             r=[B_cand, B_cvl[h]], w=[B_sc2[j % 2]])
                    yield
                    for h, j in hl:
                        S.op("dve", lambda e: e.max(out=cv[:, h, 8:16], in_=sc2[j][:, :]), r=[B_sc2[j % 2]], w=[B_cvl[h]])
                    yield
                    for h, j in hl:
                        S.op("dve", lambda e: e.max_index(out=cpos[:, h, 8:16], in_max=cv[:, h, 8:16], in_values=sc2[j][:, :]), r=[B_sc2[j], B_cvl[h]], w=[B_cpl[h]])
                    yield
                S.op("dve", lambda e: e.tensor_single_scalar(out=cpa[:], in_=cpos[:], scalar=4, op=ALU.logical_shift_right), r=B_cpl, w=[B_cpa])
                S.op("dve", lambda e: e.tensor_single_scalar(out=cpb_[:], in_=cpos[:], scalar=15, op=ALU.bitwise_and), r=B_cpl, w=[B_cpb])
                S.op("dve", lambda e: e.tensor_copy(out=cpaf[:], in_=cpa[:]), r=[B_cpa], w=[B_cpaf])
                S.op("dve", lambda e: e.tensor_copy(out=cpbf[:], in_=cpb_[:]), r=[B_cpb], w=[B_cpbf])
                yield
                for (pf, Bpf, side, dstv, Bdst) in [(cpaf, B_cpaf, 0, Iv, B_Iv), (cpbf, B_cpbf, 1, Jv, B_Jv)]:
                    for hh in range(4):
                        hsl = slice(hh * 2, hh * 2 + 2)
                        S.op("dve", lambda e: e.tensor_tensor(out=oh[:, hsl], in0=pf[:, hsl, :].unsqueeze(3).broadcast_to([128, 2, 16, 16]),
                                                              in1=io16[:].unsqueeze(1).unsqueeze(1).broadcast_to([128, 2, 16, 16]), op=ALU.is_equal),
                             r=[Bpf, B_rows], w=[B_ohh[hh]])
                        yield
                        S.op("dve", lambda e: e.tensor_tensor(out=oh[:, hsl], in0=oh[:, hsl], in1=tif4[:, hsl, side, :].unsqueeze(2).broadcast_to([128, 2, 16, 16]),
                                                              op=ALU.mult), r=[B_ohh[hh], B_tif], w=[B_ohh[hh]])
                        yield
                        S.op("dve", lambda e: e.tensor_reduce(out=dstv[:, hsl, :], in_=oh[:, hsl], axis=AX.X, op=ALU.add), r=[B_ohh[hh]], w=[Bdst])
                        yield
                S.op("dve", lambda e: e.scalar_tensor_tensor(out=idxf[:], in0=Iv[:].rearrange("p h k -> p (h k)"), scalar=128.0,
                                                             in1=Jv[:].rearrange("p h k -> p (h k)"), op0=ALU.mult, op1=ALU.add),
                     r=[B_Iv, B_Jv], w=[B_idxf])
                S.op("dve", lambda e: e.tensor_copy(out=idxi[p][:], in_=idxf[:]), r=[B_idxf], w=[B_idxi[p]])
                yield
                S.op("dve", lambda e: e.tensor_tensor(out=gate[p][:], in0=cv[:], in1=cv[:, :, 0:1].broadcast_to([128, 8, 16]), op=ALU.subtract),
                     r=B_cvl, w=[B_gate[p]])
                S.op("act", lambda e: e.activation(out=gate[p][:], in_=gate[p][:], func=AF.Exp), r=[B_gate[p]], w=[B_gate[p]])
                S.op("dve", lambda e: e.tensor_reduce(out=gsum[:], in_=gate[p][:], axis=AX.X, op=ALU.add), r=[B_gate[p]], w=[B_gsum])
                S.op("dve", lambda e: e.reciprocal(out=gsum[:], in_=gsum[:]), r=[B_gsum], w=[B_gsum])
                S.op("dve", lambda e: e.tensor_tensor(out=gate[p][:], in0=gate[p][:], in1=gsum[:].unsqueeze(2).broadcast_to([128, 8, 16]), op=ALU.mult),
                     r=[B_gate[p], B_gsum], w=[B_gate[p]])
                yield

            gcn = [0]
            dcn = [0]
            edu3 = edu_b.rearrange("e (a d) -> e a d", a=2)

            def S1(i, grp):
                p = i % 2
                ul = []
                for s_ in range(grp * GS, (grp + 1) * GS):
                    n = gcn[0]
                    gcn[0] += 1
                    u_ = n % NG
                    od = u_ * 2048
                    S.dma("pool", lambda e: e.indirect_dma_start(out=Rg[:, od:od + 2048], out_offset=None, in_=edu_b,
                                                                 in_offset=bass.IndirectOffsetOnAxis(ap=idxi[p][:, s_:s_ + 1], axis=0)),
                          r=[B_idxi[p], B_edu], w=[B_gt[u_]])
                    jb_, Bjb_ = junkr[n % 3], B_junkr[n % 3]
                    S.op("dve", lambda e: e.tensor_tensor(out=jb_[:], in0=Rg[:, od:od + 1024], in1=h2b[p][:], op=ALU.mult),
                         r=[B_gt[u_], B_h2b[p]], w=[Bjb_])
                    S.op("act", lambda e: e.activation(out=jb_[:], in_=jb_[:], func=AF.Copy, accum_out=actv[:, s_:s_ + 1]),
                         r=[Bjb_], w=[Bjb_, B_actg[grp]])
                    ul.append(u_)
                    tick()
                return ul

            def S2(i, grp):
                p = i % 2
                s0 = grp * GS
                gflat = gate[p][:].rearrange("p h k -> p (h k)")
                S.op("act", lambda e: e.activation(out=wv[:, s0:s0 + GS], in_=actv[:, s0:s0 + GS], func=AF.Gelu), r=[B_actg[grp]], w=[B_wvg[grp]])

            def S3(i, grp, ul):
                p = i % 2
                gflat = gate[p][:].rearrange("p h k -> p (h k)")
                for j, s_ in enumerate(range(grp * GS, (grp + 1) * GS)):
                    u_ = ul[j]
                    ou = u_ * 2048 + 1024
                    d_ = dcn[0] % NDG
                    dcn[0] += 1
                    S.op("dve", lambda e: e.tensor_scalar(out=dg[d_][:], in0=identb[:], scalar1=wv[:, s_:s_ + 1], scalar2=gflat[:, s_:s_ + 1],
                                                          op0=ALU.mult, op1=ALU.mult),
                         r=[B_identb, B_wvg[grp], B_gate[p]], w=[B_dg[d_]])
                    S.op("pe", [lambda e, hf=hf: e.matmul(pb[hf][:, :], lhsT=dg[d_][:], rhs=Rg[:, ou + hf * 512:ou + (hf + 1) * 512],
                                                          start=(s_ == 0), stop=(s_ == 127)) for hf in range(2)],
                         r=[B_dg[d_], B_gt[u_]], w=[Bpb[0], Bpb[1]])

            def fin(i):
                p = i % 2
                for hf in range(2):
                    S.op("dve", lambda e: e.tensor_tensor(out=ot[:, hf * 512:(hf + 1) * 512], in0=pb[hf][:, :], in1=gf_b[:, hf * 512:(hf + 1) * 512], op=ALU.mult),
                         r=[Bpb[hf], B_rows], w=[B_ot])
                S.op("dve", lambda e: e.tensor_tensor(out=ot[:], in0=ot[:], in1=x1[p][:], op=ALU.add), r=[B_ot, B_x1[p]], w=[B_ot])
                S.op("act", lambda e: e.activation(out=junkb[:], in_=ot[:], func=AF.Square, accum_out=ss5[:, 1:2]),
                     r=[B_ot], w=[B_junkb, B_ss5[1]])
                rms_rstd(ss5[:, 1:2], B_ss5[1], 1024.0, rs5[:, 1:2], B_rs5[1])
                S.op("dve", lambda e: e.scalar_tensor_tensor(out=ot[:], in0=ot[:], scalar=rs5[:, 1:2], in1=gfin_b[:], op0=ALU.mult, op1=ALU.mult),
                     r=[B_ot, B_rs5[1], B_rows], w=[B_ot])
                S.dma("sp", lambda e: e.dma_start(out=out[i * 128:(i + 1) * 128, :], in_=ot[:]), r=[B_ot])

            for _ in gen5a(0):
                pass
            TOT = 16 * NGRP
            uls = {}
            nxt = None
            nxt_box = [None]

            def tick():
                if nxt_box[0] is not None:
                    try:
                        next(nxt_box[0])
                    except StopIteration:
                        nxt_box[0] = None
            for G in range(TOT + 1):
                if G < TOT:
                    i, grp = divmod(G, NGRP)
                    if grp == 0 and nxt_box[0] is not None:
                        for _ in nxt_box[0]:
                            pass
                        nxt_box[0] = None
                    uls[G] = S1(i, grp)
                    S2(i, grp)
                if 0 <= G - 1 < TOT:
                    i2, g2 = divmod(G - 1, NGRP)
                    S3(i2, g2, uls.pop(G - 1))
                    if g2 == NGRP - 1:
                        fin(i2)
                if G < TOT and grp == 1 and i + 1 < 16:
                    nxt_box[0] = gen5a(i + 1)
            S.barrier()
        S.barrier(engines=("sp",))
        es_h.close()
    return nc


def _dft_tables():
    n = np.arange(4096, dtype=np.int64)
    prod = (n[:, None] * n[None, :]) % 4096
    ang = 2.0 * np.pi * prod.astype(np.float64) / 4096.0
    Cs = np.cos(ang) / 64.0
    Ss = np.sin(ang) / 64.0
    c = np.arange(128, dtype=np.int64)
    angc = 2.0 * np.pi * ((c[:, None] * c[None, :]) % 128).astype(np.float64) / 128.0
    csc = np.concatenate([np.cos(angc), -np.sin(angc)], axis=1) / np.sqrt(128.0)
    return Cs, Ss, csc


def _consts():
    m = np.arange(128)
    ident = np.eye(128)
    GT = (m[:, None] > m[None, :]).astype(np.float64)
    LT = (m[:, None] < m[None, :]).astype(np.float64)
    LE = (m[:, None] <= m[None, :]).astype(np.float64)
    GE = (m[:, None] >= m[None, :]).astype(np.float64)
    ones = np.ones((128, 128))
    return np.stack([ident, GT, LT, LE, GE, ones], axis=1).astype(np.float32)


def make_in_maps(inputs, cores=range(8)):
    f = lambda a: np.ascontiguousarray(np.asarray(a, dtype=np.float32))
    bf = lambda a: np.ascontiguousarray(np.asarray(a).astype(ml_dtypes.bfloat16))
    Cs, Ss, csc = _dft_tables()
    consts = _consts()
    iota16 = np.tile(np.arange(16, dtype=np.float32)[None, :], (128, 1))
    w_in = f(inputs["w_in"][0])
    w_in_sw = w_in.copy()
    w_in_sw[:, 4608:4632] = w_in[:, 4632:4656]
    w_in_sw[:, 4632:4656] = w_in[:, 4608:4632]
    conv_w = f(inputs["conv_w"][0])
    keysT = f(np.transpose(np.asarray(inputs["sub_keys"][0]).reshape(16, 128, 128), (2, 0, 1)))
    shared = {
        "w_ada": f(inputs["w_ada"][0]), "b_ada": f(inputs["b_ada"]), "gffn": f(inputs["norm_ffn_g"]),
        "gfin": f(np.asarray(inputs["final_norm_g"]).reshape(1, 1024)), "gssm": f(inputs["ssm_norm_g"]),
        "gmix_col": f(np.asarray(inputs["norm_mix_g"][0]).reshape(8, 128).T),
        "convb": f(np.asarray(inputs["conv_b"][0]).reshape(20, 128).T), "dskip": f(inputs["d_skip"]),
        "w_out": f(inputs["w_out"][0]), "w_q": f(inputs["w_query"][0]), "keysT": keysT,
        "e_down": f(inputs["expert_down"][0]), "e_up": f(inputs["expert_up"][0]),
        "consts": consts, "csc": bf(csc), "iota16": iota16,
    }
    dft = {}
    for half in (0, 1):
        if half == 0:
            dft[half] = (bf(Cs[:, :2048]), bf(Ss[:, :2048]))
        else:
            dft[half] = (bf(Cs[::-1, ::-1][:, :2048]), bf(Ss[::-1, ::-1][:, :2048]))
    maps = []
    for core in cores:
        b, half = core // 2, core % 2
        xb = np.asarray(inputs["x"][b], dtype=np.float32)
        m = dict(shared)
        if half == 0:
            m["x"] = f(xb)
            m["w_in"] = w_in
            cwT = conv_w.T
            m["alog"] = f(np.concatenate([inputs["a_log_fwd"][0], inputs["a_log_bwd"][0]]).reshape(1, 48))
            m["dtb"] = f(np.concatenate([inputs["dt_bias_fwd"][0], inputs["dt_bias_bwd"][0]]).reshape(1, 48))
        else:
            m["x"] = f(xb[::-1])
            m["w_in"] = w_in_sw
            cwT = conv_w[::-1].T
            m["alog"] = f(np.concatenate([inputs["a_log_bwd"][0], inputs["a_log_fwd"][0]]).reshape(1, 48))
            m["dtb"] = f(np.concatenate([inputs["dt_bias_bwd"][0], inputs["dt_bias_fwd"][0]]).reshape(1, 48))
        m["convw"] = f(cwT.reshape(20, 128, 5).transpose(1, 0, 2))
        m["c_col"] = f(np.asarray(inputs["c"][b]).reshape(8, 128).T)
        m["dftc"], m["dfts"] = dft[half]
        maps.append(m)
    return maps


def kernel(**inputs):
    nc = build_nc()
    in_maps = make_in_maps(inputs)
    res = run_bass_kernel_spmd(nc, in_maps, core_ids=list(range(8)))
    outp = np.zeros((4, 4096, 1024), dtype=np.float32)
    for core in range(8):
        b, half = core // 2, core % 2
        o = np.asarray(res.results[core]["out"], dtype=np.float32)
        if half == 0:
            outp[b, :2048] = o
        else:
            outp[b, 2048:] = o[::-1]
    return outp
```

```python
import numpy as np
import ml_dtypes
import concourse.bass as bass
import concourse.mybir as mybir
from concourse.bass_utils import run_bass_kernel_spmd
from contextlib import ExitStack

F32 = mybir.dt.float32
BF16 = mybir.dt.bfloat16
I32 = mybir.dt.int32
U32 = mybir.dt.uint32
ALU = mybir.AluOpType
AF = mybir.ActivationFunctionType
AX = mybir.AxisListType
EPS = 1e-6
NEG = -1.0e30


class Buf:
    __slots__ = ("name", "w", "r")

    def __init__(self, name=""):
        self.name = name
        self.w = None
        self.r = {}


class Sched:
    def __init__(self, nc, es, K=8):
        self.nc = nc
        self.eng = {"pe": nc.tensor, "act": nc.scalar, "dve": nc.vector, "pool": nc.gpsimd, "sp": nc.sync}
        self.csem = {e: es.enter_context(nc.semaphore("c_" + e)) for e in ["pe", "act", "dve", "pool"]}
        self.ccnt = {e: 0 for e in self.csem}
        self.K = K
        self.dsem = {q: [es.enter_context(nc.semaphore("d_%s%d" % (q, i))) for i in range(K)] for q in ["sp", "pool"]}
        self.dcnt = {q: 0 for q in self.dsem}
        self.seen = {f: {} for f in self.eng}

    def _sem(self, key):
        if key[0] == "x":
            return self.xsem[key[1]]
        return self.csem[key[1]] if key[0] == "c" else self.dsem[key[1]][key[2]]

    def _wait(self, F, tok, same_ok):
        if tok is None:
            return
        key, val = tok
        if key[0] == "c" and key[1] == F and (same_ok or F == "pe"):
            return
        if self.seen[F].get(key, 0) >= val:
            return
        self.eng[F].wait_ge(self._sem(key), val)
        self.seen[F][key] = val

    def _deps(self, F, r, w):
        for b in r:
            self._wait(F, b.w, False)
        for b in w:
            self._wait(F, b.w, True)
            for key, val in list(b.r.items()):
                self._wait(F, (key, val), True)

    def _mark(self, tok, r, w):
        for b in r:
            if b.r.get(tok[0], 0) < tok[1]:
                b.r[tok[0]] = tok[1]
        for b in w:
            b.w = tok
            b.r = {}

    def op(self, F, fns, r=(), w=()):
        if callable(fns):
            fns = [fns]
        self._deps(F, r, w)
        e = self.eng[F]
        ins = None
        for fn in fns:
            ins = fn(e)
        self.ccnt[F] += 1
        ins.then_inc(self.csem[F], 1)
        self._mark((("c", F), self.ccnt[F]), r, w)

    def dma(self, q, fn, r=(), w=()):
        i = self.dcnt[q]
        self.dcnt[q] += 1
        si = i % self.K
        val = 16 * (i // self.K + 1)
        key = ("d", q, si)
        if val > 16:
            self._wait(q, (key, val - 16), False)
        for b in r:
            self._wait(q, b.w, False)
        for b in w:
            self._wait(q, b.w, False)
            for k2, v2 in list(b.r.items()):
                self._wait(q, (k2, v2), False)
        ins = fn(self.eng[q])
        ins.then_inc(self.dsem[q][si], 16)
        self._mark((key, val), r, w)

    def all_tokens(self):
        toks = [(("c", e), n) for e, n in self.ccnt.items() if n > 0]
        for q, n in self.dcnt.items():
            for si in range(self.K):
                cnt = (n - si + self.K - 1) // self.K if n > si else 0
                if cnt > 0:
                    toks.append((("d", q, si), 16 * cnt))
        return toks

    def barrier(self, engines=("pe", "act", "dve", "pool", "sp")):
        toks = self.all_tokens()
        for F in engines:
            for tok in toks:
                if tok[0] == ("c", F):
                    continue
                self._wait(F, tok, False)


def build_nc(dbg=None):
    dbg = dbg or set()
    nc = bass.Bass("TRN2", target_bir_lowering=False)

    def din(name, shape, dt=F32):
        return nc.dram_tensor(name, shape, dt, kind="ExternalInput").ap()

    def dscr(name, shape, dt):
        return nc.dram_tensor(name, shape, dt, kind="Internal").ap()

    def dout(name, shape, dt=F32):
        return nc.dram_tensor(name, shape, dt, kind="ExternalOutput").ap()

    x = din("x", [4096, 1024])
    c_col = din("c_col", [128, 8])
    w_ada = din("w_ada", [1024, 6144])
    b_ada = din("b_ada", [1, 6144])
    gmix_col = din("gmix_col", [128, 8])
    gffn = din("gffn", [1, 1024])
    gfin = din("gfin", [1, 1024])
    gssm = din("gssm", [1, 1536])
    w_in = din("w_in", [1024, 4656])
    convw = din("convw", [128, 20, 5])
    convb = din("convb", [128, 20])
    alog = din("alog", [1, 48])
    dtb = din("dtb", [1, 48])
    dskip = din("dskip", [1, 24])
    w_out = din("w_out", [2048, 1024])
    w_q = din("w_q", [1024, 2048])
    keysT = din("keysT", [128, 16, 128])
    e_down = din("e_down", [16384, 1024])
    e_up = din("e_up", [16384, 1024])
    consts = din("consts", [128, 6, 128])
    csc = din("csc", [128, 256], BF16)
    dftc = din("dftc", [4096, 2048], BF16)
    dfts = din("dfts", [4096, 2048], BF16)
    iota16 = din("iota16", [128, 16])
    out = dout("out", [2048, 1024])

    projT_d = dscr("projT_d", [3072, 4096], BF16)
    z_d = dscr("z_d", [2048, 1536], BF16)
    mixedT_d = dscr("mixedT_d", [2048, 2048], BF16)
    mod_d = dscr("mod_d", [1, 6144], F32)
    edu_b = dscr("edu_b", [16384, 2048], BF16)
    wq_d = dscr("wq_d", [1024, 2048], BF16)
    wout_d = dscr("wout_d", [2048, 1024], BF16)
    B_wqd, B_woutd = Buf("wq_d"), Buf("wout_d")
    B_edu = Buf("edu_b")
    B_projT, B_z, B_mixed, B_mod = Buf("projT_d"), Buf("z_d"), Buf("mixedT_d"), Buf("mod_d")

    dbg_out = {}
    if "mod" in dbg:
        dbg_out["mod"] = dout("dbg_mod", [1, 6144])
    if "hT" in dbg:
        dbg_out["hT"] = dout("dbg_hT", [128, 8, 4096], BF16)
    if "proj" in dbg:
        dbg_out["proj"] = dout("dbg_proj", [3072, 4096], BF16)
        dbg_out["z"] = dout("dbg_z", [2048, 1536], BF16)
        dbg_out["dt"] = dout("dbg_dt", [128, 32, 48])
        dbg_out["dec"] = dout("dbg_dec", [128, 32, 144])
    if "mixed" in dbg:
        dbg_out["mixed"] = dout("dbg_mixed", [2048, 2048], BF16)
    if "post" in dbg:
        dbg_out["post"] = dout("dbg_post", [128, 3, 4096], BF16)

    with ExitStack() as es0:
        S = Sched(nc, es0)

        def sb(es, name, shape, dt):
            return es.enter_context(nc.sbuf_tensor(name, shape, dt))

        pb = [es0.enter_context(nc.psum_tensor("pb%d" % i, [128, 512], F32)) for i in range(8)]
        Bpb = [Buf("pb%d" % i) for i in range(8)]

        cst = sb(es0, "cst", [128, 6, 128], F32)
        B_cst = Buf("cst")
        identb = sb(es0, "identb", [128, 128], BF16)
        B_identb = Buf("identb")
        epst = sb(es0, "epst", [128, 1], F32)
        B_eps = Buf("eps")
        S.dma("sp", lambda e: e.dma_start(out=cst[:], in_=consts), w=[B_cst])
        S.op("dve", lambda e: e.tensor_copy(out=identb[:], in_=cst[:, 0, :]), r=[B_cst], w=[B_identb])
        S.op("dve", lambda e: e.memset(epst[:], EPS), w=[B_eps])
        ident_f = cst[:, 0, :]
        mGT, mLT, mLE, mGE, ones_f = cst[:, 1, :], cst[:, 2, :], cst[:, 3, :], cst[:, 4, :], cst[:, 5, :]

        def rms_rstd(ssap, Bss, n, rstd_ap, Brstd):
            S.op("act", lambda e: e.activation(out=rstd_ap, in_=ssap, func=AF.Ln, bias=epst[:, 0:1], scale=1.0 / n),
                 r=[Bss, B_eps], w=[Brstd])
            S.op("act", lambda e: e.activation(out=rstd_ap, in_=rstd_ap, func=AF.Exp, scale=-0.5), r=[Brstd], w=[Brstd])

        es_h = ExitStack()
        B_hT = [Buf("hT%d" % i) for i in range(32)]
        g1col = sb(es_h, "g1col", [128, 8], F32)
        shcol = sb(es_h, "shcol", [128, 8], F32)
        B_g1, B_sh = Buf("g1col"), Buf("shcol")

        with ExitStack() as es:
            ccol = sb(es, "ccol", [128, 8], F32)
            cact = sb(es, "cact", [128, 8], F32)
            gmc = sb(es, "gmc", [128, 8], F32)
            modrow = sb(es, "modrow", [1, 6144], F32)
            brow = sb(es, "brow", [1, 6144], F32)
            wa = [sb(es, "wa%d" % i, [128, 8, 1024], F32) for i in range(2)]
            modcol = sb(es, "modcol", [128, 16], F32)
            B_ccol, B_cact, B_gmc, B_modrow, B_brow, B_modcol = (Buf() for _ in range(6))
            B_wa = [Buf(), Buf()]
            S.dma("sp", lambda e: e.dma_start(out=ccol[:], in_=c_col), w=[B_ccol])
            S.dma("sp", lambda e: e.dma_start(out=gmc[:], in_=gmix_col), w=[B_gmc])
            S.dma("sp", lambda e: e.dma_start(out=brow[:], in_=b_ada), w=[B_brow])
            S.op("act", lambda e: e.activation(out=cact[:], in_=ccol[:], func=AF.Silu), r=[B_ccol], w=[B_cact])
            wav = w_ada.rearrange("(k p) n -> p k n", p=128)
            for v in range(6):
                wb_, Bw_ = wa[v % 2], B_wa[v % 2]
                for hf in range(2):
                    S.dma("sp", lambda e: e.dma_start(out=wb_[:, :, hf * 512:(hf + 1) * 512],
                                                      in_=wav[:, :, v * 1024 + hf * 512: v * 1024 + (hf + 1) * 512]), w=[Bw_])
                for hf in range(2):
                    bank = (v * 2 + hf) % 4
                    off = v * 1024 + hf * 512
                    S.op("pe", [lambda e, k=k: e.matmul(pb[bank][0:1, :], lhsT=cact[:, k:k + 1],
                                                        rhs=wb_[:, k, hf * 512:(hf + 1) * 512], start=(k == 0), stop=(k == 7))
                                for k in range(8)], r=[B_cact, Bw_], w=[Bpb[bank]])
                    S.op("dve", lambda e: e.tensor_tensor(out=modrow[0:1, off:off + 512], in0=pb[bank][0:1, :],
                                                          in1=brow[0:1, off:off + 512], op=ALU.add),
                         r=[Bpb[bank], B_brow], w=[B_modrow])
            S.dma("sp", lambda e: e.dma_start(out=mod_d, in_=modrow[0:1, :]), r=[B_modrow], w=[B_mod])
            if "mod" in dbg:
                S.dma("sp", lambda e: e.dma_start(out=dbg_out["mod"], in_=modrow[0:1, :]), r=[B_modrow])
            S.op("pe", [lambda e, j=j: e.matmul(pb[4][:, j:j + 1], lhsT=modrow[0:1, j * 128:(j + 1) * 128],
                                                rhs=cst[0:1, 5, 0:1], start=True, stop=True) for j in range(16)],
                 r=[B_modrow, B_cst], w=[Bpb[4]])
            S.op("dve", lambda e: e.tensor_copy(out=modcol[:], in_=pb[4][:, 0:16]), r=[Bpb[4]], w=[B_modcol])
            S.op("dve", lambda e: e.tensor_copy(out=shcol[:], in_=modcol[:, 0:8]), r=[B_modcol], w=[B_sh])
            S.op("dve", lambda e: e.scalar_tensor_tensor(out=g1col[:], in0=modcol[:, 8:16], scalar=1.0, in1=gmc[:],
                                                         op0=ALU.add, op1=ALU.mult), r=[B_modcol, B_gmc], w=[B_g1])
            S.barrier()

        es_ssd = ExitStack()
        dt_all = sb(es_ssd, "dt_all", [128, 32, 48], F32)
        a_all = sb(es_ssd, "a_all", [128, 32, 48], F32)
        dec_all = sb(es_ssd, "dec_all", [128, 32, 144], F32)
        wd_all = sb(es_ssd, "wd_all", [128, 32, 48], F32)
        B_dt, B_a, B_dec, B_wd = Buf("dt"), Buf("a"), Buf("dec"), Buf("wd")
        es_hT = ExitStack()
        hT = sb(es_hT, "hT", [128, 8, 4096], BF16)
        with ExitStack() as es:
            xt = [sb(es, "xt%d" % i, [128, 1024], F32) for i in range(3)]
            B_xt = [Buf() for _ in range(3)]
            xn = [sb(es, "xn%d" % i, [128, 1024], BF16) for i in range(2)]
            B_xn = [Buf() for _ in range(2)]
            junk = sb(es, "junk1", [128, 1024], F32)
            B_junk = Buf()
            ssq = sb(es, "ssq", [128, 32], F32)
            rstd = sb(es, "rstd", [128, 32], F32)
            B_ssq = [Buf() for _ in range(32)]
            B_rstd = [Buf() for _ in range(32)]
            for i in range(32):
                t_, Bt_ = xt[i % 3], B_xt[i % 3]
                n_, Bn_ = xn[i % 2], B_xn[i % 2]
                S.dma("sp", lambda e: e.dma_start(out=t_[:], in_=x[i * 128:(i + 1) * 128, :]), w=[Bt_])
                S.op("act", lambda e: e.activation(out=junk[:], in_=t_[:], func=AF.Square, accum_out=ssq[:, i:i + 1]),
                     r=[Bt_], w=[B_junk, B_ssq[i]])
                rms_rstd(ssq[:, i:i + 1], B_ssq[i], 1024.0, rstd[:, i:i + 1], B_rstd[i])
                S.op("dve", lambda e: e.tensor_scalar(out=n_[:], in0=t_[:], scalar1=rstd[:, i:i + 1], scalar2=None,
                                                      op0=ALU.mult), r=[Bt_, B_rstd[i]], w=[Bn_])
                bank = i % 2
                pT = pb[bank][:].bitcast(BF16)
                S.op("pe", [lambda e, k=k: e.transpose(out=pT[:, k * 128:(k + 1) * 128], in_=n_[:, k * 128:(k + 1) * 128],
                                                       identity=identb[:]) for k in range(8)],
                     r=[Bn_, B_identb], w=[Bpb[bank]])
                for k in range(8):
                    if k % 2 == 0:
                        S.op("act", lambda e: e.activation(out=hT[:, k, i * 128:(i + 1) * 128], in_=pT[:, k * 128:(k + 1) * 128],
                                                           func=AF.Identity, bias=shcol[:, k:k + 1], scale=g1col[:, k:k + 1]),
                             r=[Bpb[bank], B_g1, B_sh], w=[B_hT[i]])
                    else:
                        S.op("dve", lambda e: e.tensor_scalar(out=hT[:, k, i * 128:(i + 1) * 128], in0=pT[:, k * 128:(k + 1) * 128],
                                                              scalar1=g1col[:, k:k + 1], scalar2=shcol[:, k:k + 1],
                                                              op0=ALU.mult, op1=ALU.add),
                             r=[Bpb[bank], B_g1, B_sh], w=[B_hT[i]])
            if "hT" in dbg:
                S.dma("sp", lambda e: e.dma_start(out=dbg_out["hT"], in_=hT[:]), r=B_hT)
            S.barrier()

        with ExitStack() as es:
            wt = [sb(es, "wt%d" % i, [128, 8, 128], BF16) for i in range(2)]
            B_wt = [Buf(), Buf()]
            stg = [sb(es, "stg%d" % i, [128, 4096], BF16) for i in range(2)]
            B_stg = [Buf(), Buf()]
            wz = sb(es, "wz", [128, 8, 1536], BF16)
            B_wz = Buf()
            zst = [sb(es, "zst%d" % i, [128, 1536], BF16) for i in range(2)]
            B_zst = [Buf(), Buf()]
            wdt = sb(es, "wdt", [128, 8, 48], BF16)
            B_wdt = Buf()
            dtb_b = sb(es, "dtb_b", [128, 48], F32)
            aneg_b = sb(es, "aneg_b", [128, 48], F32)
            B_dtb, B_aneg = Buf(), Buf()
            w_in_v = w_in.rearrange("(k p) n -> p k n", p=128)
            S.dma("pool", lambda e: e.dma_start(out=wdt[:], in_=w_in_v[:, :, 4608:4656]), w=[B_wdt])
            S.dma("sp", lambda e: e.dma_start(out=dtb_b[:], in_=dtb.partition_broadcast(128)), w=[B_dtb])
            S.dma("sp", lambda e: e.dma_start(out=aneg_b[:], in_=alog.partition_broadcast(128)), w=[B_aneg])
            S.op("act", lambda e: e.activation(out=aneg_b[:], in_=aneg_b[:], func=AF.Exp), r=[B_aneg], w=[B_aneg])
            S.op("dve", lambda e: e.tensor_scalar(out=aneg_b[:], in0=aneg_b[:], scalar1=-1.0, scalar2=None, op0=ALU.mult),
                 r=[B_aneg], w=[B_aneg])
            for i in range(32):
                bank = 4 + (i % 2)
                S.op("pe", [lambda e, k=k: e.matmul(pb[bank][:, 0:48], lhsT=hT[:, k, i * 128:(i + 1) * 128], rhs=wdt[:, k, :],
                                                    start=(k == 0), stop=(k == 7)) for k in range(8)],
                     r=[B_hT[i], B_wdt], w=[Bpb[bank]])
                S.op("dve", lambda e: e.tensor_tensor(out=dt_all[:, i, :], in0=pb[bank][:, 0:48], in1=dtb_b[:], op=ALU.add),
                     r=[Bpb[bank], B_dtb], w=[B_dt])
            S.op("act", lambda e: e.activation(out=dt_all[:], in_=dt_all[:], func=AF.Exp), r=[B_dt], w=[B_dt])
            S.op("act", lambda e: e.activation(out=dt_all[:], in_=dt_all[:], func=AF.Ln, bias=1.0, scale=1.0), r=[B_dt], w=[B_dt])
            S.op("dve", lambda e: e.tensor_tensor(out=a_all[:], in0=dt_all[:], in1=aneg_b[:].unsqueeze(1).broadcast_to([128, 32, 48]),
                                                  op=ALU.mult), r=[B_dt, B_aneg], w=[B_a])
            for i in range(32):
                bank = 4 + (i % 2)
                S.op("pe", [
                    lambda e: e.matmul(pb[bank][:, 0:24], lhsT=mLE, rhs=a_all[:, i, 0:24], start=True, stop=True),
                    lambda e: e.matmul(pb[bank][:, 24:48], lhsT=mGT, rhs=a_all[:, i, 0:24], start=True, stop=True),
                    lambda e: e.matmul(pb[bank][:, 48:72], lhsT=mLT, rhs=a_all[:, i, 24:48], start=True, stop=True),
                    lambda e: e.matmul(pb[bank][:, 72:96], lhsT=mGE, rhs=a_all[:, i, 24:48], start=True, stop=True),
                    lambda e: e.matmul(pb[bank][:, 96:144], lhsT=ones_f, rhs=a_all[:, i, 0:48], start=True, stop=True),
                ], r=[B_a, B_cst], w=[Bpb[bank]])
                S.op("act", lambda e: e.activation(out=dec_all[:, i, :], in_=pb[bank][:, 0:144], func=AF.Exp),
                     r=[Bpb[bank]], w=[B_dec])
            S.op("dve", lambda e: e.tensor_tensor(out=wd_all[:], in0=dt_all[:], in1=dec_all[:, :, 24:72], op=ALU.mult),
                 r=[B_dt, B_dec], w=[B_wd])
            if "proj" in dbg:
                S.dma("sp", lambda e: e.dma_start(out=dbg_out["dt"], in_=dt_all[:]), r=[B_dt])
                S.dma("sp", lambda e: e.dma_start(out=dbg_out["dec"], in_=dec_all[:]), r=[B_dec])
            ev = 0
            for j in range(24):
                col0 = j * 128 if j < 4 else 2048 + (j - 4) * 128
                w_, Bw_ = wt[j % 2], B_wt[j % 2]
                s_, Bs_ = stg[j % 2], B_stg[j % 2]
                S.dma("pool", lambda e: e.dma_start(out=w_[:], in_=w_in_v[:, :, col0:col0 + 128]), w=[Bw_])
                for tt in range(8):
                    bank = tt % 4
                    S.op("pe", [lambda e, k=k: e.matmul(pb[bank][:, :], lhsT=w_[:, k, :], rhs=hT[:, k, tt * 512:(tt + 1) * 512],
                                                        start=(k == 0), stop=(k == 7)) for k in range(8)],
                         r=[Bw_] + B_hT[tt * 4:(tt + 1) * 4], w=[Bpb[bank]])
                    if ev % 2 == 0:
                        S.op("act", lambda e: e.activation(out=s_[:, tt * 512:(tt + 1) * 512], in_=pb[bank][:, :], func=AF.Copy),
                             r=[Bpb[bank]], w=[Bs_])
                    else:
                        S.op("dve", lambda e: e.tensor_copy(out=s_[:, tt * 512:(tt + 1) * 512], in_=pb[bank][:, :]),
                             r=[Bpb[bank]], w=[Bs_])
                    ev += 1
                S.dma("sp", lambda e: e.dma_start(out=projT_d[j * 128:(j + 1) * 128, :], in_=s_[:]), r=[Bs_], w=[B_projT])
            S.dma("pool", lambda e: e.dma_start(out=wz[:], in_=w_in_v[:, :, 512:2048]), w=[B_wz])
            for i in range(16):
                z_, Bz_ = zst[i % 2], B_zst[i % 2]
                for n3 in range(3):
                    bank = (i * 3 + n3) % 4
                    S.op("pe", [lambda e, k=k: e.matmul(pb[bank][:, :], lhsT=hT[:, k, i * 128:(i + 1) * 128],
                                                        rhs=wz[:, k, n3 * 512:(n3 + 1) * 512], start=(k == 0), stop=(k == 7))
                                for k in range(8)], r=[B_hT[i], B_wz], w=[Bpb[bank]])
                    S.op("act", lambda e: e.activation(out=z_[:, n3 * 512:(n3 + 1) * 512], in_=pb[bank][:, :], func=AF.Silu),
                         r=[Bpb[bank]], w=[Bz_])
                S.dma("sp", lambda e: e.dma_start(out=z_d[i * 128:(i + 1) * 128, :], in_=z_[:]), r=[Bz_], w=[B_z])
            S.barrier()
            if "proj" in dbg:
                S.dma("sp", lambda e: e.dma_start(out=dbg_out["proj"], in_=projT_d), r=[B_projT])
                S.dma("sp", lambda e: e.dma_start(out=dbg_out["z"], in_=z_d), r=[B_z])
                S.barrier()
        es_hT.close()

        with ExitStack() as es:
            fT = [sb(es, "fT%d" % i, [128, 4096], BF16) for i in range(2)]
            B_fT = [Buf(), Buf()]
            Z = sb(es, "Z", [128, 32, 4, 256], BF16)
            B_Z = [Buf() for _ in range(4)]
            csct = sb(es, "csct", [128, 256], BF16)
            B_csc = Buf()
            dc = [sb(es, "dc%d" % i, [128, 16, 512], BF16) for i in range(2)]
            ds_ = [sb(es, "ds%d" % i, [128, 16, 512], BF16) for i in range(2)]
            B_dc = [Buf(), Buf()]
            B_ds = [Buf(), Buf()]
            ost = [sb(es, "ost%d" % i, [128, 512], BF16) for i in range(2)]
            B_ost = [Buf(), Buf()]
            S.dma("sp", lambda e: e.dma_start(out=csct[:], in_=csc), w=[B_csc])
            dftc_v = dftc.rearrange("(i p) k -> p i k", p=128)
            dfts_v = dfts.rearrange("(i p) k -> p i k", p=128)
            oc = 0
            for g in range(4):
                f_, Bf_ = fT[g % 2], B_fT[g % 2]
                S.dma("sp", lambda e: e.dma_start(out=f_[:], in_=projT_d[g * 128:(g + 1) * 128, :]), r=[B_projT], w=[Bf_])
                for i in range(32):
                    bank = i % 4
                    S.op("pe", lambda e: e.matmul(pb[bank][:, 0:256], lhsT=f_[:, i * 128:(i + 1) * 128], rhs=csct[:],
                                                  start=True, stop=True), r=[Bf_, B_csc], w=[Bpb[bank]])
                    if i % 2 == 0:
                        S.op("act", lambda e: e.activation(out=Z[:, i, g, :], in_=pb[bank][:, 0:256], func=AF.Copy),
                             r=[Bpb[bank]], w=[B_Z[g]])
                    else:
                        S.op("dve", lambda e: e.tensor_copy(out=Z[:, i, g, :], in_=pb[bank][:, 0:256]),
                             r=[Bpb[bank]], w=[B_Z[g]])
            for kt in range(4):
                for hf in range(2):
                    S.dma("sp", lambda e: e.dma_start(out=dc[hf][:], in_=dftc_v[:, hf * 16:(hf + 1) * 16, kt * 512:(kt + 1) * 512]),
                          w=[B_dc[hf]])
                    S.dma("sp", lambda e: e.dma_start(out=ds_[hf][:], in_=dfts_v[:, hf * 16:(hf + 1) * 16, kt * 512:(kt + 1) * 512]),
                          w=[B_ds[hf]])
                for hf in range(2):
                    for g in range(4):
                        bank = 4 + g
                        fns = []
                        for ii in range(16):
                            i = hf * 16 + ii
                            fns.append(lambda e, i=i, ii=ii: e.matmul(pb[bank][:, :], lhsT=Z[:, i, g, 0:128], rhs=dc[hf][:, ii, :],
                                                                      start=(i == 0), stop=False))
                            fns.append(lambda e, i=i, ii=ii: e.matmul(pb[bank][:, :], lhsT=Z[:, i, g, 128:256], rhs=ds_[hf][:, ii, :],
                                                                      start=False, stop=(i == 31)))
                        S.op("pe", fns, r=[B_Z[g], B_dc[hf], B_ds[hf]], w=[Bpb[bank]])
                for g in range(4):
                    bank = 4 + g
                    o_, Bo_ = ost[oc % 2], B_ost[oc % 2]
                    oc += 1
                    if g % 2 == 0:
                        S.op("act", lambda e: e.activation(out=o_[:], in_=pb[bank][:, :], func=AF.Copy), r=[Bpb[bank]], w=[Bo_])
                    else:
                        S.op("dve", lambda e: e.tensor_copy(out=o_[:], in_=pb[bank][:, :]), r=[Bpb[bank]], w=[Bo_])
                    row0 = g * 128
                    S.dma("sp", lambda e: e.dma_start(out=mixedT_d[row0:row0 + 128, kt * 512:(kt + 1) * 512], in_=o_[:]),
                          r=[Bo_], w=[B_mixed])
            S.barrier()

        convsem = es0.enter_context(nc.semaphore("convsem"))
        for k in range(16):
            nc.gpsimd.dma_start(out=edu_b[k * 1024:(k + 1) * 1024, 0:1024], in_=e_down[k * 1024:(k + 1) * 1024, :]).then_inc(convsem, 16)
            nc.gpsimd.dma_start(out=edu_b[k * 1024:(k + 1) * 1024, 1024:2048], in_=e_up[k * 1024:(k + 1) * 1024, :]).then_inc(convsem, 16)
        nc.gpsimd.dma_start(out=wq_d, in_=w_q).then_inc(convsem, 16)
        nc.gpsimd.dma_start(out=wout_d, in_=w_out).then_inc(convsem, 16)
        S.xsem = {"conv": convsem}
        B_edu.w = (("x", "conv"), 16 * 34)
        B_wqd.w = (("x", "conv"), 16 * 34)
        B_woutd.w = (("x", "conv"), 16 * 34)
        with ExitStack() as es:
            pre = [sb(es, "pre%d" % i, [128, 4100], BF16) for i in range(2)]
            B_pre = [Buf(), Buf()]
            dgc = [sb(es, "dgc%d" % i, [128, 5, 128], BF16) for i in range(2)]
            B_dgc = [Buf(), Buf()]
            cvb = [0]
            cw = sb(es, "cw", [128, 20, 5], F32)
            cb = sb(es, "cb", [128, 20], F32)
            B_cw = Buf()
            xTt = sb(es, "xTt", [128, 3, 4096], BF16)
            BTt = sb(es, "BTt", [128, 4096], BF16)
            CTt = sb(es, "CTt", [128, 2048], BF16)
            B_xT = [Buf() for _ in range(3)]
            B_BT, B_CT = Buf(), Buf()
            xtok = sb(es, "xtok", [128, 16, 384], BF16)
            Btok = sb(es, "Btok", [128, 16, 128], BF16)
            B_xtok = [Buf() for _ in range(16)]
            B_Btok = [Buf() for _ in range(16)]
            xtk = [sb(es, "xtk%d" % i, [128, 512], BF16) for i in range(2)]
            B_xtk = [Buf(), Buf()]
            Sb_all = sb(es, "Sb_all", [128, 16, 384], BF16)
            B_Sb = [Buf() for _ in range(16)]
            Sf32 = [sb(es, "Sf32_%d" % i, [128, 384], F32) for i in range(2)]
            B_S32 = [Buf(), Buf()]
            Sfb = sb(es, "Sfb", [128, 384], BF16)
            B_Sfb = Buf()
            xw = [sb(es, "xw%d" % i, [128, 384], BF16) for i in range(4)]
            B_xw = [Buf() for _ in range(4)]
            wsm = sb(es, "wsm", [128, 8, 6], F32)
            Rt = [sb(es, "Rt%d" % i, [128, 6, 128], F32) for i in range(2)]
            Et = [sb(es, "Et%d" % i, [128, 6, 128], F32) for i in range(2)]
            MT = [sb(es, "MT%d" % i, [128, 6, 128], BF16) for i in range(2)]
            B_R, B_E, B_MT = [Buf(), Buf()], [Buf(), Buf()], [Buf(), Buf()]
            CBm = [sb(es, "CBm%d" % i, [128, 128], F32) for i in range(2)]
            B_CBm = [Buf(), Buf()]
            xwp = [[sb(es, "xwp%d_%d" % (q, i), [128, 384], BF16) for i in range(3)] for q in range(2)]
            B_xwp = [[Buf() for _ in range(3)] for _ in range(2)]
            tCp = [sb(es, "tCp%d" % q, [128, 384], F32) for q in range(2)]
            B_tCp = [Buf(), Buf()]
            CBmp = [[sb(es, "CBmp%d_%d" % (q, i), [128, 128], F32) for i in range(2)] for q in range(2)]
            B_CBmp = [[Buf(), Buf()], [Buf(), Buf()]]
            MTp = [[sb(es, "MTp%d_%d" % (q, i), [128, 6, 128], BF16) for i in range(2)] for q in range(2)]
            B_MTp = [[Buf(), Buf()], [Buf(), Buf()]]
            tA = sb(es, "tA", [128, 384], F32)
            tB = sb(es, "tB", [128, 384], F32)
            tC = sb(es, "tC", [128, 384], F32)
            ynb = sb(es, "ynb", [128, 384], BF16)
            B_tA, B_tB, B_tC, B_ynb = Buf(), Buf(), Buf(), Buf()
            zt = [sb(es, "zt%d" % i, [128, 384], BF16) for i in range(2)]
            B_zt = [Buf(), Buf()]
            mst = sb(es, "mst", [128, 3, 2048], BF16)
            B_mst = Buf()
            gssm_b = sb(es, "gssm_b", [128, 1536], F32)
            dsk_b = sb(es, "dsk_b", [128, 24], F32)
            B_gssm, B_dsk = Buf(), Buf()
            ss4 = sb(es, "ss4", [128, 1], F32)
            rs4 = sb(es, "rs4", [128, 1], F32)
            B_ss4, B_rs4 = Buf(), Buf()
            junk4 = sb(es, "junk4", [128, 384], F32)
            B_junk4 = Buf()
            S.dma("sp", lambda e: e.dma_start(out=cw[:], in_=convw), w=[B_cw])
            S.dma("sp", lambda e: e.dma_start(out=cb[:], in_=convb), w=[B_cw])
            S.dma("sp", lambda e: e.dma_start(out=gssm_b[:], in_=gssm.partition_broadcast(128)), w=[B_gssm])
            S.dma("sp", lambda e: e.dma_start(out=dsk_b[:], in_=dskip.partition_broadcast(128)), w=[B_dsk])
            for p_ in pre:
                S.op("dve", lambda e: e.memset(p_[:, 0:2], 0.0), w=[B_pre[0], B_pre[1]])
                S.op("dve", lambda e: e.memset(p_[:, 4098:4100], 0.0), w=[B_pre[0], B_pre[1]])
            pc = 0
            for g in range(4):
                tiles = [(512 + g * 384 + i * 128, g * 3 + i, xTt[:, i, :], B_xT[i], 4096) for i in range(3)]
                tiles.append((512 + 1536 + g * 128, 12 + g, BTt[:, :], B_BT, 4096))
                tiles.append((512 + 2048 + g * 128, 16 + g, CTt[:, :], B_CT, 2048))
                for (row0, ci, dst, Bdst, ntok) in tiles:
                    p_, Bp_ = pre[pc % 2], B_pre[pc % 2]
                    dgc_, Bdgc_ = dgc[pc % 2], B_dgc[pc % 2]
                    pc += 1
                    S.dma("sp", lambda e: e.dma_start(out=p_[:, 2:4098], in_=projT_d[row0:row0 + 128, :]), r=[B_projT], w=[Bp_])
                    for k in range(5):
                        S.op("dve", lambda e: e.tensor_scalar(out=dgc_[:, k, :], in0=identb[:], scalar1=cw[:, ci, k:k + 1], scalar2=None, op0=ALU.mult),
                             r=[B_identb, B_cw], w=[Bdgc_])
                    for tb in range(ntok // 512):
                        bank = 2 + (cvb[0] % 4)
                        cvb[0] += 1
                        o0 = tb * 512
                        S.op("pe", [lambda e, k=k: e.matmul(pb[bank][:, :], lhsT=dgc_[:, k, :], rhs=p_[:, o0 + k:o0 + k + 512],
                                                            start=(k == 0), stop=(k == 4)) for k in range(5)],
                             r=[Bdgc_, Bp_], w=[Bpb[bank]])
                        S.op("act", lambda e: e.activation(out=dst[:, o0:o0 + 512], in_=pb[bank][:, :], func=AF.Silu, bias=cb[:, ci:ci + 1], scale=1.0),
                             r=[Bpb[bank], B_cw], w=[Bdst])
                if "post" in dbg and g == 0:
                    S.dma("sp", lambda e: e.dma_start(out=dbg_out["post"], in_=xTt[:]), r=B_xT)

                hs0 = g * 6

                def dests(c):
                    if c < 16:
                        return xtok[:, c, :], B_xtok[c], Btok[:, c, :], B_Btok[c]
                    k_ = xtk[c % 2]
                    return k_[:, 0:384], B_xtk[c % 2], k_[:, 384:512], B_xtk[c % 2]

                def stT(c):
                    tb_ = 0 if c % 2 == 0 else 3
                    pT = pb[tb_][:].bitcast(BF16)
                    fns = [lambda e, i=i: e.transpose(out=pT[:, i * 128:(i + 1) * 128], in_=xTt[:, i, c * 128:(c + 1) * 128],
                                                      identity=identb[:]) for i in range(3)]
                    fns.append(lambda e: e.transpose(out=pT[:, 384:512], in_=BTt[:, c * 128:(c + 1) * 128], identity=identb[:]))
                    S.op("pe", fns, r=B_xT + [B_BT, B_identb], w=[Bpb[tb_]])

                def stE(c):
                    tb_ = 0 if c % 2 == 0 else 3
                    pT = pb[tb_][:].bitcast(BF16)
                    xdst, Bx, bdst, Bb = dests(c)
                    S.op("act", lambda e: e.activation(out=xdst, in_=pT[:, 0:384], func=AF.Copy), r=[Bpb[tb_]], w=[Bx])
                    S.op("act", lambda e: e.activation(out=bdst, in_=pT[:, 384:512], func=AF.Copy), r=[Bpb[tb_]], w=[Bb])

                def weighted(dst, Bd, xsrc, Bx, wap):
                    S.op("dve", lambda e: e.tensor_tensor(out=dst.rearrange("l (h p) -> l h p", h=6),
                                                          in0=xsrc.rearrange("l (h p) -> l h p", h=6),
                                                          in1=wap.unsqueeze(2).broadcast_to([128, 6, 64]), op=ALU.mult),
                         r=[Bx, B_dt, B_wd, B_dec, B_dsk], w=[Bd])

                def recur(Sold, Bold, Snew, Bnew, st_bank, etot_ap, first):
                    if first:
                        S.op("dve", lambda e: e.tensor_copy(out=Snew[:], in_=pb[st_bank][:, 0:384]), r=[Bpb[st_bank]], w=[Bnew])
                    else:
                        S.op("dve", lambda e: e.tensor_tensor(out=Snew[:].rearrange("n (h p) -> n h p", h=6),
                                                              in0=Sold[:].rearrange("n (h p) -> n h p", h=6),
                                                              in1=etot_ap.unsqueeze(2).broadcast_to([128, 6, 64]), op=ALU.mult),
                             r=[Bold, B_dec], w=[Bnew])
                        S.op("dve", lambda e: e.tensor_tensor(out=Snew[:], in0=Snew[:], in1=pb[st_bank][:, 0:384], op=ALU.add),
                             r=[Bnew, Bpb[st_bank]], w=[Bnew])

                curA = [0]

                def stW(c):
                    xd, Bx, bd, Bb = dests(c)
                    weighted(xw[c % 2][:], B_xw[c % 2], xd, Bx, wd_all[:, c, 24 + hs0:24 + hs0 + 6])

                def stM(c):
                    xd, Bx, bd, Bb = dests(c)
                    sbk = 1 + (c % 2)
                    S.op("pe", lambda e: e.matmul(pb[sbk][:, 0:384], lhsT=bd, rhs=xw[c % 2][:], start=True, stop=True),
                         r=[Bb, B_xw[c % 2]], w=[Bpb[sbk]])

                def stR(c):
                    sbk = 1 + (c % 2)
                    cur = curA[0]
                    nxt = 1 - cur
                    recur(Sf32[cur], B_S32[cur], Sf32[nxt], B_S32[nxt], sbk, dec_all[:, c, 96 + 24 + hs0:96 + 24 + hs0 + 6], first=(c == 31))
                    curA[0] = nxt
                    if c - 1 < 16:
                        S.op("act", lambda e: e.activation(out=Sb_all[:, c - 1, :], in_=Sf32[nxt][:], func=AF.Copy),
                             r=[B_S32[nxt]], w=[B_Sb[c - 1]])

                stages = [stT, stE, stW, stM, stR]
                for k in range(32 + 4):
                    for si in (4, 3, 2, 1, 0):
                        c = 31 - (k - si)
                        if c < 0 or c > 31:
                            continue
                        if si >= 2 and c == 0:
                            continue
                        stages[si](c)

                curB = [0]

                def front(c):
                    q = c % 2
                    z_, Bz_ = zt[q], B_zt[q]
                    S.dma("sp", lambda e: e.dma_start(out=z_[:], in_=z_d[c * 128:(c + 1) * 128, g * 384:(g + 1) * 384]), r=[B_z], w=[Bz_])
                    xs, Bxs = xtok[:, c, :], B_xtok[c]
                    S.op("pe", lambda e: e.matmul(pb[4][:, 0:128], lhsT=BTt[:, c * 128:(c + 1) * 128], rhs=CTt[:, c * 128:(c + 1) * 128],
                                                  start=True, stop=True), r=[B_BT, B_CT], w=[Bpb[4]])
                    yield
                    for d in range(2):
                        acol = d * 24 + hs0
                        U = mLE if d == 0 else mGE
                        S.op("dve", lambda e: e.tensor_tensor(out=Rt[d][:], in0=U.unsqueeze(1).broadcast_to([128, 6, 128]),
                                                              in1=a_all[:, c, acol:acol + 6].unsqueeze(2).broadcast_to([128, 6, 128]),
                                                              op=ALU.mult), r=[B_cst, B_a], w=[B_R[d]])
                        yield
                    for d in range(2):
                        acol = d * 24 + hs0
                        weighted(xwp[q][d][:], B_xwp[q][d], xs, Bxs, dt_all[:, c, acol:acol + 6])
                        yield
                    for d in range(2):
                        LT = mGT if d == 0 else mLT
                        Rf = Rt[d][:].rearrange("m h l -> m (h l)")
                        S.op("pe", [lambda e: e.matmul(pb[2][:, :], lhsT=LT, rhs=Rf[:, 0:512], start=True, stop=True),
                                    lambda e: e.matmul(pb[3][:, 0:256], lhsT=LT, rhs=Rf[:, 512:768], start=True, stop=True)],
                             r=[B_R[d], B_cst], w=[Bpb[2], Bpb[3]])
                        yield
                        if d == 0:
                            if c < 15:
                                weighted(xwp[q][2][:], B_xwp[q][2], xs, Bxs, wd_all[:, c, hs0:hs0 + 6])
                                yield
                            weighted(tCp[q][:], B_tCp[q], xs, Bxs, dsk_b[:, hs0:hs0 + 6])
                            yield
                        Ef = Et[d][:].rearrange("m h l -> m (h l)")
                        S.op("act", lambda e: e.activation(out=Ef[:, 0:512], in_=pb[2][:, :], func=AF.Exp), r=[Bpb[2]], w=[B_E[d]])
                        yield
                        S.op("act", lambda e: e.activation(out=Ef[:, 512:768], in_=pb[3][:, 0:256], func=AF.Exp), r=[Bpb[3]], w=[B_E[d]])
                        yield
                        if d == 0:
                            S.op("dve", lambda e: e.tensor_tensor(out=CBmp[q][0][:], in0=pb[4][:, 0:128], in1=mLE, op=ALU.mult),
                                 r=[Bpb[4], B_cst], w=[B_CBmp[q][0]])
                            yield
                            S.op("dve", lambda e: e.tensor_tensor(out=CBmp[q][1][:], in0=pb[4][:, 0:128], in1=mGE, op=ALU.mult),
                                 r=[Bpb[4], B_cst], w=[B_CBmp[q][1]])
                            yield
                    for d in range(2):
                        S.op("dve", lambda e: e.tensor_tensor(out=MTp[q][d][:], in0=Et[d][:],
                                                              in1=CBmp[q][d][:].unsqueeze(1).broadcast_to([128, 6, 128]), op=ALU.mult),
                             r=[B_E[d], B_CBmp[q][d]], w=[B_MTp[q][d]])
                        yield

                def back(c):
                    q = c % 2
                    z_, Bz_ = zt[q], B_zt[q]
                    MT_, B_MT_ = MTp[q], B_MTp[q]
                    xw_, B_xw_ = xwp[q], B_xwp[q]
                    fns = []
                    for h in range(6):
                        fns.append(lambda e, h=h: e.matmul(pb[6][:, h * 64:(h + 1) * 64], lhsT=MT_[0][:, h, :], rhs=xw_[0][:, h * 64:(h + 1) * 64],
                                                           start=True, stop=False))
                        fns.append(lambda e, h=h: e.matmul(pb[6][:, h * 64:(h + 1) * 64], lhsT=MT_[1][:, h, :], rhs=xw_[1][:, h * 64:(h + 1) * 64],
                                                           start=False, stop=True))
                    S.op("pe", fns, r=[B_MT_[0], B_MT_[1], B_xw_[0], B_xw_[1]], w=[Bpb[6]])
                    yield
                    S.op("pe", lambda e: e.matmul(pb[7][:, 0:384], lhsT=CTt[:, c * 128:(c + 1) * 128], rhs=Sb_all[:, c, :], start=True, stop=True),
                         r=[B_CT, B_Sb[c]], w=[Bpb[7]])
                    yield
                    if c < 15:
                        S.op("pe", lambda e: e.matmul(pb[1][:, 0:384], lhsT=Btok[:, c, :], rhs=xw_[2][:], start=True, stop=True),
                             r=[B_Btok[c], B_xw_[2]], w=[Bpb[1]])
                        yield
                    S.op("dve", lambda e: e.tensor_tensor(out=tA[:].rearrange("l (h p) -> l h p", h=6),
                                                          in0=pb[7][:, 0:384].rearrange("l (h p) -> l h p", h=6),
                                                          in1=dec_all[:, c, 72 + hs0:72 + hs0 + 6].unsqueeze(2).broadcast_to([128, 6, 64]),
                                                          op=ALU.mult), r=[Bpb[7], B_dec], w=[B_tA])
                    yield
                    if c > 0:
                        S.op("pe", lambda e: e.matmul(pb[7][:, 0:384], lhsT=CTt[:, c * 128:(c + 1) * 128], rhs=Sfb[:], start=True, stop=True),
                             r=[B_CT, B_Sfb], w=[Bpb[7]])
                        yield
                    if c < 15:
                        cur = curB[0]
                        nxt = 1 - cur
                        recur(Sf32[cur], B_S32[cur], Sf32[nxt], B_S32[nxt], 1, dec_all[:, c, 96 + hs0:96 + hs0 + 6], first=(c == 0))
                        curB[0] = nxt
                        yield
                    if c > 0:
                        S.op("dve", lambda e: e.tensor_tensor(out=tB[:].rearrange("l (h p) -> l h p", h=6),
                                                              in0=pb[7][:, 0:384].rearrange("l (h p) -> l h p", h=6),
                                                              in1=dec_all[:, c, hs0:hs0 + 6].unsqueeze(2).broadcast_to([128, 6, 64]),
                                                              op=ALU.mult), r=[Bpb[7], B_dec], w=[B_tB])
                        yield
                        S.op("dve", lambda e: e.tensor_tensor(out=tA[:], in0=tA[:], in1=tB[:], op=ALU.add), r=[B_tA, B_tB], w=[B_tA])
                        yield
                    if c < 15:
                        S.op("act", lambda e: e.activation(out=Sfb[:], in_=Sf32[curB[0]][:], func=AF.Copy), r=[B_S32[curB[0]]], w=[B_Sfb])
                        yield
                    S.op("dve", lambda e: e.tensor_tensor(out=tA[:], in0=tA[:], in1=pb[6][:, 0:384], op=ALU.add), r=[B_tA, Bpb[6]], w=[B_tA])
                    yield
                    S.op("dve", lambda e: e.tensor_tensor(out=tA[:], in0=tA[:], in1=tCp[q][:], op=ALU.add), r=[B_tA, B_tCp[q]], w=[B_tA])
                    yield
                    S.op("dve", lambda e: e.tensor_tensor(out=tA[:], in0=tA[:], in1=z_[:], op=ALU.mult), r=[B_tA, Bz_], w=[B_tA])
                    yield
                    S.op("act", lambda e: e.activation(out=junk4[:], in_=tA[:], func=AF.Square, accum_out=ss4[:, 0:1]),
                         r=[B_tA], w=[B_junk4, B_ss4])
                    yield
                    rms_rstd(ss4[:, 0:1], B_ss4, 384.0, rs4[:, 0:1], B_rs4)
                    yield
                    S.op("dve", lambda e: e.scalar_tensor_tensor(out=ynb[:], in0=tA[:], scalar=rs4[:, 0:1], in1=gssm_b[:, g * 384:(g + 1) * 384],
                                                                 op0=ALU.mult, op1=ALU.mult), r=[B_tA, B_rs4, B_gssm], w=[B_ynb])
                    yield
                    pT = pb[0][:].bitcast(BF16)
                    S.op("pe", [lambda e, i=i: e.transpose(out=pT[:, i * 128:(i + 1) * 128], in_=ynb[:, i * 128:(i + 1) * 128], identity=identb[:])
                                for i in range(3)], r=[B_ynb, B_identb], w=[Bpb[0]])
                    yield
                    S.op("act", lambda e: e.activation(out=mst[:, :, c * 128:(c + 1) * 128], in_=pT[:, 0:384].rearrange("p (i l) -> p i l", i=3),
                                                       func=AF.Copy), r=[Bpb[0]], w=[B_mst])
                    yield

                for _ in front(0):
                    pass
                for c in range(16):
                    gb = back(c)
                    gf = front(c + 1) if c + 1 < 16 else None
                    while gb is not None or gf is not None:
                        if gb is not None:
                            try:
                                next(gb)
                            except StopIteration:
                                gb = None
                        if gf is not None:
                            try:
                                next(gf)
                            except StopIteration:
                                gf = None
                for i in range(3):
                    row0 = 512 + g * 384 + i * 128
                    S.dma("sp", lambda e: e.dma_start(out=mixedT_d[row0:row0 + 128, :], in_=mst[:, i, :]), r=[B_mst], w=[B_mixed])
            S.barrier()
        es_ssd.close()
        if "mixed" in dbg:
            S.dma("sp", lambda e: e.dma_start(out=dbg_out["mixed"], in_=mixedT_d), r=[B_mixed])
            S.barrier()

        with ExitStack() as es:
            wout = sb(es, "wout", [128, 16, 1024], BF16)
            wq = sb(es, "wq", [128, 8, 2048], BF16)
            kT = sb(es, "kT", [128, 16, 128], BF16)
            B_wout, B_wq, B_kT = Buf(), Buf(), Buf()
            S.dma("sp", lambda e: e.dma_start(out=wout[:], in_=wout_d.rearrange("(k p) n -> p k n", p=128)), r=[B_woutd], w=[B_wout])
            S.dma("sp", lambda e: e.dma_start(out=wq[:], in_=wq_d.rearrange("(k p) n -> p k n", p=128)), r=[B_wqd], w=[B_wq])
            S.dma("pool", lambda e: e.dma_start(out=kT[:], in_=keysT), w=[B_kT])
            gm_b = sb(es, "gm_b", [128, 1024], F32)
            g2_b = sb(es, "g2_b", [128, 1024], F32)
            sf_b = sb(es, "sf_b", [128, 1024], F32)
            gf_b = sb(es, "gf_b", [128, 1024], F32)
            gfin_b = sb(es, "gfin_b", [128, 1024], F32)
            ot = sb(es, "ot", [128, 1024], F32)
            B_rows, B_ot = Buf(), Buf()
            io16 = sb(es, "io16", [128, 16], F32)
            S.dma("sp", lambda e: e.dma_start(out=io16[:], in_=iota16), w=[B_rows])
            S.dma("sp", lambda e: e.dma_start(out=gm_b[:], in_=mod_d[0:1, 2048:3072].partition_broadcast(128)), r=[B_mod], w=[B_rows])
            S.dma("sp", lambda e: e.dma_start(out=sf_b[:], in_=mod_d[0:1, 3072:4096].partition_broadcast(128)), r=[B_mod], w=[B_rows])
            S.dma("sp", lambda e: e.dma_start(out=g2_b[:], in_=mod_d[0:1, 4096:5120].partition_broadcast(128)), r=[B_mod], w=[B_rows])
            S.dma("sp", lambda e: e.dma_start(out=gf_b[:], in_=mod_d[0:1, 5120:6144].partition_broadcast(128)), r=[B_mod], w=[B_rows])
            S.dma("sp", lambda e: e.dma_start(out=gfin_b[:], in_=gfin.partition_broadcast(128)), w=[B_rows])
            S.dma("sp", lambda e: e.dma_start(out=ot[:], in_=gffn.partition_broadcast(128)), w=[B_ot])
            S.op("dve", lambda e: e.scalar_tensor_tensor(out=g2_b[:], in0=g2_b[:], scalar=1.0, in1=ot[:], op0=ALU.add, op1=ALU.mult),
                 r=[B_rows, B_ot], w=[B_rows])
            mT = sb(es, "mT", [128, 16, 128], BF16)
            xt5 = sb(es, "xt5", [128, 1024], F32)
            B_mT, B_xt5 = Buf(), Buf()
            h2f, B_h2f = xt5, B_xt5
            x1 = [sb(es, "x1_%d" % i, [128, 1024], F32) for i in range(2)]
            h2b = [sb(es, "h2b_%d" % i, [128, 1024], BF16) for i in range(2)]
            B_x1, B_h2b = [Buf(), Buf()], [Buf(), Buf()]
            h2T = sb(es, "h2T", [128, 8, 128], BF16)
            qT = sb(es, "qT", [128, 16, 128], BF16)
            sc = [sb(es, "sc%d" % i, [128, 4, 128], F32) for i in range(2)]
            sc2 = [sb(es, "sc2_%d" % i, [128, 256], F32) for i in range(2)]
            B_sc2 = [Buf() for _ in range(2)]
            B_h2T, B_qT = Buf(), Buf()
            B_tl = [Buf() for _ in range(16)]
            B_ohh = [Buf() for _ in range(4)]
            B_il = [Buf() for _ in range(16)]
            B_cvl = [Buf() for _ in range(8)]
            B_cpl = [Buf() for _ in range(8)]
            B_sc = [Buf(), Buf()]
            junkr = [sb(es, "junkr%d" % i, [128, 1024], BF16) for i in range(3)]
            B_junkr = [Buf() for _ in range(3)]
            junkb, B_junkb = junkr[0], B_junkr[0]
            ss5 = sb(es, "ss5", [128, 2], F32)
            rs5 = sb(es, "rs5", [128, 2], F32)
            B_ss5, B_rs5 = [Buf(), Buf()], [Buf(), Buf()]
            tv = sb(es, "tv", [128, 16, 16], F32)
            ti = sb(es, "ti", [128, 16, 16], U32)
            tif = sb(es, "tif", [128, 16, 16], F32)
            B_tv, B_ti, B_tif = Buf(), Buf(), Buf()
            cand = sb(es, "cand", [128, 8, 256], F32)
            B_cand = Buf()
            oh = cand[:].rearrange("p h (a b) -> p h a b", a=16)
            cv = sb(es, "cv", [128, 8, 16], F32)
            cpos = sb(es, "cpos", [128, 8, 16], U32)
            cpa = sb(es, "cpa", [128, 8, 16], U32)
            cpb_ = sb(es, "cpb", [128, 8, 16], U32)
            cpaf = sb(es, "cpaf", [128, 8, 16], F32)
            cpbf = sb(es, "cpbf", [128, 8, 16], F32)
            B_cv, B_cpos, B_cpa, B_cpb, B_cpaf, B_cpbf = (Buf() for _ in range(6))
            Iv = sb(es, "Iv", [128, 8, 16], F32)
            Jv = sb(es, "Jv", [128, 8, 16], F32)
            idxf = sb(es, "idxf", [128, 128], F32)
            idxi = [sb(es, "idxi%d" % i, [128, 128], I32) for i in range(2)]
            gate = [sb(es, "gate%d" % i, [128, 8, 16], F32) for i in range(2)]
            gsum = sb(es, "gsum", [128, 8], F32)
            B_Iv, B_Jv, B_idxf, B_gsum = (Buf() for _ in range(4))
            B_idxi, B_gate = [Buf(), Buf()], [Buf(), Buf()]
            actv = sb(es, "actv", [128, 128], F32)
            wv = sb(es, "wv", [128, 128], F32)
            GS = 4
            NGRP = 128 // GS
            B_actg = [Buf() for _ in range(NGRP)]
            B_wvg = [Buf() for _ in range(NGRP)]
            NG = 13
            Rg = sb(es, "Rg", [128, NG * 2048], BF16)
            B_gt = [Buf() for _ in range(NG)]
            NDG = 3
            dg = [sb(es, "dg%d" % i, [128, 128], BF16) for i in range(NDG)]
            B_dg = [Buf() for _ in range(NDG)]
            mixv = mixedT_d.rearrange("(k p) t -> p k t", p=128)
            rot = [0]

            def qbank():
                rot[0] += 1
                return 5 + rot[0] % 3

            def gen5a(i):
                p = i % 2
                S.dma("sp", lambda e: e.dma_start(out=mT[:], in_=mixv[:, :, i * 128:(i + 1) * 128]), r=[B_mixed], w=[B_mT])
                S.dma("sp", lambda e: e.dma_start(out=xt5[:], in_=x[i * 128:(i + 1) * 128, :]), w=[B_xt5])

                yield
                for kk in range(4):
                    fns = []
                    for hf in range(2):
                        for k4 in range(4):
                            k = kk * 4 + k4
                            fns.append(lambda e, k=k, hf=hf: e.matmul(pb[2 + hf][:, :], lhsT=mT[:, k, :], rhs=wout[:, k, hf * 512:(hf + 1) * 512],
                                                                      start=(k == 0), stop=(k == 15)))
                    S.op("pe", fns, r=[B_mT, B_wout], w=[Bpb[2], Bpb[3]])
                    yield
                yield
                for hf in range(2):
                    bk = 2 + hf
                    S.op("dve", lambda e: e.tensor_tensor(out=x1[p][:, hf * 512:(hf + 1) * 512], in0=pb[bk][:, :], in1=gm_b[:, hf * 512:(hf + 1) * 512],
                                                          op=ALU.mult), r=[Bpb[bk], B_rows], w=[B_x1[p]])
                yield
                S.op("dve", lambda e: e.tensor_tensor(out=x1[p][:], in0=x1[p][:], in1=xt5[:], op=ALU.add), r=[B_x1[p], B_xt5], w=[B_x1[p]])
                yield
                S.op("act", lambda e: e.activation(out=h2f[:], in_=x1[p][:], func=AF.Square, accum_out=ss5[:, 0:1]),
                     r=[B_x1[p]], w=[B_h2f, B_ss5[0]])
                yield
                S.op("act", lambda e: e.activation(out=rs5[:, 0:1], in_=ss5[:, 0:1], func=AF.Ln, bias=epst[:, 0:1], scale=1.0 / 1024.0),
                     r=[B_ss5[0], B_eps], w=[B_rs5[0]])
                yield
                S.op("act", lambda e: e.activation(out=rs5[:, 0:1], in_=rs5[:, 0:1], func=AF.Exp, scale=-0.5), r=[B_rs5[0]], w=[B_rs5[0]])
                yield
                S.op("dve", lambda e: e.scalar_tensor_tensor(out=h2f[:], in0=x1[p][:], scalar=rs5[:, 0:1], in1=g2_b[:], op0=ALU.mult, op1=ALU.mult),
                     r=[B_x1[p], B_rs5[0], B_rows], w=[B_h2f])
                yield
                S.op("dve", lambda e: e.tensor_tensor(out=h2b[p][:], in0=h2f[:], in1=sf_b[:], op=ALU.add), r=[B_h2f, B_rows], w=[B_h2b[p]])

                yield
                yield
                pT = pb[4][:].bitcast(BF16)
                S.op("pe", [lambda e, k=k: e.transpose(out=pT[:, k * 128:(k + 1) * 128], in_=h2b[p][:, k * 128:(k + 1) * 128], identity=identb[:])
                            for k in range(8)], r=[B_h2b[p], B_identb], w=[Bpb[4]])
                yield
                yield
                S.op("act", lambda e: e.activation(out=h2T[:].rearrange("p k t -> p (k t)"), in_=pT[:, :], func=AF.Copy), r=[Bpb[4]], w=[B_h2T])
                yield
                yield
                qb = [5, 6, 7, 5]
                for q4 in range(5):
                    if q4 < 4:
                        bk = qb[q4]
                        fns = []
                        for j in range(4):
                            hs = q4 * 4 + j
                            for k in range(8):
                                fns.append(lambda e, hs=hs, j=j, k=k: e.matmul(pb[bk][:, j * 128:(j + 1) * 128], lhsT=wq[:, k, hs * 128:(hs + 1) * 128],
                                                                               rhs=h2T[:, k, :], start=(k == 0), stop=(k == 7)))
                        S.op("pe", fns, r=[B_wq, B_h2T], w=[Bpb[bk]])
                        yield
                    if q4 > 0:
                        qq = q4 - 1
                        bk = qb[qq]
                        S.op("act", lambda e: e.activation(out=qT[:, qq * 4:(qq + 1) * 4, :].rearrange("p j t -> p (j t)"), in_=pb[bk][:, :], func=AF.Copy),
                             r=[Bpb[bk]], w=[B_qT])
                        yield
                yield
                sb_ = [6, 7, 5, 6]

                def sc_pe(q4):
                    bk = sb_[q4]
                    S.op("pe", [lambda e, j=j: e.matmul(pb[bk][:, j * 128:(j + 1) * 128], lhsT=qT[:, q4 * 4 + j, :], rhs=kT[:, q4 * 4 + j, :],
                                                        start=True, stop=True) for j in range(4)], r=[B_qT, B_kT], w=[Bpb[bk]])

                def sc_act(q4):
                    bk = sb_[q4]
                    s_, Bs_ = sc[q4 % 2], B_sc[q4 % 2]
                    S.op("act", lambda e: e.activation(out=s_[:].rearrange("p j t -> p (j t)"), in_=pb[bk][:, :], func=AF.Copy),
                         r=[Bpb[bk]], w=[Bs_])

                sc_pe(0)
                yield
                sc_pe(1)
                yield
                sc_act(0)
                yield
                for q4 in range(4):
                    s_, Bs_ = sc[q4 % 2], B_sc[q4 % 2]
                    if q4 == 0:
                        sc_act(1)
                    for pr in range(2):
                        js = [pr * 2, pr * 2 + 1]
                        for j in js:
                            hs = q4 * 4 + j
                            S.op("dve", lambda e: e.max(out=tv[:, hs, 0:8], in_=s_[:, j, :]), r=[Bs_], w=[B_tl[hs]])
                        yield
                        for j in js:
                            hs = q4 * 4 + j
                            S.op("dve", lambda e: e.max_index(out=ti[:, hs, 0:8], in_max=tv[:, hs, 0:8], in_values=s_[:, j, :]), r=[Bs_, B_tl[hs]], w=[B_il[hs]])
                        yield
                        for j in js:
                            hs = q4 * 4 + j
                            S.op("dve", lambda e: e.match_replace(out=sc2[j % 2][:, 0:128], in_to_replace=tv[:, hs, 0:8], in_values=s_[:, j, :], imm_value=NEG),
                                 r=[Bs_, B_tl[hs]], w=[B_sc2[j % 2]])
                        if pr == 0 and q4 + 2 < 4:
                            sc_pe(q4 + 2)
                        yield
                        for j in js:
                            hs = q4 * 4 + j
                            S.op("dve", lambda e: e.max(out=tv[:, hs, 8:16], in_=sc2[j % 2][:, 0:128]), r=[B_sc2[j % 2]], w=[B_tl[hs]])
                        yield
                        for j in js:
                            hs = q4 * 4 + j
                            S.op("dve", lambda e: e.max_index(out=ti[:, hs, 8:16], in_max=tv[:, hs, 8:16], in_values=sc2[j % 2][:, 0:128]), r=[B_sc2[j % 2], B_tl[hs]], w=[B_il[hs]])
                        yield
                    if 2 <= q4 + 1 < 4:
                        sc_act(q4 + 1)
                        yield
                S.op("dve", lambda e: e.tensor_copy(out=tif[:], in_=ti[:]), r=B_il, w=[B_tif])
                tv4 = tv[:].rearrange("p (h j) a -> p h j a", j=2)
                tif4 = tif[:].rearrange("p (h j) a -> p h j a", j=2)
                S.op("dve", lambda e: e.tensor_tensor(out=cand[:].rearrange("p h (a b) -> p h a b", a=16),
                                                      in0=tv4[:, :, 0, :].unsqueeze(3).broadcast_to([128, 8, 16, 16]),
                                                      in1=tv4[:, :, 1, :].unsqueeze(2).broadcast_to([128, 8, 16, 16]), op=ALU.add),
                     r=B_tl, w=[B_cand])
                yield
                for h2_ in range(4):
                    hl = [(h2_ * 2, 0), (h2_ * 2 + 1, 1)]
                    for h, j in hl:
                        S.op("dve", lambda e: e.max(out=cv[:, h, 0:8], in_=cand[:, h, :]), r=[B_cand], w=[B_cvl[h]])
                    yield
                    for h, j in hl:
                        S.op("dve", lambda e: e.max_index(out=cpos[:, h, 0:8], in_max=cv[:, h, 0:8], in_values=cand[:, h, :]), r=[B_cand, B_cvl[h]], w=[B_cpl[h]])
                    yield
                    for h, j in hl:
                        S.op("dve", lambda e: e.match_replace(out=sc2[j][:, :], in_to_replace=cv[:, h, 0:8], in_values=cand[:, h, :], imm_value=NEG),
                             r=[B_cand, B_cvl[h]], w=[B_sc2[j % 2]])
                    yield
                    for h, j in hl:
                        S.op("dve", lambda e: e.max(out=cv[:, h, 8:16], in_=sc2[j][:, :]), r=[B_sc2[j % 2]], w=[B_cvl[h]])
                    yield
                    for h, j in hl:
                        S.op("dve", lambda e: e.max_index(out=cpos[:, h, 8:16], in_max=cv[:, h, 8:16], in_values=sc2[j][:, :]), r=[B_sc2[j], B_cvl[h]], w=[B_cpl[h]])
                    yield
                S.op("dve", lambda e: e.tensor_single_scalar(out=cpa[:], in_=cpos[:], scalar=4, op=ALU.logical_shift_right), r=B_cpl, w=[B_cpa])
                S.op("dve", lambda e: e.tensor_single_scalar(out=cpb_[:], in_=cpos[:], scalar=15, op=ALU.bitwise_and), r=B_cpl, w=[B_cpb])
                S.op("dve", lambda e: e.tensor_copy(out=cpaf[:], in_=cpa[:]), r=[B_cpa], w=[B_cpaf])
                S.op("dve", lambda e: e.tensor_copy(out=cpbf[:], in_=cpb_[:]), r=[B_cpb], w=[B_cpbf])
                yield
                for (pf, Bpf, side, dstv, Bdst) in [(cpaf, B_cpaf, 0, Iv, B_Iv), (cpbf, B_cpbf, 1, Jv, B_Jv)]:
                    for hh in range(4):
                        hsl = slice(hh * 2, hh * 2 + 2)
                        S.op("dve", lambda e: e.tensor_tensor(out=oh[:, hsl], in0=pf[:, hsl, :].unsqueeze(3).broadcast_to([128, 2, 16, 16]),
                                                              in1=io16[:].unsqueeze(1).unsqueeze(1).broadcast_to([128, 2, 16, 16]), op=ALU.is_equal),
                             r=[Bpf, B_rows], w=[B_ohh[hh]])
                        yield
                        S.op("dve", lambda e: e.tensor_tensor(out=oh[:, hsl], in0=oh[:, hsl], in1=tif4[:, hsl, side, :].unsqueeze(2).broadcast_to([128, 2, 16, 16]),
                                                              op=ALU.mult), r=[B_ohh[hh], B_tif], w=[B_ohh[hh]])
                        yield
                        S.op("dve", lambda e: e.tensor_reduce(out=dstv[:, hsl, :], in_=oh[:, hsl], axis=AX.X, op=ALU.add), r=[B_ohh[hh]], w=[Bdst])
                        yield
                S.op("dve", lambda e: e.scalar_tensor_tensor(out=idxf[:], in0=Iv[:].rearrange("p h k -> p (h k)"), scalar=128.0,
                                                             in1=Jv[:].rearrange("p h k -> p (h k)"), op0=ALU.mult, op1=ALU.add),
                     r=[B_Iv, B_Jv], w=[B_idxf])
                S.op("dve", lambda e: e.tensor_copy(out=idxi[p][:], in_=idxf[:]), r=[B_idxf], w=[B_idxi[p]])
                yield
                S.op("dve", lambda e: e.tensor_tensor(out=gate[p][:], in0=cv[:], in1=cv[:, :, 0:1].broadcast_to([128, 8, 16]), op=ALU.subtract),
                     r=B_cvl, w=[B_gate[p]])
                S.op("act", lambda e: e.activation(out=gate[p][:], in_=gate[p][:], func=AF.Exp), r=[B_gate[p]], w=[B_gate[p]])
                S.op("dve", lambda e: e.tensor_reduce(out=gsum[:], in_=gate[p][:], axis=AX.X, op=ALU.add), r=[B_gate[p]], w=[B_gsum])
                S.op("dve", lambda e: e.reciprocal(out=gsum[:], in_=gsum[:]), r=[B_gsum], w=[B_gsum])
                S.op("dve", lambda e: e.tensor_tensor(out=gate[p][:], in0=gate[p][:], in1=gsum[:].unsqueeze(2).broadcast_to([128, 8, 16]), op=ALU.mult),
                     r=[B_gate[p], B_gsum], w=[B_gate[p]])
                yield

            gcn = [0]
            dcn = [0]
            edu3 = edu_b.rearrange("e (a d) -> e a d", a=2)

            def S1(i, grp):
                p = i % 2
                ul = []
                for s_ in range(grp * GS, (grp + 1) * GS):
                    n = gcn[0]
                    gcn[0] += 1
                    u_ = n % NG
                    od = u_ * 2048
                    S.dma("pool", lambda e: e.indirect_dma_start(out=Rg[:, od:od + 2048], out_offset=None, in_=edu_b,
                                                                 in_offset=bass.IndirectOffsetOnAxis(ap=idxi[p][:, s_:s_ + 1], axis=0)),
                          r=[B_idxi[p], B_edu], w=[B_gt[u_]])
                    jb_, Bjb_ = junkr[n % 3], B_junkr[n % 3]
                    S.op("dve", lambda e: e.tensor_tensor(out=jb_[:], in0=Rg[:, od:od + 1024], in1=h2b[p][:], op=ALU.mult),
                         r=[B_gt[u_], B_h2b[p]], w=[Bjb_])
                    S.op("act", lambda e: e.activation(out=jb_[:], in_=jb_[:], func=AF.Copy, accum_out=actv[:, s_:s_ + 1]),
                         r=[Bjb_], w=[Bjb_, B_actg[grp]])
                    ul.append(u_)
                    tick()
                return ul

            def S2(i, grp):
                p = i % 2
                s0 = grp * GS
                gflat = gate[p][:].rearrange("p h k -> p (h k)")
                S.op("act", lambda e: e.activation(out=wv[:, s0:s0 + GS], in_=actv[:, s0:s0 + GS], func=AF.Gelu), r=[B_actg[grp]], w=[B_wvg[grp]])

            def S3(i, grp, ul):
                p = i % 2
                gflat = gate[p][:].rearrange("p h k -> p (h k)")
                for j, s_ in enumerate(range(grp * GS, (grp + 1) * GS)):
                    u_ = ul[j]
                    ou = u_ * 2048 + 1024
                    d_ = dcn[0] % NDG
                    dcn[0] += 1
                    S.op("dve", lambda e: e.tensor_scalar(out=dg[d_][:], in0=identb[:], scalar1=wv[:, s_:s_ + 1], scalar2=gflat[:, s_:s_ + 1],
                                                          op0=ALU.mult, op1=ALU.mult),
                         r=[B_identb, B_wvg[grp], B_gate[p]], w=[B_dg[d_]])
                    S.op("pe", [lambda e, hf=hf: e.matmul(pb[hf][:, :], lhsT=dg[d_][:], rhs=Rg[:, ou + hf * 512:ou + (hf + 1) * 512],
                                                          start=(s_ == 0), stop=(s_ == 127)) for hf in range(2)],
                         r=[B_dg[d_], B_gt[u_]], w=[Bpb[0], Bpb[1]])

            def fin(i):
                p = i % 2
                for hf in range(2):
                    S.op("dve", lambda e: e.tensor_tensor(out=ot[:, hf * 512:(hf + 1) * 512], in0=pb[hf][:, :], in1=gf_b[:, hf * 512:(hf + 1) * 512], op=ALU.mult),
                         r=[Bpb[hf], B_rows], w=[B_ot])
                S.op("dve", lambda e: e.tensor_tensor(out=ot[:], in0=ot[:], in1=x1[p][:], op=ALU.add), r=[B_ot, B_x1[p]], w=[B_ot])
                S.op("act", lambda e: e.activation(out=junkb[:], in_=ot[:], func=AF.Square, accum_out=ss5[:, 1:2]),
                     r=[B_ot], w=[B_junkb, B_ss5[1]])
                rms_rstd(ss5[:, 1:2], B_ss5[1], 1024.0, rs5[:, 1:2], B_rs5[1])
                S.op("dve", lambda e: e.scalar_tensor_tensor(out=ot[:], in0=ot[:], scalar=rs5[:, 1:2], in1=gfin_b[:], op0=ALU.mult, op1=ALU.mult),
                     r=[B_ot, B_rs5[1], B_rows], w=[B_ot])
                S.dma("sp", lambda e: e.dma_start(out=out[i * 128:(i + 1) * 128, :], in_=ot[:]), r=[B_ot])

            for _ in gen5a(0):
                pass
            TOT = 16 * NGRP
            uls = {}
            nxt = None
            nxt_box = [None]

            def tick():
                if nxt_box[0] is not None:
                    try:
                        next(nxt_box[0])
                    except StopIteration:
                        nxt_box[0] = None
            for G in range(TOT + 1):
                if G < TOT:
                    i, grp = divmod(G, NGRP)
                    if grp == 0 and nxt_box[0] is not None:
                        for _ in nxt_box[0]:
                            pass
                        nxt_box[0] = None
                    uls[G] = S1(i, grp)
                    S2(i, grp)
                if 0 <= G - 1 < TOT:
                    i2, g2 = divmod(G - 1, NGRP)
                    S3(i2, g2, uls.pop(G - 1))
                    if g2 == NGRP - 1:
                        fin(i2)
                if G < TOT and grp == 1 and i + 1 < 16:
                    nxt_box[0] = gen5a(i + 1)
            S.barrier()
        S.barrier(engines=("sp",))
        es_h.close()
    return nc


def _dft_tables():
    n = np.arange(4096, dtype=np.int64)
    prod = (n[:, None] * n[None, :]) % 4096
    ang = 2.0 * np.pi * prod.astype(np.float64) / 4096.0
    Cs = np.cos(ang) / 64.0
    Ss = np.sin(ang) / 64.0
    c = np.arange(128, dtype=np.int64)
    angc = 2.0 * np.pi * ((c[:, None] * c[None, :]) % 128).astype(np.float64) / 128.0
    csc = np.concatenate([np.cos(angc), -np.sin(angc)], axis=1) / np.sqrt(128.0)
    return Cs, Ss, csc


def _consts():
    m = np.arange(128)
    ident = np.eye(128)
    GT = (m[:, None] > m[None, :]).astype(np.float64)
    LT = (m[:, None] < m[None, :]).astype(np.float64)
    LE = (m[:, None] <= m[None, :]).astype(np.float64)
    GE = (m[:, None] >= m[None, :]).astype(np.float64)
    ones = np.ones((128, 128))
    return np.stack([ident, GT, LT, LE, GE, ones], axis=1).astype(np.float32)


def make_in_maps(inputs, cores=range(8)):
    f = lambda a: np.ascontiguousarray(np.asarray(a, dtype=np.float32))
    bf = lambda a: np.ascontiguousarray(np.asarray(a).astype(ml_dtypes.bfloat16))
    Cs, Ss, csc = _dft_tables()
    consts = _consts()
    iota16 = np.tile(np.arange(16, dtype=np.float32)[None, :], (128, 1))
    w_in = f(inputs["w_in"][0])
    w_in_sw = w_in.copy()
    w_in_sw[:, 4608:4632] = w_in[:, 4632:4656]
    w_in_sw[:, 4632:4656] = w_in[:, 4608:4632]
    conv_w = f(inputs["conv_w"][0])
    keysT = f(np.transpose(np.asarray(inputs["sub_keys"][0]).reshape(16, 128, 128), (2, 0, 1)))
    shared = {
        "w_ada": f(inputs["w_ada"][0]), "b_ada": f(inputs["b_ada"]), "gffn": f(inputs["norm_ffn_g"]),
        "gfin": f(np.asarray(inputs["final_norm_g"]).reshape(1, 1024)), "gssm": f(inputs["ssm_norm_g"]),
        "gmix_col": f(np.asarray(inputs["norm_mix_g"][0]).reshape(8, 128).T),
        "convb": f(np.asarray(inputs["conv_b"][0]).reshape(20, 128).T), "dskip": f(inputs["d_skip"]),
        "w_out": f(inputs["w_out"][0]), "w_q": f(inputs["w_query"][0]), "keysT": keysT,
        "e_down": f(inputs["expert_down"][0]), "e_up": f(inputs["expert_up"][0]),
        "consts": consts, "csc": bf(csc), "iota16": iota16,
    }
    dft = {}
    for half in (0, 1):
        if half == 0:
            dft[half] = (bf(Cs[:, :2048]), bf(Ss[:, :2048]))
        else:
            dft[half] = (bf(Cs[::-1, ::-1][:, :2048]), bf(Ss[::-1, ::-1][:, :2048]))
    maps = []
    for core in cores:
        b, half = core // 2, core % 2
        xb = np.asarray(inputs["x"][b], dtype=np.float32)
        m = dict(shared)
        if half == 0:
            m["x"] = f(xb)
            m["w_in"] = w_in
            cwT = conv_w.T
            m["alog"] = f(np.concatenate([inputs["a_log_fwd"][0], inputs["a_log_bwd"][0]]).reshape(1, 48))
            m["dtb"] = f(np.concatenate([inputs["dt_bias_fwd"][0], inputs["dt_bias_bwd"][0]]).reshape(1, 48))
        else:
            m["x"] = f(xb[::-1])
            m["w_in"] = w_in_sw
            cwT = conv_w[::-1].T
            m["alog"] = f(np.concatenate([inputs["a_log_bwd"][0], inputs["a_log_fwd"][0]]).reshape(1, 48))
            m["dtb"] = f(np.concatenate([inputs["dt_bias_bwd"][0], inputs["dt_bias_fwd"][0]]).reshape(1, 48))
        m["convw"] = f(cwT.reshape(20, 128, 5).transpose(1, 0, 2))
        m["c_col"] = f(np.asarray(inputs["c"][b]).reshape(8, 128).T)
        m["dftc"], m["dfts"] = dft[half]
        maps.append(m)
    return maps


def kernel(**inputs):
    nc = build_nc()
    in_maps = make_in_maps(inputs)
    res = run_bass_kernel_spmd(nc, in_maps, core_ids=list(range(8)))
    outp = np.zeros((4, 4096, 1024), dtype=np.float32)
    for core in range(8):
        b, half = core // 2, core % 2
        o = np.asarray(res.results[core]["out"], dtype=np.float32)
        if half == 0:
            outp[b, :2048] = o
        else:
            outp[b, 2048:] = o[::-1]
    return outp
```

```python
import numpy as np
import ml_dtypes
import concourse.bass as bass
import concourse.mybir as mybir
from concourse.bass_utils import run_bass_kernel_spmd
from contextlib import ExitStack

F32 = mybir.dt.float32
BF16 = mybir.dt.bfloat16
I32 = mybir.dt.int32
U32 = mybir.dt.uint32
ALU = mybir.AluOpType
AF = mybir.ActivationFunctionType
AX = mybir.AxisListType
EPS = 1e-6
NEG = -1.0e30


class Buf:
    __slots__ = ("name", "w", "r")

    def __init__(self, name=""):
        self.name = name
        self.w = None
        self.r = {}


class Sched:
    def __init__(self, nc, es, K=8):
        self.nc = nc
        self.eng = {"pe": nc.tensor, "act": nc.scalar, "dve": nc.vector, "pool": nc.gpsimd, "sp": nc.sync}
        self.csem = {e: es.enter_context(nc.semaphore("c_" + e)) for e in ["pe", "act", "dve", "pool"]}
        self.ccnt = {e: 0 for e in self.csem}
        self.K = K
        self.dsem = {q: [es.enter_context(nc.semaphore("d_%s%d" % (q, i))) for i in range(K)] for q in ["sp", "pool"]}
        self.dcnt = {q: 0 for q in self.dsem}
        self.seen = {f: {} for f in self.eng}

    def _sem(self, key):
        if key[0] == "x":
            return self.xsem[key[1]]
        return self.csem[key[1]] if key[0] == "c" else self.dsem[key[1]][key[2]]

    def _wait(self, F, tok, same_ok):
        if tok is None:
            return
        key, val = tok
        if key[0] == "c" and key[1] == F and (same_ok or F == "pe"):
            return
        if self.seen[F].get(key, 0) >= val:
            return
        self.eng[F].wait_ge(self._sem(key), val)
        self.seen[F][key] = val

    def _deps(self, F, r, w):
        for b in r:
            self._wait(F, b.w, False)
        for b in w:
            self._wait(F, b.w, True)
            for key, val in list(b.r.items()):
                self._wait(F, (key, val), True)

    def _mark(self, tok, r, w):
        for b in r:
            if b.r.get(tok[0], 0) < tok[1]:
                b.r[tok[0]] = tok[1]
        for b in w:
            b.w = tok
            b.r = {}

    def op(self, F, fns, r=(), w=()):
        if callable(fns):
            fns = [fns]
        self._deps(F, r, w)
        e = self.eng[F]
        ins = None
        for fn in fns:
            ins = fn(e)
        self.ccnt[F] += 1
        ins.then_inc(self.csem[F], 1)
        self._mark((("c", F), self.ccnt[F]), r, w)

    def dma(self, q, fn, r=(), w=()):
        i = self.dcnt[q]
        self.dcnt[q] += 1
        si = i % self.K
        val = 16 * (i // self.K + 1)
        key = ("d", q, si)
        if val > 16:
            self._wait(q, (key, val - 16), False)
        for b in r:
            self._wait(q, b.w, False)
        for b in w:
            self._wait(q, b.w, False)
            for k2, v2 in list(b.r.items()):
                self._wait(q, (k2, v2), False)
        ins = fn(self.eng[q])
        ins.then_inc(self.dsem[q][si], 16)
        self._mark((key, val), r, w)

    def all_tokens(self):
        toks = [(("c", e), n) for e, n in self.ccnt.items() if n > 0]
        for q, n in self.dcnt.items():
            for si in range(self.K):
                cnt = (n - si + self.K - 1) // self.K if n > si else 0
                if cnt > 0:
                    toks.append((("d", q, si), 16 * cnt))
        return toks

    def barrier(self, engines=("pe", "act", "dve", "pool", "sp")):
        toks = self.all_tokens()
        for F in engines:
            for tok in toks:
                if tok[0] == ("c", F):
                    continue
                self._wait(F, tok, False)


def build_nc(dbg=None):
    dbg = dbg or set()
    nc = bass.Bass("TRN2", target_bir_lowering=False)

    def din(name, shape, dt=F32):
        return nc.dram_tensor(name, shape, dt, kind="ExternalInput").ap()

    def dscr(name, shape, dt):
        return nc.dram_tensor(name, shape, dt, kind="Internal").ap()

    def dout(name, shape, dt=F32):
        return nc.dram_tensor(name, shape, dt, kind="ExternalOutput").ap()

    x = din("x", [4096, 1024])
    c_col = din("c_col", [128, 8])
    w_ada = din("w_ada", [1024, 6144])
    b_ada = din("b_ada", [1, 6144])
    gmix_col = din("gmix_col", [128, 8])
    gffn = din("gffn", [1, 1024])
    gfin = din("gfin", [1, 1024])
    gssm = din("gssm", [1, 1536])
    w_in = din("w_in", [1024, 4656])
    convw = din("convw", [128, 20, 5])
    convb = din("convb", [128, 20])
    alog = din("alog", [1, 48])
    dtb = din("dtb", [1, 48])
    dskip = din("dskip", [1, 24])
    w_out = din("w_out", [2048, 1024])
    w_q = din("w_q", [1024, 2048])
    keysT = din("keysT", [128, 16, 128])
    e_down = din("e_down", [16384, 1024])
    e_up = din("e_up", [16384, 1024])
    consts = din("consts", [128, 6, 128])
    csc = din("csc", [128, 256], BF16)
    dftc = din("dftc", [4096, 2048], BF16)
    dfts = din("dfts", [4096, 2048], BF16)
    iota16 = din("iota16", [128, 16])
    out = dout("out", [2048, 1024])

    projT_d = dscr("projT_d", [3072, 4096], BF16)
    z_d = dscr("z_d", [2048, 1536], BF16)
    mixedT_d = dscr("mixedT_d", [2048, 2048], BF16)
    mod_d = dscr("mod_d", [1, 6144], F32)
    edu_b = dscr("edu_b", [16384, 2048], BF16)
    wq_d = dscr("wq_d", [1024, 2048], BF16)
    wout_d = dscr("wout_d", [2048, 1024], BF16)
    B_wqd, B_woutd = Buf("wq_d"), Buf("wout_d")
    B_edu = Buf("edu_b")
    B_projT, B_z, B_mixed, B_mod = Buf("projT_d"), Buf("z_d"), Buf("mixedT_d"), Buf("mod_d")

    dbg_out = {}
    if "mod" in dbg:
        dbg_out["mod"] = dout("dbg_mod", [1, 6144])
    if "hT" in dbg:
        dbg_out["hT"] = dout("dbg_hT", [128, 8, 4096], BF16)
    if "proj" in dbg:
        dbg_out["proj"] = dout("dbg_proj", [3072, 4096], BF16)
        dbg_out["z"] = dout("dbg_z", [2048, 1536], BF16)
        dbg_out["dt"] = dout("dbg_dt", [128, 32, 48])
        dbg_out["dec"] = dout("dbg_dec", [128, 32, 144])
    if "mixed" in dbg:
        dbg_out["mixed"] = dout("dbg_mixed", [2048, 2048], BF16)
    if "post" in dbg:
        dbg_out["post"] = dout("dbg_post", [128, 3, 4096], BF16)

    with ExitStack() as es0:
        S = Sched(nc, es0)

        def sb(es, name, shape, dt):
            return es.enter_context(nc.sbuf_tensor(name, shape, dt))

        pb = [es0.enter_context(nc.psum_tensor("pb%d" % i, [128, 512], F32)) for i in range(8)]
        Bpb = [Buf("pb%d" % i) for i in range(8)]

        cst = sb(es0, "cst", [128, 6, 128], F32)
        B_cst = Buf("cst")
        identb = sb(es0, "identb", [128, 128], BF16)
        B_identb = Buf("identb")
        epst = sb(es0, "epst", [128, 1], F32)
        B_eps = Buf("eps")
        S.dma("sp", lambda e: e.dma_start(out=cst[:], in_=consts), w=[B_cst])
        S.op("dve", lambda e: e.tensor_copy(out=identb[:], in_=cst[:, 0, :]), r=[B_cst], w=[B_identb])
        S.op("dve", lambda e: e.memset(epst[:], EPS), w=[B_eps])
        ident_f = cst[:, 0, :]
        mGT, mLT, mLE, mGE, ones_f = cst[:, 1, :], cst[:, 2, :], cst[:, 3, :], cst[:, 4, :], cst[:, 5, :]

        def rms_rstd(ssap, Bss, n, rstd_ap, Brstd):
            S.op("act", lambda e: e.activation(out=rstd_ap, in_=ssap, func=AF.Ln, bias=epst[:, 0:1], scale=1.0 / n),
                 r=[Bss, B_eps], w=[Brstd])
            S.op("act", lambda e: e.activation(out=rstd_ap, in_=rstd_ap, func=AF.Exp, scale=-0.5), r=[Brstd], w=[Brstd])

        es_h = ExitStack()
        B_hT = [Buf("hT%d" % i) for i in range(32)]
        g1col = sb(es_h, "g1col", [128, 8], F32)
        shcol = sb(es_h, "shcol", [128, 8], F32)
        B_g1, B_sh = Buf("g1col"), Buf("shcol")

        with ExitStack() as es:
            ccol = sb(es, "ccol", [128, 8], F32)
            cact = sb(es, "cact", [128, 8], F32)
            gmc = sb(es, "gmc", [128, 8], F32)
            modrow = sb(es, "modrow", [1, 6144], F32)
            brow = sb(es, "brow", [1, 6144], F32)
            wa = [sb(es, "wa%d" % i, [128, 8, 1024], F32) for i in range(2)]
            modcol = sb(es, "modcol", [128, 16], F32)
            B_ccol, B_cact, B_gmc, B_modrow, B_brow, B_modcol = (Buf() for _ in range(6))
            B_wa = [Buf(), Buf()]
            S.dma("sp", lambda e: e.dma_start(out=ccol[:], in_=c_col), w=[B_ccol])
            S.dma("sp", lambda e: e.dma_start(out=gmc[:], in_=gmix_col), w=[B_gmc])
            S.dma("sp", lambda e: e.dma_start(out=brow[:], in_=b_ada), w=[B_brow])
            S.op("act", lambda e: e.activation(out=cact[:], in_=ccol[:], func=AF.Silu), r=[B_ccol], w=[B_cact])
            wav = w_ada.rearrange("(k p) n -> p k n", p=128)
            for v in range(6):
                wb_, Bw_ = wa[v % 2], B_wa[v % 2]
                for hf in range(2):
                    S.dma("sp", lambda e: e.dma_start(out=wb_[:, :, hf * 512:(hf + 1) * 512],
                                                      in_=wav[:, :, v * 1024 + hf * 512: v * 1024 + (hf + 1) * 512]), w=[Bw_])
                for hf in range(2):
                    bank = (v * 2 + hf) % 4
                    off = v * 1024 + hf * 512
                    S.op("pe", [lambda e, k=k: e.matmul(pb[bank][0:1, :], lhsT=cact[:, k:k + 1],
                                                        rhs=wb_[:, k, hf * 512:(hf + 1) * 512], start=(k == 0), stop=(k == 7))
                                for k in range(8)], r=[B_cact, Bw_], w=[Bpb[bank]])
                    S.op("dve", lambda e: e.tensor_tensor(out=modrow[0:1, off:off + 512], in0=pb[bank][0:1, :],
                                                          in1=brow[0:1, off:off + 512], op=ALU.add),
                         r=[Bpb[bank], B_brow], w=[B_modrow])
            S.dma("sp", lambda e: e.dma_start(out=mod_d, in_=modrow[0:1, :]), r=[B_modrow], w=[B_mod])
            if "mod" in dbg:
                S.dma("sp", lambda e: e.dma_start(out=dbg_out["mod"], in_=modrow[0:1, :]), r=[B_modrow])
            S.op("pe", [lambda e, j=j: e.matmul(pb[4][:, j:j + 1], lhsT=modrow[0:1, j * 128:(j + 1) * 128],
                                                rhs=cst[0:1, 5, 0:1], start=True, stop=True) for j in range(16)],
                 r=[B_modrow, B_cst], w=[Bpb[4]])
            S.op("dve", lambda e: e.tensor_copy(out=modcol[:], in_=pb[4][:, 0:16]), r=[Bpb[4]], w=[B_modcol])
            S.op("dve", lambda e: e.tensor_copy(out=shcol[:], in_=modcol[:, 0:8]), r=[B_modcol], w=[B_sh])
            S.op("dve", lambda e: e.scalar_tensor_tensor(out=g1col[:], in0=modcol[:, 8:16], scalar=1.0, in1=gmc[:],
                                                         op0=ALU.add, op1=ALU.mult), r=[B_modcol, B_gmc], w=[B_g1])
            S.barrier()

        es_ssd = ExitStack()
        dt_all = sb(es_ssd, "dt_all", [128, 32, 48], F32)
        a_all = sb(es_ssd, "a_all", [128, 32, 48], F32)
        dec_all = sb(es_ssd, "dec_all", [128, 32, 144], F32)
        wd_all = sb(es_ssd, "wd_all", [128, 32, 48], F32)
        B_dt, B_a, B_dec, B_wd = Buf("dt"), Buf("a"), Buf("dec"), Buf("wd")
        es_hT = ExitStack()
        hT = sb(es_hT, "hT", [128, 8, 4096], BF16)
        with ExitStack() as es:
            xt = [sb(es, "xt%d" % i, [128, 1024], F32) for i in range(3)]
            B_xt = [Buf() for _ in range(3)]
            xn = [sb(es, "xn%d" % i, [128, 1024], BF16) for i in range(2)]
            B_xn = [Buf() for _ in range(2)]
            junk = sb(es, "junk1", [128, 1024], F32)
            B_junk = Buf()
            ssq = sb(es, "ssq", [128, 32], F32)
            rstd = sb(es, "rstd", [128, 32], F32)
            B_ssq = [Buf() for _ in range(32)]
            B_rstd = [Buf() for _ in range(32)]
            for i in range(32):
                t_, Bt_ = xt[i % 3], B_xt[i % 3]
                n_, Bn_ = xn[i % 2], B_xn[i % 2]
                S.dma("sp", lambda e: e.dma_start(out=t_[:], in_=x[i * 128:(i + 1) * 128, :]), w=[Bt_])
                S.op("act", lambda e: e.activation(out=junk[:], in_=t_[:], func=AF.Square, accum_out=ssq[:, i:i + 1]),
                     r=[Bt_], w=[B_junk, B_ssq[i]])
                rms_rstd(ssq[:, i:i + 1], B_ssq[i], 1024.0, rstd[:, i:i + 1], B_rstd[i])
                S.op("dve", lambda e: e.tensor_scalar(out=n_[:], in0=t_[:], scalar1=rstd[:, i:i + 1], scalar2=None,
                                                      op0=ALU.mult), r=[Bt_, B_rstd[i]], w=[Bn_])
                bank = i % 2
                pT = pb[bank][:].bitcast(BF16)
                S.op("pe", [lambda e, k=k: e.transpose(out=pT[:, k * 128:(k + 1) * 128], in_=n_[:, k * 128:(k + 1) * 128],
                                                       identity=identb[:]) for k in range(8)],
                     r=[Bn_, B_identb], w=[Bpb[bank]])
                for k in range(8):
                    if k % 2 == 0:
                        S.op("act", lambda e: e.activation(out=hT[:, k, i * 128:(i + 1) * 128], in_=pT[:, k * 128:(k + 1) * 128],
                                                           func=AF.Identity, bias=shcol[:, k:k + 1], scale=g1col[:, k:k + 1]),
                             r=[Bpb[bank], B_g1, B_sh], w=[B_hT[i]])
                    else:
                        S.op("dve", lambda e: e.tensor_scalar(out=hT[:, k, i * 128:(i + 1) * 128], in0=pT[:, k * 128:(k + 1) * 128],
                                                              scalar1=g1col[:, k:k + 1], scalar2=shcol[:, k:k + 1],
                                                              op0=ALU.mult, op1=ALU.add),
                             r=[Bpb[bank], B_g1, B_sh], w=[B_hT[i]])
            if "hT" in dbg:
                S.dma("sp", lambda e: e.dma_start(out=dbg_out["hT"], in_=hT[:]), r=B_hT)
            S.barrier()

        with ExitStack() as es:
            wt = [sb(es, "wt%d" % i, [128, 8, 128], BF16) for i in range(2)]
            B_wt = [Buf(), Buf()]
            stg = [sb(es, "stg%d" % i, [128, 4096], BF16) for i in range(2)]
            B_stg = [Buf(), Buf()]
            wz = sb(es, "wz", [128, 8, 1536], BF16)
            B_wz = Buf()
            zst = [sb(es, "zst%d" % i, [128, 1536], BF16) for i in range(2)]
            B_zst = [Buf(), Buf()]
            wdt = sb(es, "wdt", [128, 8, 48], BF16)
            B_wdt = Buf()
            dtb_b = sb(es, "dtb_b", [128, 48], F32)
            aneg_b = sb(es, "aneg_b", [128, 48], F32)
            B_dtb, B_aneg = Buf(), Buf()
            w_in_v = w_in.rearrange("(k p) n -> p k n", p=128)
            S.dma("pool", lambda e: e.dma_start(out=wdt[:], in_=w_in_v[:, :, 4608:4656]), w=[B_wdt])
            S.dma("sp", lambda e: e.dma_start(out=dtb_b[:], in_=dtb.partition_broadcast(128)), w=[B_dtb])
            S.dma("sp", lambda e: e.dma_start(out=aneg_b[:], in_=alog.partition_broadcast(128)), w=[B_aneg])
            S.op("act", lambda e: e.activation(out=aneg_b[:], in_=aneg_b[:], func=AF.Exp), r=[B_aneg], w=[B_aneg])
            S.op("dve", lambda e: e.tensor_scalar(out=aneg_b[:], in0=aneg_b[:], scalar1=-1.0, scalar2=None, op0=ALU.mult),
                 r=[B_aneg], w=[B_aneg])
            for i in range(32):
                bank = 4 + (i % 2)
                S.op("pe", [lambda e, k=k: e.matmul(pb[bank][:, 0:48], lhsT=hT[:, k, i * 128:(i + 1) * 128], rhs=wdt[:, k, :],
                                                    start=(k == 0), stop=(k == 7)) for k in range(8)],
                     r=[B_hT[i], B_wdt], w=[Bpb[bank]])
                S.op("dve", lambda e: e.tensor_tensor(out=dt_all[:, i, :], in0=pb[bank][:, 0:48], in1=dtb_b[:], op=ALU.add),
                     r=[Bpb[bank], B_dtb], w=[B_dt])
            S.op("act", lambda e: e.activation(out=dt_all[:], in_=dt_all[:], func=AF.Exp), r=[B_dt], w=[B_dt])
            S.op("act", lambda e: e.activation(out=dt_all[:], in_=dt_all[:], func=AF.Ln, bias=1.0, scale=1.0), r=[B_dt], w=[B_dt])
            S.op("dve", lambda e: e.tensor_tensor(out=a_all[:], in0=dt_all[:], in1=aneg_b[:].unsqueeze(1).broadcast_to([128, 32, 48]),
                                                  op=ALU.mult), r=[B_dt, B_aneg], w=[B_a])
            for i in range(32):
                bank = 4 + (i % 2)
                S.op("pe", [
                    lambda e: e.matmul(pb[bank][:, 0:24], lhsT=mLE, rhs=a_all[:, i, 0:24], start=True, stop=True),
                    lambda e: e.matmul(pb[bank][:, 24:48], lhsT=mGT, rhs=a_all[:, i, 0:24], start=True, stop=True),
                    lambda e: e.matmul(pb[bank][:, 48:72], lhsT=mLT, rhs=a_all[:, i, 24:48], start=True, stop=True),
                    lambda e: e.matmul(pb[bank][:, 72:96], lhsT=mGE, rhs=a_all[:, i, 24:48], start=True, stop=True),
                    lambda e: e.matmul(pb[bank][:, 96:144], lhsT=ones_f, rhs=a_all[:, i, 0:48], start=True, stop=True),
                ], r=[B_a, B_cst], w=[Bpb[bank]])
                S.op("act", lambda e: e.activation(out=dec_all[:, i, :], in_=pb[bank][:, 0:144], func=AF.Exp),
                     r=[Bpb[bank]], w=[B_dec])
            S.op("dve", lambda e: e.tensor_tensor(out=wd_all[:], in0=dt_all[:], in1=dec_all[:, :, 24:72], op=ALU.mult),
                 r=[B_dt, B_dec], w=[B_wd])
            if "proj" in dbg:
                S.dma("sp", lambda e: e.dma_start(out=dbg_out["dt"], in_=dt_all[:]), r=[B_dt])
                S.dma("sp", lambda e: e.dma_start(out=dbg_out["dec"], in_=dec_all[:]), r=[B_dec])
            ev = 0
            for j in range(24):
                col0 = j * 128 if j < 4 else 2048 + (j - 4) * 128
                w_, Bw_ = wt[j % 2], B_wt[j % 2]
                s_, Bs_ = stg[j % 2], B_stg[j % 2]
                S.dma("pool", lambda e: e.dma_start(out=w_[:], in_=w_in_v[:, :, col0:col0 + 128]), w=[Bw_])
                for tt in range(8):
                    bank = tt % 4
                    S.op("pe", [lambda e, k=k: e.matmul(pb[bank][:, :], lhsT=w_[:, k, :], rhs=hT[:, k, tt * 512:(tt + 1) * 512],
                                                        start=(k == 0), stop=(k == 7)) for k in range(8)],
                         r=[Bw_] + B_hT[tt * 4:(tt + 1) * 4], w=[Bpb[bank]])
                    if ev % 2 == 0:
                        S.op("act", lambda e: e.activation(out=s_[:, tt * 512:(tt + 1) * 512], in_=pb[bank][:, :], func=AF.Copy),
                             r=[Bpb[bank]], w=[Bs_])
                    else:
                        S.op("dve", lambda e: e.tensor_copy(out=s_[:, tt * 512:(tt + 1) * 512], in_=pb[bank][:, :]),
                             r=[Bpb[bank]], w=[Bs_])
                    ev += 1
                S.dma("sp", lambda e: e.dma_start(out=projT_d[j * 128:(j + 1) * 128, :], in_=s_[:]), r=[Bs_], w=[B_projT])
            S.dma("pool", lambda e: e.dma_start(out=wz[:], in_=w_in_v[:, :, 512:2048]), w=[B_wz])
            for i in range(16):
                z_, Bz_ = zst[i % 2], B_zst[i % 2]
                for n3 in range(3):
                    bank = (i * 3 + n3) % 4
                    S.op("pe", [lambda e, k=k: e.matmul(pb[bank][:, :], lhsT=hT[:, k, i * 128:(i + 1) * 128],
                                                        rhs=wz[:, k, n3 * 512:(n3 + 1) * 512], start=(k == 0), stop=(k == 7))
                                for k in range(8)], r=[B_hT[i], B_wz], w=[Bpb[bank]])
                    S.op("act", lambda e: e.activation(out=z_[:, n3 * 512:(n3 + 1) * 512], in_=pb[bank][:, :], func=AF.Silu),
                         r=[Bpb[bank]], w=[Bz_])
                S.dma("sp", lambda e: e.dma_start(out=z_d[i * 128:(i + 1) * 128, :], in_=z_[:]), r=[Bz_], w=[B_z])
            S.barrier()
            if "proj" in dbg:
                S.dma("sp", lambda e: e.dma_start(out=dbg_out["proj"], in_=projT_d), r=[B_projT])
                S.dma("sp", lambda e: e.dma_start(out=dbg_out["z"], in_=z_d), r=[B_z])
                S.barrier()
        es_hT.close()

        with ExitStack() as es:
            fT = [sb(es, "fT%d" % i, [128, 4096], BF16) for i in range(2)]
            B_fT = [Buf(), Buf()]
            Z = sb(es, "Z", [128, 32, 4, 256], BF16)
            B_Z = [Buf() for _ in range(4)]
            csct = sb(es, "csct", [128, 256], BF16)
            B_csc = Buf()
            dc = [sb(es, "dc%d" % i, [128, 16, 512], BF16) for i in range(2)]
            ds_ = [sb(es, "ds%d" % i, [128, 16, 512], BF16) for i in range(2)]
            B_dc = [Buf(), Buf()]
            B_ds = [Buf(), Buf()]
            ost = [sb(es, "ost%d" % i, [128, 512], BF16) for i in range(2)]
            B_ost = [Buf(), Buf()]
            S.dma("sp", lambda e: e.dma_start(out=csct[:], in_=csc), w=[B_csc])
            dftc_v = dftc.rearrange("(i p) k -> p i k", p=128)
            dfts_v = dfts.rearrange("(i p) k -> p i k", p=128)
            oc = 0
            for g in range(4):
                f_, Bf_ = fT[g % 2], B_fT[g % 2]
                S.dma("sp", lambda e: e.dma_start(out=f_[:], in_=projT_d[g * 128:(g + 1) * 128, :]), r=[B_projT], w=[Bf_])
                for i in range(32):
                    bank = i % 4
                    S.op("pe", lambda e: e.matmul(pb[bank][:, 0:256], lhsT=f_[:, i * 128:(i + 1) * 128], rhs=csct[:],
                                                  start=True, stop=True), r=[Bf_, B_csc], w=[Bpb[bank]])
                    if i % 2 == 0:
                        S.op("act", lambda e: e.activation(out=Z[:, i, g, :], in_=pb[bank][:, 0:256], func=AF.Copy),
                             r=[Bpb[bank]], w=[B_Z[g]])
                    else:
                        S.op("dve", lambda e: e.tensor_copy(out=Z[:, i, g, :], in_=pb[bank][:, 0:256]),
                             r=[Bpb[bank]], w=[B_Z[g]])
            for kt in range(4):
                for hf in range(2):
                    S.dma("sp", lambda e: e.dma_start(out=dc[hf][:], in_=dftc_v[:, hf * 16:(hf + 1) * 16, kt * 512:(kt + 1) * 512]),
                          w=[B_dc[hf]])
                    S.dma("sp", lambda e: e.dma_start(out=ds_[hf][:], in_=dfts_v[:, hf * 16:(hf + 1) * 16, kt * 512:(kt + 1) * 512]),
                          w=[B_ds[hf]])
                for hf in range(2):
                    for g in range(4):
                        bank = 4 + g
                        fns = []
                        for ii in range(16):
                            i = hf * 16 + ii
                            fns.append(lambda e, i=i, ii=ii: e.matmul(pb[bank][:, :], lhsT=Z[:, i, g, 0:128], rhs=dc[hf][:, ii, :],
                                                                      start=(i == 0), stop=False))
                            fns.append(lambda e, i=i, ii=ii: e.matmul(pb[bank][:, :], lhsT=Z[:, i, g, 128:256], rhs=ds_[hf][:, ii, :],
                                                                      start=False, stop=(i == 31)))
                        S.op("pe", fns, r=[B_Z[g], B_dc[hf], B_ds[hf]], w=[Bpb[bank]])
                for g in range(4):
                    bank = 4 + g
                    o_, Bo_ = ost[oc % 2], B_ost[oc % 2]
                    oc += 1
                    if g % 2 == 0:
                        S.op("act", lambda e: e.activation(out=o_[:], in_=pb[bank][:, :], func=AF.Copy), r=[Bpb[bank]], w=[Bo_])
                    else:
                        S.op("dve", lambda e: e.tensor_copy(out=o_[:], in_=pb[bank][:, :]), r=[Bpb[bank]], w=[Bo_])
                    row0 = g * 128
                    S.dma("sp", lambda e: e.dma_start(out=mixedT_d[row0:row0 + 128, kt * 512:(kt + 1) * 512], in_=o_[:]),
                          r=[Bo_], w=[B_mixed])
            S.barrier()

        convsem = es0.enter_context(nc.semaphore("convsem"))
        for k in range(16):
            nc.gpsimd.dma_start(out=edu_b[k * 1024:(k + 1) * 1024, 0:1024], in_=e_down[k * 1024:(k + 1) * 1024, :]).then_inc(convsem, 16)
            nc.gpsimd.dma_start(out=edu_b[k * 1024:(k + 1) * 1024, 1024:2048], in_=e_up[k * 1024:(k + 1) * 1024, :]).then_inc(convsem, 16)
        nc.gpsimd.dma_start(out=wq_d, in_=w_q).then_inc(convsem, 16)
        nc.gpsimd.dma_start(out=wout_d, in_=w_out).then_inc(convsem, 16)
        S.xsem = {"conv": convsem}
        B_edu.w = (("x", "conv"), 16 * 34)
        B_wqd.w = (("x", "conv"), 16 * 34)
        B_woutd.w = (("x", "conv"), 16 * 34)
        with ExitStack() as es:
            pre = [sb(es, "pre%d" % i, [128, 4100], BF16) for i in range(2)]
            B_pre = [Buf(), Buf()]
            dgc = [sb(es, "dgc%d" % i, [128, 5, 128], BF16) for i in range(2)]
            B_dgc = [Buf(), Buf()]
            cvb = [0]
            cw = sb(es, "cw", [128, 20, 5], F32)
            cb = sb(es, "cb", [128, 20], F32)
            B_cw = Buf()
            xTt = sb(es, "xTt", [128, 3, 4096], BF16)
            BTt = sb(es, "BTt", [128, 4096], BF16)
            CTt = sb(es, "CTt", [128, 2048], BF16)
            B_xT = [Buf() for _ in range(3)]
            B_BT, B_CT = Buf(), Buf()
            xtok = sb(es, "xtok", [128, 16, 384], BF16)
            Btok = sb(es, "Btok", [128, 16, 128], BF16)
            B_xtok = [Buf() for _ in range(16)]
            B_Btok = [Buf() for _ in range(16)]
            xtk = [sb(es, "xtk%d" % i, [128, 512], BF16) for i in range(2)]
            B_xtk = [Buf(), Buf()]
            Sb_all = sb(es, "Sb_all", [128, 16, 384], BF16)
            B_Sb = [Buf() for _ in range(16)]
            Sf32 = [sb(es, "Sf32_%d" % i, [128, 384], F32) for i in range(2)]
            B_S32 = [Buf(), Buf()]
            Sfb = sb(es, "Sfb", [128, 384], BF16)
            B_Sfb = Buf()
            xw = [sb(es, "xw%d" % i, [128, 384], BF16) for i in range(4)]
            B_xw = [Buf() for _ in range(4)]
            wsm = sb(es, "wsm", [128, 8, 6], F32)
            Rt = [sb(es, "Rt%d" % i, [128, 6, 128], F32) for i in range(2)]
            Et = [sb(es, "Et%d" % i, [128, 6, 128], BF16) for i in range(2)]
            MT = [sb(es, "MT%d" % i, [128, 6, 128], BF16) for i in range(2)]
            B_R, B_E, B_MT = [Buf(), Buf()], [Buf(), Buf()], [Buf(), Buf()]
            CBm = [sb(es, "CBm%d" % i, [128, 128], F32) for i in range(2)]
            B_CBm = [Buf(), Buf()]
            xwp = [[sb(es, "xwp%d_%d" % (q, i), [128, 384], BF16) for i in range(3)] for q in range(2)]
            B_xwp = [[Buf() for _ in range(3)] for _ in range(2)]
            tCp = [sb(es, "tCp%d" % q, [128, 384], BF16) for q in range(2)]
            B_tCp = [Buf(), Buf()]
            CBmp = [[sb(es, "CBmp%d_%d" % (q, i), [128, 128], BF16) for i in range(2)] for q in range(2)]
            B_CBmp = [[Buf(), Buf()], [Buf(), Buf()]]
            MTp = [[sb(es, "MTp%d_%d" % (q, i), [128, 6, 128], BF16) for i in range(2)] for q in range(2)]
            B_MTp = [[Buf(), Buf()], [Buf(), Buf()]]
            tA = sb(es, "tA", [128, 384], F32)
            tB = sb(es, "tB", [128, 384], F32)
            tC = sb(es, "tC", [128, 384], F32)
            ynb = sb(es, "ynb", [128, 384], BF16)
            B_tA, B_tB, B_tC, B_ynb = Buf(), Buf(), Buf(), Buf()
            zt = [sb(es, "zt%d" % i, [128, 384], BF16) for i in range(2)]
            B_zt = [Buf(), Buf()]
            mst = sb(es, "mst", [128, 3, 2048], BF16)
            B_mst = Buf()
            gssm_b = sb(es, "gssm_b", [128, 1536], F32)
            dsk_b = sb(es, "dsk_b", [128, 24], F32)
            B_gssm, B_dsk = Buf(), Buf()
            ss4 = sb(es, "ss4", [128, 1], F32)
            rs4 = sb(es, "rs4", [128, 1], F32)
            B_ss4, B_rs4 = Buf(), Buf()
            junk4 = sb(es, "junk4", [128, 384], F32)
            B_junk4 = Buf()
            S.dma("sp", lambda e: e.dma_start(out=cw[:], in_=convw), w=[B_cw])
            S.dma("sp", lambda e: e.dma_start(out=cb[:], in_=convb), w=[B_cw])
            S.dma("sp", lambda e: e.dma_start(out=gssm_b[:], in_=gssm.partition_broadcast(128)), w=[B_gssm])
            S.dma("sp", lambda e: e.dma_start(out=dsk_b[:], in_=dskip.partition_broadcast(128)), w=[B_dsk])
            for p_ in pre:
                S.op("dve", lambda e: e.memset(p_[:, 0:2], 0.0), w=[B_pre[0], B_pre[1]])
                S.op("dve", lambda e: e.memset(p_[:, 4098:4100], 0.0), w=[B_pre[0], B_pre[1]])
            pc = 0
            for g in range(4):
                tiles = [(512 + g * 384 + i * 128, g * 3 + i, xTt[:, i, :], B_xT[i], 4096) for i in range(3)]
                tiles.append((512 + 1536 + g * 128, 12 + g, BTt[:, :], B_BT, 4096))
                tiles.append((512 + 2048 + g * 128, 16 + g, CTt[:, :], B_CT, 2048))
                for (row0, ci, dst, Bdst, ntok) in tiles:
                    p_, Bp_ = pre[pc % 2], B_pre[pc % 2]
                    dgc_, Bdgc_ = dgc[pc % 2], B_dgc[pc % 2]
                    pc += 1
                    S.dma("sp", lambda e: e.dma_start(out=p_[:, 2:4098], in_=projT_d[row0:row0 + 128, :]), r=[B_projT], w=[Bp_])
                    for k in range(5):
                        S.op("dve", lambda e: e.tensor_scalar(out=dgc_[:, k, :], in0=identb[:], scalar1=cw[:, ci, k:k + 1], scalar2=None, op0=ALU.mult),
                             r=[B_identb, B_cw], w=[Bdgc_])
                    for tb in range(ntok // 512):
                        bank = 2 + (cvb[0] % 4)
                        cvb[0] += 1
                        o0 = tb * 512
                        S.op("pe", [lambda e, k=k: e.matmul(pb[bank][:, :], lhsT=dgc_[:, k, :], rhs=p_[:, o0 + k:o0 + k + 512],
                                                            start=(k == 0), stop=(k == 4)) for k in range(5)],
                             r=[Bdgc_, Bp_], w=[Bpb[bank]])
                        S.op("act", lambda e: e.activation(out=dst[:, o0:o0 + 512], in_=pb[bank][:, :], func=AF.Silu, bias=cb[:, ci:ci + 1], scale=1.0),
                             r=[Bpb[bank], B_cw], w=[Bdst])
                if "post" in dbg and g == 0:
                    S.dma("sp", lambda e: e.dma_start(out=dbg_out["post"], in_=xTt[:]), r=B_xT)

                hs0 = g * 6

                def dests(c):
                    if c < 16:
                        return xtok[:, c, :], B_xtok[c], Btok[:, c, :], B_Btok[c]
                    k_ = xtk[c % 2]
                    return k_[:, 0:384], B_xtk[c % 2], k_[:, 384:512], B_xtk[c % 2]

                def stT(c):
                    tb_ = 0 if c % 2 == 0 else 3
                    pT = pb[tb_][:].bitcast(BF16)
                    fns = [lambda e, i=i: e.transpose(out=pT[:, i * 128:(i + 1) * 128], in_=xTt[:, i, c * 128:(c + 1) * 128],
                                                      identity=identb[:]) for i in range(3)]
                    fns.append(lambda e: e.transpose(out=pT[:, 384:512], in_=BTt[:, c * 128:(c + 1) * 128], identity=identb[:]))
                    S.op("pe", fns, r=B_xT + [B_BT, B_identb], w=[Bpb[tb_]])

                def stE(c):
                    tb_ = 0 if c % 2 == 0 else 3
                    pT = pb[tb_][:].bitcast(BF16)
                    xdst, Bx, bdst, Bb = dests(c)
                    S.op("act", lambda e: e.activation(out=xdst, in_=pT[:, 0:384], func=AF.Copy), r=[Bpb[tb_]], w=[Bx])
                    S.op("act", lambda e: e.activation(out=bdst, in_=pT[:, 384:512], func=AF.Copy), r=[Bpb[tb_]], w=[Bb])

                def weighted(dst, Bd, xsrc, Bx, wap):
                    S.op("dve", lambda e: e.tensor_tensor(out=dst.rearrange("l (h p) -> l h p", h=6),
                                                          in0=xsrc.rearrange("l (h p) -> l h p", h=6),
                                                          in1=wap.unsqueeze(2).broadcast_to([128, 6, 64]), op=ALU.mult),
                         r=[Bx, B_dt, B_wd, B_dec, B_dsk], w=[Bd])

                def recur(Sold, Bold, Snew, Bnew, st_bank, etot_ap, first):
                    if first:
                        S.op("dve", lambda e: e.tensor_copy(out=Snew[:], in_=pb[st_bank][:, 0:384]), r=[Bpb[st_bank]], w=[Bnew])
                    else:
                        S.op("dve", lambda e: e.tensor_tensor(out=Snew[:].rearrange("n (h p) -> n h p", h=6),
                                                              in0=Sold[:].rearrange("n (h p) -> n h p", h=6),
                                                              in1=etot_ap.unsqueeze(2).broadcast_to([128, 6, 64]), op=ALU.mult),
                             r=[Bold, B_dec], w=[Bnew])
                        S.op("dve", lambda e: e.tensor_tensor(out=Snew[:], in0=Snew[:], in1=pb[st_bank][:, 0:384], op=ALU.add),
                             r=[Bnew, Bpb[st_bank]], w=[Bnew])

                curA = [0]

                def stW(c):
                    xd, Bx, bd, Bb = dests(c)
                    weighted(xw[c % 2][:], B_xw[c % 2], xd, Bx, wd_all[:, c, 24 + hs0:24 + hs0 + 6])

                def stM(c):
                    xd, Bx, bd, Bb = dests(c)
                    sbk = 1 + (c % 2)
                    S.op("pe", lambda e: e.matmul(pb[sbk][:, 0:384], lhsT=bd, rhs=xw[c % 2][:], start=True, stop=True),
                         r=[Bb, B_xw[c % 2]], w=[Bpb[sbk]])

                def stR(c):
                    sbk = 1 + (c % 2)
                    cur = curA[0]
                    nxt = 1 - cur
                    recur(Sf32[cur], B_S32[cur], Sf32[nxt], B_S32[nxt], sbk, dec_all[:, c, 96 + 24 + hs0:96 + 24 + hs0 + 6], first=(c == 31))
                    curA[0] = nxt
                    if c - 1 < 16:
                        S.op("act", lambda e: e.activation(out=Sb_all[:, c - 1, :], in_=Sf32[nxt][:], func=AF.Copy),
                             r=[B_S32[nxt]], w=[B_Sb[c - 1]])

                stages = [stT, stE, stW, stM, stR]
                for k in range(32 + 4):
                    for si in (4, 3, 2, 1, 0):
                        c = 31 - (k - si)
                        if c < 0 or c > 31:
                            continue
                        if si >= 2 and c == 0:
                            continue
                        stages[si](c)

                curB = [0]

                def front(c):
                    q = c % 2
                    z_, Bz_ = zt[q], B_zt[q]
                    S.dma("sp", lambda e: e.dma_start(out=z_[:], in_=z_d[c * 128:(c + 1) * 128, g * 384:(g + 1) * 384]), r=[B_z], w=[Bz_])
                    xs, Bxs = xtok[:, c, :], B_xtok[c]
                    S.op("pe", lambda e: e.matmul(pb[4][:, 0:128], lhsT=BTt[:, c * 128:(c + 1) * 128], rhs=CTt[:, c * 128:(c + 1) * 128],
                                                  start=True, stop=True), r=[B_BT, B_CT], w=[Bpb[4]])
                    yield
                    for d in range(2):
                        acol = d * 24 + hs0
                        U = mLE if d == 0 else mGE
                        S.op("act", [lambda e, h=h: e.activation(out=Rt[d][:, h, :], in_=U, func=AF.Copy, scale=a_all[:, c, acol + h:acol + h + 1])
                                     for h in range(6)], r=[B_cst, B_a], w=[B_R[d]])
                        yield
                    for d in range(2):
                        acol = d * 24 + hs0
                        weighted(xwp[q][d][:], B_xwp[q][d], xs, Bxs, dt_all[:, c, acol:acol + 6])
                        yield
                    for d in range(2):
                        LT = mGT if d == 0 else mLT
                        Rf = Rt[d][:].rearrange("m h l -> m (h l)")
                        S.op("pe", [lambda e: e.matmul(pb[2][:, :], lhsT=LT, rhs=Rf[:, 0:512], start=True, stop=True),
                                    lambda e: e.matmul(pb[3][:, 0:256], lhsT=LT, rhs=Rf[:, 512:768], start=True, stop=True)],
                             r=[B_R[d], B_cst], w=[Bpb[2], Bpb[3]])
                        yield
                        if d == 0:
                            if c < 15:
                                weighted(xwp[q][2][:], B_xwp[q][2], xs, Bxs, wd_all[:, c, hs0:hs0 + 6])
                                yield
                            weighted(tCp[q][:], B_tCp[q], xs, Bxs, dsk_b[:, hs0:hs0 + 6])
                            yield
                        Ef = Et[d][:].rearrange("m h l -> m (h l)")
                        S.op("act", lambda e: e.activation(out=Ef[:, 0:512], in_=pb[2][:, :], func=AF.Exp), r=[Bpb[2]], w=[B_E[d]])
                        yield
                        S.op("act", lambda e: e.activation(out=Ef[:, 512:768], in_=pb[3][:, 0:256], func=AF.Exp), r=[Bpb[3]], w=[B_E[d]])
                        yield
                        if d == 0:
                            S.op("dve", lambda e: e.tensor_tensor(out=CBmp[q][0][:], in0=pb[4][:, 0:128], in1=mLE, op=ALU.mult),
                                 r=[Bpb[4], B_cst], w=[B_CBmp[q][0]])
                            yield
                            S.op("dve", lambda e: e.tensor_tensor(out=CBmp[q][1][:], in0=pb[4][:, 0:128], in1=mGE, op=ALU.mult),
                                 r=[Bpb[4], B_cst], w=[B_CBmp[q][1]])
                            yield
                    for d in range(2):
                        S.op("dve", lambda e: e.tensor_tensor(out=MTp[q][d][:], in0=Et[d][:],
                                                              in1=CBmp[q][d][:].unsqueeze(1).broadcast_to([128, 6, 128]), op=ALU.mult),
                             r=[B_E[d], B_CBmp[q][d]], w=[B_MTp[q][d]])
                        yield

                def back(c):
                    q = c % 2
                    z_, Bz_ = zt[q], B_zt[q]
                    MT_, B_MT_ = MTp[q], B_MTp[q]
                    xw_, B_xw_ = xwp[q], B_xwp[q]
                    fns = []
                    for h in range(6):
                        fns.append(lambda e, h=h: e.matmul(pb[6][:, h * 64:(h + 1) * 64], lhsT=MT_[0][:, h, :], rhs=xw_[0][:, h * 64:(h + 1) * 64],
                                                           start=True, stop=False))
                        fns.append(lambda e, h=h: e.matmul(pb[6][:, h * 64:(h + 1) * 64], lhsT=MT_[1][:, h, :], rhs=xw_[1][:, h * 64:(h + 1) * 64],
                                                           start=False, stop=False))
                        fns.append(lambda e, h=h: e.matmul(pb[6][:, h * 64:(h + 1) * 64], lhsT=identb[:], rhs=tCp[q][:, h * 64:(h + 1) * 64],
                                                           start=False, stop=True))
                    S.op("pe", fns, r=[B_MT_[0], B_MT_[1], B_xw_[0], B_xw_[1], B_tCp[q], B_identb], w=[Bpb[6]])
                    yield
                    S.op("pe", lambda e: e.matmul(pb[7][:, 0:384], lhsT=CTt[:, c * 128:(c + 1) * 128], rhs=Sb_all[:, c, :], start=True, stop=True),
                         r=[B_CT, B_Sb[c]], w=[Bpb[7]])
                    yield
                    if c < 15:
                        S.op("pe", lambda e: e.matmul(pb[1][:, 0:384], lhsT=Btok[:, c, :], rhs=xw_[2][:], start=True, stop=True),
                             r=[B_Btok[c], B_xw_[2]], w=[Bpb[1]])
                        yield
                    S.op("dve", lambda e: e.tensor_tensor(out=tA[:].rearrange("l (h p) -> l h p", h=6),
                                                          in0=pb[7][:, 0:384].rearrange("l (h p) -> l h p", h=6),
                                                          in1=dec_all[:, c, 72 + hs0:72 + hs0 + 6].unsqueeze(2).broadcast_to([128, 6, 64]),
                                                          op=ALU.mult), r=[Bpb[7], B_dec], w=[B_tA])
                    yield
                    if c > 0:
                        S.op("pe", lambda e: e.matmul(pb[7][:, 0:384], lhsT=CTt[:, c * 128:(c + 1) * 128], rhs=Sfb[:], start=True, stop=True),
                             r=[B_CT, B_Sfb], w=[Bpb[7]])
                        yield
                    if c < 15:
                        cur = curB[0]
                        nxt = 1 - cur
                        recur(Sf32[cur], B_S32[cur], Sf32[nxt], B_S32[nxt], 1, dec_all[:, c, 96 + hs0:96 + hs0 + 6], first=(c == 0))
                        curB[0] = nxt
                        yield
                    if c > 0:
                        S.op("dve", lambda e: e.tensor_tensor(out=tB[:].rearrange("l (h p) -> l h p", h=6),
                                                              in0=pb[7][:, 0:384].rearrange("l (h p) -> l h p", h=6),
                                                              in1=dec_all[:, c, hs0:hs0 + 6].unsqueeze(2).broadcast_to([128, 6, 64]),
                                                              op=ALU.mult), r=[Bpb[7], B_dec], w=[B_tB])
                        yield
                        S.op("dve", lambda e: e.tensor_tensor(out=tA[:], in0=tA[:], in1=tB[:], op=ALU.add), r=[B_tA, B_tB], w=[B_tA])
                        yield
                    if c < 15:
                        S.op("act", lambda e: e.activation(out=Sfb[:], in_=Sf32[curB[0]][:], func=AF.Copy), r=[B_S32[curB[0]]], w=[B_Sfb])
                        yield
                    S.op("dve", lambda e: e.tensor_tensor(out=tA[:], in0=tA[:], in1=pb[6][:, 0:384], op=ALU.add), r=[B_tA, Bpb[6]], w=[B_tA])
                    yield
                    S.op("dve", lambda e: e.tensor_tensor(out=tA[:], in0=tA[:], in1=z_[:], op=ALU.mult), r=[B_tA, Bz_], w=[B_tA])
                    yield
                    S.op("act", lambda e: e.activation(out=junk4[:], in_=tA[:], func=AF.Square, accum_out=ss4[:, 0:1]),
                         r=[B_tA], w=[B_junk4, B_ss4])
                    yield
                    rms_rstd(ss4[:, 0:1], B_ss4, 384.0, rs4[:, 0:1], B_rs4)
                    yield
                    S.op("dve", lambda e: e.scalar_tensor_tensor(out=ynb[:], in0=tA[:], scalar=rs4[:, 0:1], in1=gssm_b[:, g * 384:(g + 1) * 384],
                                                                 op0=ALU.mult, op1=ALU.mult), r=[B_tA, B_rs4, B_gssm], w=[B_ynb])
                    yield
                    pT = pb[0][:].bitcast(BF16)
                    S.op("pe", [lambda e, i=i: e.transpose(out=pT[:, i * 128:(i + 1) * 128], in_=ynb[:, i * 128:(i + 1) * 128], identity=identb[:])
                                for i in range(3)], r=[B_ynb, B_identb], w=[Bpb[0]])
                    yield
                    S.op("act", lambda e: e.activation(out=mst[:, :, c * 128:(c + 1) * 128], in_=pT[:, 0:384].rearrange("p (i l) -> p i l", i=3),
                                                       func=AF.Copy), r=[Bpb[0]], w=[B_mst])
                    yield

                for _ in front(0):
                    pass
                for c in range(16):
                    gb = back(c)
                    gf = front(c + 1) if c + 1 < 16 else None
                    while gb is not None or gf is not None:
                        if gb is not None:
                            try:
                                next(gb)
                            except StopIteration:
                                gb = None
                        if gf is not None:
                            try:
                                next(gf)
                            except StopIteration:
                                gf = None
                for i in range(3):
                    row0 = 512 + g * 384 + i * 128
                    S.dma("sp", lambda e: e.dma_start(out=mixedT_d[row0:row0 + 128, :], in_=mst[:, i, :]), r=[B_mst], w=[B_mixed])
            S.barrier()
        es_ssd.close()
        if "mixed" in dbg:
            S.dma("sp", lambda e: e.dma_start(out=dbg_out["mixed"], in_=mixedT_d), r=[B_mixed])
            S.barrier()

        with ExitStack() as es:
            wout = sb(es, "wout", [128, 16, 1024], BF16)
            wq = sb(es, "wq", [128, 8, 2048], BF16)
            kT = sb(es, "kT", [128, 16, 128], BF16)
            B_wout, B_wq, B_kT = Buf(), Buf(), Buf()
            S.dma("sp", lambda e: e.dma_start(out=wout[:], in_=wout_d.rearrange("(k p) n -> p k n", p=128)), r=[B_woutd], w=[B_wout])
            S.dma("sp", lambda e: e.dma_start(out=wq[:], in_=wq_d.rearrange("(k p) n -> p k n", p=128)), r=[B_wqd], w=[B_wq])
            S.dma("pool", lambda e: e.dma_start(out=kT[:], in_=keysT), w=[B_kT])
            gm_b = sb(es, "gm_b", [128, 1024], F32)
            g2_b = sb(es, "g2_b", [128, 1024], F32)
            sf_b = sb(es, "sf_b", [128, 1024], F32)
            gf_b = sb(es, "gf_b", [128, 1024], F32)
            gfin_b = sb(es, "gfin_b", [128, 1024], F32)
            ot = sb(es, "ot", [128, 1024], F32)
            B_rows, B_ot = Buf(), Buf()
            io16 = sb(es, "io16", [128, 16], F32)
            S.dma("sp", lambda e: e.dma_start(out=io16[:], in_=iota16), w=[B_rows])
            S.dma("sp", lambda e: e.dma_start(out=gm_b[:], in_=mod_d[0:1, 2048:3072].partition_broadcast(128)), r=[B_mod], w=[B_rows])
            S.dma("sp", lambda e: e.dma_start(out=sf_b[:], in_=mod_d[0:1, 3072:4096].partition_broadcast(128)), r=[B_mod], w=[B_rows])
            S.dma("sp", lambda e: e.dma_start(out=g2_b[:], in_=mod_d[0:1, 4096:5120].partition_broadcast(128)), r=[B_mod], w=[B_rows])
            S.dma("sp", lambda e: e.dma_start(out=gf_b[:], in_=mod_d[0:1, 5120:6144].partition_broadcast(128)), r=[B_mod], w=[B_rows])
            S.dma("sp", lambda e: e.dma_start(out=gfin_b[:], in_=gfin.partition_broadcast(128)), w=[B_rows])
            S.dma("sp", lambda e: e.dma_start(out=ot[:], in_=gffn.partition_broadcast(128)), w=[B_ot])
            S.op("dve", lambda e: e.scalar_tensor_tensor(out=g2_b[:], in0=g2_b[:], scalar=1.0, in1=ot[:], op0=ALU.add, op1=ALU.mult),
                 r=[B_rows, B_ot], w=[B_rows])
            mT = sb(es, "mT", [128, 16, 128], BF16)
            xt5 = sb(es, "xt5", [128, 1024], F32)
            B_mT, B_xt5 = Buf(), Buf()
            h2f, B_h2f = xt5, B_xt5
            x1 = [sb(es, "x1_%d" % i, [128, 1024], F32) for i in range(2)]
            h2b = [sb(es, "h2b_%d" % i, [128, 1024], BF16) for i in range(2)]
            B_x1, B_h2b = [Buf(), Buf()], [Buf(), Buf()]
            h2T = sb(es, "h2T", [128, 8, 128], BF16)
            qT = sb(es, "qT", [128, 16, 128], BF16)
            sc = [sb(es, "sc%d" % i, [128, 4, 128], F32) for i in range(2)]
            sc2 = [sb(es, "sc2_%d" % i, [128, 256], F32) for i in range(2)]
            B_sc2 = [Buf() for _ in range(2)]
            B_h2T, B_qT = Buf(), Buf()
            B_tl = [Buf() for _ in range(16)]
            B_ohh = [Buf() for _ in range(4)]
            B_il = [Buf() for _ in range(16)]
            B_cvl = [Buf() for _ in range(8)]
            B_cpl = [Buf() for _ in range(8)]
            B_sc = [Buf(), Buf()]
            junkr = [sb(es, "junkr%d" % i, [128, 1024], BF16) for i in range(3)]
            B_junkr = [Buf() for _ in range(3)]
            junkb, B_junkb = junkr[0], B_junkr[0]
            ss5 = sb(es, "ss5", [128, 2], F32)
            rs5 = sb(es, "rs5", [128, 2], F32)
            B_ss5, B_rs5 = [Buf(), Buf()], [Buf(), Buf()]
            tv = sb(es, "tv", [128, 16, 16], F32)
            ti = sb(es, "ti", [128, 16, 16], U32)
            tif = sb(es, "tif", [128, 16, 16], F32)
            B_tv, B_ti, B_tif = Buf(), Buf(), Buf()
            cand = sb(es, "cand", [128, 8, 256], F32)
            B_cand = Buf()
            oh = cand[:].rearrange("p h (a b) -> p h a b", a=16)
            cv = sb(es, "cv", [128, 8, 16], F32)
            cpos = sb(es, "cpos", [128, 8, 16], U32)
            cpa = sb(es, "cpa", [128, 8, 16], U32)
            cpb_ = sb(es, "cpb", [128, 8, 16], U32)
            cpaf = sb(es, "cpaf", [128, 8, 16], F32)
            cpbf = sb(es, "cpbf", [128, 8, 16], F32)
            B_cv, B_cpos, B_cpa, B_cpb, B_cpaf, B_cpbf = (Buf() for _ in range(6))
            Iv = sb(es, "Iv", [128, 8, 16], F32)
            Jv = sb(es, "Jv", [128, 8, 16], F32)
            idxf = sb(es, "idxf", [128, 128], F32)
            idxi = [sb(es, "idxi%d" % i, [128, 128], I32) for i in range(2)]
            gate = [sb(es, "gate%d" % i, [128, 8, 16], F32) for i in range(2)]
            gsum = sb(es, "gsum", [128, 8], F32)
            B_Iv, B_Jv, B_idxf, B_gsum = (Buf() for _ in range(4))
            B_idxi, B_gate = [Buf(), Buf()], [Buf(), Buf()]
            actv = sb(es, "actv", [128, 128], F32)
            wv = sb(es, "wv", [128, 128], F32)
            GS = 4
            NGRP = 128 // GS
            B_actg = [Buf() for _ in range(NGRP)]
            B_wvg = [Buf() for _ in range(NGRP)]
            NG = 13
            Rg = sb(es, "Rg", [128, NG * 2048], BF16)
            B_gt = [Buf() for _ in range(NG)]
            NDG = 3
            dg = [sb(es, "dg%d" % i, [128, 128], BF16) for i in range(NDG)]
            B_dg = [Buf() for _ in range(NDG)]
            mixv = mixedT_d.rearrange("(k p) t -> p k t", p=128)
            rot = [0]

            def qbank():
                rot[0] += 1
                return 5 + rot[0] % 3

            def gen5a(i):
                p = i % 2
                S.dma("sp", lambda e: e.dma_start(out=mT[:], in_=mixv[:, :, i * 128:(i + 1) * 128]), r=[B_mixed], w=[B_mT])
                S.dma("sp", lambda e: e.dma_start(out=xt5[:], in_=x[i * 128:(i + 1) * 128, :]), w=[B_xt5])

                yield
                for kk in range(4):
                    fns = []
                    for hf in range(2):
                        for k4 in range(4):
                            k = kk * 4 + k4
                            fns.append(lambda e, k=k, hf=hf: e.matmul(pb[2 + hf][:, :], lhsT=mT[:, k, :], rhs=wout[:, k, hf * 512:(hf + 1) * 512],
                                                                      start=(k == 0), stop=(k == 15)))
                    S.op("pe", fns, r=[B_mT, B_wout], w=[Bpb[2], Bpb[3]])
                    yield
                yield
                for hf in range(2):
                    bk = 2 + hf
                    S.op("dve", lambda e: e.tensor_tensor(out=x1[p][:, hf * 512:(hf + 1) * 512], in0=pb[bk][:, :], in1=gm_b[:, hf * 512:(hf + 1) * 512],
                                                          op=ALU.mult), r=[Bpb[bk], B_rows], w=[B_x1[p]])
                yield
                S.op("dve", lambda e: e.tensor_tensor(out=x1[p][:], in0=x1[p][:], in1=xt5[:], op=ALU.add), r=[B_x1[p], B_xt5], w=[B_x1[p]])
                yield
                S.op("act", lambda e: e.activation(out=h2f[:], in_=x1[p][:], func=AF.Square, accum_out=ss5[:, 0:1]),
                     r=[B_x1[p]], w=[B_h2f, B_ss5[0]])
                yield
                S.op("act", lambda e: e.activation(out=rs5[:, 0:1], in_=ss5[:, 0:1], func=AF.Ln, bias=epst[:, 0:1], scale=1.0 / 1024.0),
                     r=[B_ss5[0], B_eps], w=[B_rs5[0]])
                yield
                S.op("act", lambda e: e.activation(out=rs5[:, 0:1], in_=rs5[:, 0:1], func=AF.Exp, scale=-0.5), r=[B_rs5[0]], w=[B_rs5[0]])
                yield
                S.op("dve", lambda e: e.scalar_tensor_tensor(out=h2f[:], in0=x1[p][:], scalar=rs5[:, 0:1], in1=g2_b[:], op0=ALU.mult, op1=ALU.mult),
                     r=[B_x1[p], B_rs5[0], B_rows], w=[B_h2f])
                yield
                S.op("dve", lambda e: e.tensor_tensor(out=h2b[p][:], in0=h2f[:], in1=sf_b[:], op=ALU.add), r=[B_h2f, B_rows], w=[B_h2b[p]])

                yield
                yield
                pT = pb[4][:].bitcast(BF16)
                S.op("pe", [lambda e, k=k: e.transpose(out=pT[:, k * 128:(k + 1) * 128], in_=h2b[p][:, k * 128:(k + 1) * 128], identity=identb[:])
                            for k in range(8)], r=[B_h2b[p], B_identb], w=[Bpb[4]])
                yield
                yield
                S.op("act", lambda e: e.activation(out=h2T[:].rearrange("p k t -> p (k t)"), in_=pT[:, :], func=AF.Copy), r=[Bpb[4]], w=[B_h2T])
                yield
                yield
                qb = [5, 6, 7, 5]
                for q4 in range(5):
                    if q4 < 4:
                        bk = qb[q4]
                        fns = []
                        for j in range(4):
                            hs = q4 * 4 + j
                            for k in range(8):
                                fns.append(lambda e, hs=hs, j=j, k=k: e.matmul(pb[bk][:, j * 128:(j + 1) * 128], lhsT=wq[:, k, hs * 128:(hs + 1) * 128],
                                                                               rhs=h2T[:, k, :], start=(k == 0), stop=(k == 7)))
                        S.op("pe", fns, r=[B_wq, B_h2T], w=[Bpb[bk]])
                        yield
                    if q4 > 0:
                        qq = q4 - 1
                        bk = qb[qq]
                        S.op("act", lambda e: e.activation(out=qT[:, qq * 4:(qq + 1) * 4, :].rearrange("p j t -> p (j t)"), in_=pb[bk][:, :], func=AF.Copy),
                             r=[Bpb[bk]], w=[B_qT])
                        yield
                yield
                sb_ = [6, 7, 5, 6]

                def sc_pe(q4):
                    bk = sb_[q4]
                    S.op("pe", [lambda e, j=j: e.matmul(pb[bk][:, j * 128:(j + 1) * 128], lhsT=qT[:, q4 * 4 + j, :], rhs=kT[:, q4 * 4 + j, :],
                                                        start=True, stop=True) for j in range(4)], r=[B_qT, B_kT], w=[Bpb[bk]])

                def sc_act(q4):
                    bk = sb_[q4]
                    s_, Bs_ = sc[q4 % 2], B_sc[q4 % 2]
                    S.op("act", lambda e: e.activation(out=s_[:].rearrange("p j t -> p (j t)"), in_=pb[bk][:, :], func=AF.Copy),
                         r=[Bpb[bk]], w=[Bs_])

                sc_pe(0)
                yield
                sc_pe(1)
                yield
                sc_act(0)
                yield
                for q4 in range(4):
                    s_, Bs_ = sc[q4 % 2], B_sc[q4 % 2]
                    if q4 == 0:
                        sc_act(1)
                    for pr in range(2):
                        js = [pr * 2, pr * 2 + 1]
                        for j in js:
                            hs = q4 * 4 + j
                            S.op("dve", lambda e: e.max(out=tv[:, hs, 0:8], in_=s_[:, j, :]), r=[Bs_], w=[B_tl[hs]])
                        yield
                        for j in js:
                            hs = q4 * 4 + j
                            S.op("dve", lambda e: e.max_index(out=ti[:, hs, 0:8], in_max=tv[:, hs, 0:8], in_values=s_[:, j, :]), r=[Bs_, B_tl[hs]], w=[B_il[hs]])
                        yield
                        for j in js:
                            hs = q4 * 4 + j
                            S.op("dve", lambda e: e.match_replace(out=sc2[j % 2][:, 0:128], in_to_replace=tv[:, hs, 0:8], in_values=s_[:, j, :], imm_value=NEG),
                                 r=[Bs_, B_tl[hs]], w=[B_sc2[j % 2]])
                        if pr == 0 and q4 + 2 < 4:
                            sc_pe(q4 + 2)
                        yield
                        for j in js:
                            hs = q4 * 4 + j
                            S.op("dve", lambda e: e.max(out=tv[:, hs, 8:16], in_=sc2[j % 2][:, 0:128]), r=[B_sc2[j % 2]], w=[B_tl[hs]])
                        yield
                        for j in js:
                            hs = q4 * 4 + j
                            S.op("dve", lambda e: e.max_index(out=ti[:, hs, 8:16], in_max=tv[:, hs, 8:16], in_values=sc2[j % 2][:, 0:128]), r=[B_sc2[j % 2], B_tl[hs]], w=[B_il[hs]])
                        yield
                    if 2 <= q4 + 1 < 4:
                        sc_act(q4 + 1)
                        yield
                S.op("dve", lambda e: e.tensor_copy(out=tif[:], in_=ti[:]), r=B_il, w=[B_tif])
                tv4 = tv[:].rearrange("p (h j) a -> p h j a", j=2)
                tif4 = tif[:].rearrange("p (h j) a -> p h j a", j=2)
                S.op("dve", lambda e: e.tensor_tensor(out=cand[:].rearrange("p h (a b) -> p h a b", a=16),
                                                      in0=tv4[:, :, 0, :].unsqueeze(3).broadcast_to([128, 8, 16, 16]),
                                                      in1=tv4[:, :, 1, :].unsqueeze(2).broadcast_to([128, 8, 16, 16]), op=ALU.add),
                     r=B_tl, w=[B_cand])
                yield
                for h2_ in range(4):
                    hl = [(h2_ * 2, 0), (h2_ * 2 + 1, 1)]
                    for h, j in hl:
                        S.op("dve", lambda e: e.max(out=cv[:, h, 0:8], in_=cand[:, h, :]), r=[B_cand], w=[B_cvl[h]])
                    yield
                    for h, j in hl:
                        S.op("dve", lambda e: e.max_index(out=cpos[:, h, 0:8], in_max=cv[:, h, 0:8], in_values=cand[:, h, :]), r=[B_cand, B_cvl[h]], w=[B_cpl[h]])
                    yield
                    for h, j in hl:
                        S.op("dve", lambda e: e.match_replace(out=sc2[j][:, :], in_to_replace=cv[:, h, 0:8], in_values=cand[:, h, :], imm_value=NEG),
                             r=[B_cand, B_cvl[h]], w=[B_sc2[j % 2]])
                    yield
                    for h, j in hl:
                        S.op("dve", lambda e: e.max(out=cv[:, h, 8:16], in_=sc2[j][:, :]), r=[B_sc2[j % 2]], w=[B_cvl[h]])
                    yield
                    for h, j in hl:
                        S.op("dve", lambda e: e.max_index(out=cpos[:, h, 8:16], in_max=cv[:, h, 8:16], in_values=sc2[j][:, :]), r=[B_sc2[j], B_cvl[h]], w=[B_cpl[h]])
                    yield
                S.op("dve", lambda e: e.tensor_single_scalar(out=cpa[:], in_=cpos[:], scalar=4, op=ALU.logical_shift_right), r=B_cpl, w=[B_cpa])
                S.op("dve", lambda e: e.tensor_single_scalar(out=cpb_[:], in_=cpos[:], scalar=15, op=ALU.bitwise_and), r=B_cpl, w=[B_cpb])
                S.op("dve", lambda e: e.tensor_copy(out=cpaf[:], in_=cpa[:]), r=[B_cpa], w=[B_cpaf])
                S.op("dve", lambda e: e.tensor_copy(out=cpbf[:], in_=cpb_[:]), r=[B_cpb], w=[B_cpbf])
                yield
                for (pf, Bpf, side, dstv, Bdst) in [(cpaf, B_cpaf, 0, Iv, B_Iv), (cpbf, B_cpbf, 1, Jv, B_Jv)]:
                    for hh in range(4):
                        hsl = slice(hh * 2, hh * 2 + 2)
                        S.op("dve", lambda e: e.tensor_tensor(out=oh[:, hsl], in0=pf[:, hsl, :].unsqueeze(3).broadcast_to([128, 2, 16, 16]),
                                                              in1=io16[:].unsqueeze(1).unsqueeze(1).broadcast_to([128, 2, 16, 16]), op=ALU.is_equal),
                             r=[Bpf, B_rows], w=[B_ohh[hh]])
                        yield
                        S.op("dve", lambda e: e.tensor_tensor(out=oh[:, hsl], in0=oh[:, hsl], in1=tif4[:, hsl, side, :].unsqueeze(2).broadcast_to([128, 2, 16, 16]),
                                                              op=ALU.mult), r=[B_ohh[hh], B_tif], w=[B_ohh[hh]])
                        yield
                        S.op("dve", lambda e: e.tensor_reduce(out=dstv[:, hsl, :], in_=oh[:, hsl], axis=AX.X, op=ALU.add), r=[B_ohh[hh]], w=[Bdst])
                        yield
                S.op("dve", lambda e: e.scalar_tensor_tensor(out=idxf[:], in0=Iv[:].rearrange("p h k -> p (h k)"), scalar=128.0,
                                                             in1=Jv[:].rearrange("p h k -> p (h k)"), op0=ALU.mult, op1=ALU.add),
                     r=[B_Iv, B_Jv], w=[B_idxf])
                S.op("dve", lambda e: e.tensor_copy(out=idxi[p][:], in_=idxf[:]), r=[B_idxf], w=[B_idxi[p]])
                yield
                S.op("dve", lambda e: e.tensor_tensor(out=gate[p][:], in0=cv[:], in1=cv[:, :, 0:1].broadcast_to([128, 8, 16]), op=ALU.subtract),
                     r=B_cvl, w=[B_gate[p]])
                S.op("act", lambda e: e.activation(out=gate[p][:], in_=gate[p][:], func=AF.Exp), r=[B_gate[p]], w=[B_gate[p]])
                S.op("dve", lambda e: e.tensor_reduce(out=gsum[:], in_=gate[p][:], axis=AX.X, op=ALU.add), r=[B_gate[p]], w=[B_gsum])
                S.op("dve", lambda e: e.reciprocal(out=gsum[:], in_=gsum[:]), r=[B_gsum], w=[B_gsum])
                S.op("dve", lambda e: e.tensor_tensor(out=gate[p][:], in0=gate[p][:], in1=gsum[:].unsqueeze(2).broadcast_to([128, 8, 16]), op=ALU.mult),
                     r=[B_gate[p], B_gsum], w=[B_gate[p]])
                yield

            gcn = [0]
            dcn = [0]
            edu3 = edu_b.rearrange("e (a d) -> e a d", a=2)

            def S1(i, grp):
                p = i % 2
                ul = []
                for s_ in range(grp * GS, (grp + 1) * GS):
                    n = gcn[0]
                    gcn[0] += 1
                    u_ = n % NG
                    od = u_ * 2048
                    S.dma("pool", lambda e: e.indirect_dma_start(out=Rg[:, od:od + 2048], out_offset=None, in_=edu_b,
                                                                 in_offset=bass.IndirectOffsetOnAxis(ap=idxi[p][:, s_:s_ + 1], axis=0)),
                          r=[B_idxi[p], B_edu], w=[B_gt[u_]])
                    jb_, Bjb_ = junkr[n % 3], B_junkr[n % 3]
                    S.op("dve", lambda e: e.tensor_tensor(out=jb_[:], in0=Rg[:, od:od + 1024], in1=h2b[p][:], op=ALU.mult),
                         r=[B_gt[u_], B_h2b[p]], w=[Bjb_])
                    S.op("act", lambda e: e.activation(out=jb_[:], in_=jb_[:], func=AF.Copy, accum_out=actv[:, s_:s_ + 1]),
                         r=[Bjb_], w=[Bjb_, B_actg[grp]])
                    ul.append(u_)
                    tick()
                return ul

            def S2(i, grp):
                p = i % 2
                s0 = grp * GS
                gflat = gate[p][:].rearrange("p h k -> p (h k)")
                S.op("act", lambda e: e.activation(out=wv[:, s0:s0 + GS], in_=actv[:, s0:s0 + GS], func=AF.Gelu), r=[B_actg[grp]], w=[B_wvg[grp]])

            def S3(i, grp, ul):
                p = i % 2
                gflat = gate[p][:].rearrange("p h k -> p (h k)")
                for j, s_ in enumerate(range(grp * GS, (grp + 1) * GS)):
                    u_ = ul[j]
                    ou = u_ * 2048 + 1024
                    d_ = dcn[0] % NDG
                    dcn[0] += 1
                    S.op("dve", lambda e: e.tensor_scalar(out=dg[d_][:], in0=identb[:], scalar1=wv[:, s_:s_ + 1], scalar2=gflat[:, s_:s_ + 1],
                                                          op0=ALU.mult, op1=ALU.mult),
                         r=[B_identb, B_wvg[grp], B_gate[p]], w=[B_dg[d_]])
                    S.op("pe", [lambda e, hf=hf: e.matmul(pb[hf][:, :], lhsT=dg[d_][:], rhs=Rg[:, ou + hf * 512:ou + (hf + 1) * 512],
                                                          start=(s_ == 0), stop=(s_ == 127)) for hf in range(2)],
                         r=[B_dg[d_], B_gt[u_]], w=[Bpb[0], Bpb[1]])

            def fin(i):
                p = i % 2
                for hf in range(2):
                    S.op("dve", lambda e: e.tensor_tensor(out=ot[:, hf * 512:(hf + 1) * 512], in0=pb[hf][:, :], in1=gf_b[:, hf * 512:(hf + 1) * 512], op=ALU.mult),
                         r=[Bpb[hf], B_rows], w=[B_ot])
                S.op("dve", lambda e: e.tensor_tensor(out=ot[:], in0=ot[:], in1=x1[p][:], op=ALU.add), r=[B_ot, B_x1[p]], w=[B_ot])
                S.op("act", lambda e: e.activation(out=junkb[:], in_=ot[:], func=AF.Square, accum_out=ss5[:, 1:2]),
                     r=[B_ot], w=[B_junkb, B_ss5[1]])
                rms_rstd(ss5[:, 1:2], B_ss5[1], 1024.0, rs5[:, 1:2], B_rs5[1])
                S.op("dve", lambda e: e.scalar_tensor_tensor(out=ot[:], in0=ot[:], scalar=rs5[:, 1:2], in1=gfin_b[:], op0=ALU.mult, op1=ALU.mult),
                     r=[B_ot, B_rs5[1], B_rows], w=[B_ot])
                S.dma("sp", lambda e: e.dma_start(out=out[i * 128:(i + 1) * 128, :], in_=ot[:]), r=[B_ot])

            for _ in gen5a(0):
                pass
            TOT = 16 * NGRP
            uls = {}
            nxt = None
            nxt_box = [None]

            def tick():
                if nxt_box[0] is not None:
                    try:
                        next(nxt_box[0])
                    except StopIteration:
                        nxt_box[0] = None
            for G in range(TOT + 1):
                if G < TOT:
                    i, grp = divmod(G, NGRP)
                    if grp == 0 and nxt_box[0] is not None:
                        for _ in nxt_box[0]:
                            pass
                        nxt_box[0] = None
                    uls[G] = S1(i, grp)
                    S2(i, grp)
                if 0 <= G - 1 < TOT:
                    i2, g2 = divmod(G - 1, NGRP)
                    S3(i2, g2, uls.pop(G - 1))
                    if g2 == NGRP - 1:
                        fin(i2)
                if G < TOT and grp == 1 and i + 1 < 16:
                    nxt_box[0] = gen5a(i + 1)
            S.barrier()
        S.barrier(engines=("sp",))
        es_h.close()
    return nc


def _dft_tables():
    n = np.arange(4096, dtype=np.int64)
    prod = (n[:, None] * n[None, :]) % 4096
    ang = 2.0 * np.pi * prod.astype(np.float64) / 4096.0
    Cs = np.cos(ang) / 64.0
    Ss = np.sin(ang) / 64.0
    c = np.arange(128, dtype=np.int64)
    angc = 2.0 * np.pi * ((c[:, None] * c[None, :]) % 128).astype(np.float64) / 128.0
    csc = np.concatenate([np.cos(angc), -np.sin(angc)], axis=1) / np.sqrt(128.0)
    return Cs, Ss, csc


def _consts():
    m = np.arange(128)
    ident = np.eye(128)
    GT = (m[:, None] > m[None, :]).astype(np.float64)
    LT = (m[:, None] < m[None, :]).astype(np.float64)
    LE = (m[:, None] <= m[None, :]).astype(np.float64)
    GE = (m[:, None] >= m[None, :]).astype(np.float64)
    ones = np.ones((128, 128))
    return np.stack([ident, GT, LT, LE, GE, ones], axis=1).astype(np.float32)


def make_in_maps(inputs, cores=range(8)):
    f = lambda a: np.ascontiguousarray(np.asarray(a, dtype=np.float32))
    bf = lambda a: np.ascontiguousarray(np.asarray(a).astype(ml_dtypes.bfloat16))
    Cs, Ss, csc = _dft_tables()
    consts = _consts()
    iota16 = np.tile(np.arange(16, dtype=np.float32)[None, :], (128, 1))
    w_in = f(inputs["w_in"][0])
    w_in_sw = w_in.copy()
    w_in_sw[:, 4608:4632] = w_in[:, 4632:4656]
    w_in_sw[:, 4632:4656] = w_in[:, 4608:4632]
    conv_w = f(inputs["conv_w"][0])
    keysT = f(np.transpose(np.asarray(inputs["sub_keys"][0]).reshape(16, 128, 128), (2, 0, 1)))
    shared = {
        "w_ada": f(inputs["w_ada"][0]), "b_ada": f(inputs["b_ada"]), "gffn": f(inputs["norm_ffn_g"]),
        "gfin": f(np.asarray(inputs["final_norm_g"]).reshape(1, 1024)), "gssm": f(inputs["ssm_norm_g"]),
        "gmix_col": f(np.asarray(inputs["norm_mix_g"][0]).reshape(8, 128).T),
        "convb": f(np.asarray(inputs["conv_b"][0]).reshape(20, 128).T), "dskip": f(inputs["d_skip"]),
        "w_out": f(inputs["w_out"][0]), "w_q": f(inputs["w_query"][0]), "keysT": keysT,
        "e_down": f(inputs["expert_down"][0]), "e_up": f(inputs["expert_up"][0]),
        "consts": consts, "csc": bf(csc), "iota16": iota16,
    }
    dft = {}
    for half in (0, 1):
        if half == 0:
            dft[half] = (bf(Cs[:, :2048]), bf(Ss[:, :2048]))
        else:
            dft[half] = (bf(Cs[::-1, ::-1][:, :2048]), bf(Ss[::-1, ::-1][:, :2048]))
    maps = []
    for core in cores:
        b, half = core // 2, core % 2
        xb = np.asarray(inputs["x"][b], dtype=np.float32)
        m = dict(shared)
        if half == 0:
            m["x"] = f(xb)
            m["w_in"] = w_in
            cwT = conv_w.T
            m["alog"] = f(np.concatenate([inputs["a_log_fwd"][0], inputs["a_log_bwd"][0]]).reshape(1, 48))
            m["dtb"] = f(np.concatenate([inputs["dt_bias_fwd"][0], inputs["dt_bias_bwd"][0]]).reshape(1, 48))
        else:
            m["x"] = f(xb[::-1])
            m["w_in"] = w_in_sw
            cwT = conv_w[::-1].T
            m["alog"] = f(np.concatenate([inputs["a_log_bwd"][0], inputs["a_log_fwd"][0]]).reshape(1, 48))
            m["dtb"] = f(np.concatenate([inputs["dt_bias_bwd"][0], inputs["dt_bias_fwd"][0]]).reshape(1, 48))
        m["convw"] = f(cwT.reshape(20, 128, 5).transpose(1, 0, 2))
        m["c_col"] = f(np.asarray(inputs["c"][b]).reshape(8, 128).T)
        m["dftc"], m["dfts"] = dft[half]
        maps.append(m)
    return maps


def kernel(**inputs):
    nc = build_nc()
    in_maps = make_in_maps(inputs)
    res = run_bass_kernel_spmd(nc, in_maps, core_ids=list(range(8)))
    outp = np.zeros((4, 4096, 1024), dtype=np.float32)
    for core in range(8):
        b, half = core // 2, core % 2
        o = np.asarray(res.results[core]["out"], dtype=np.float32)
        if half == 0:
            outp[b, :2048] = o
        else:
            outp[b, 2048:] = o[::-1]
    return outp
```
